# Optimizing a Trainium2 kernel written in Bass

```python
import jax, jax.numpy as jnp
from jax import lax
import numpy as np

D_MODEL = 1024
BATCH = 8
SEQ = 4096
DEPTH = 2

MLA_HEADS = 8
QK_NOPE_DIM = 64
QK_ROPE_DIM = 32
V_HEAD_DIM = 64
QK_HEAD_DIM = QK_NOPE_DIM + QK_ROPE_DIM
Q_LORA_RANK = D_MODEL // 4
KV_LORA_RANK = D_MODEL // 8
ROPE_BASE = 10000.0
Q_BLOCK = 128
LRU_WIDTH = D_MODEL // 2
LRU_HEADS = 8
LRU_HEAD_DIM = LRU_WIDTH // LRU_HEADS
CONV_WIDTH = 4
LRU_C = 8.0
IN_SPLITS = (Q_LORA_RANK, KV_LORA_RANK, QK_ROPE_DIM, LRU_WIDTH, LRU_WIDTH, D_MODEL, D_MODEL)
D_IN = sum(IN_SPLITS)
D_FF = 11 * D_MODEL // 4
N_EXPERTS = 8
TOP_K = 2
D_FF_EXPERT = 7 * D_MODEL // 2
MOE_BLOCK = 256
N_DENSE = (DEPTH + 1) // 2
N_MOE = DEPTH // 2
RMS_EPS = 1e-6

kernel_name = 'hybrid_mla_rglru_moe_block'


def rms_norm(x, g):
    xf = x.astype(jnp.float32)
    y = xf * lax.rsqrt(jnp.mean(xf * xf, axis=-1, keepdims=True) + RMS_EPS)
    return (y * g.astype(jnp.float32)).astype(x.dtype)


def rope_tables(positions):
    inv_freq = ROPE_BASE ** (-jnp.arange(0, QK_ROPE_DIM, 2, dtype=jnp.float32) / QK_ROPE_DIM)
    ang = positions.astype(jnp.float32)[..., None] * inv_freq
    return jnp.cos(ang)[:, :, None, :], jnp.sin(ang)[:, :, None, :]


def apply_rope(x, cos, sin):
    x1, x2 = jnp.split(x.astype(jnp.float32), 2, axis=-1)
    return jnp.concatenate([x1 * cos - x2 * sin, x2 * cos + x1 * sin], axis=-1).astype(x.dtype)


def causal_block_attention(q, k, v):
    b, s, h, dq = q.shape
    n_blk = s // Q_BLOCK
    scale = QK_HEAD_DIM ** -0.5
    q_blocks = q.reshape(b, n_blk, Q_BLOCK, h, dq).transpose(1, 0, 2, 3, 4)
    key_idx = jnp.arange(s)

    def one_block(args):
        q_blk, i = args
        sc = jnp.einsum('bqhd,bkhd->bhqk', q_blk, k, preferred_element_type=jnp.float32) * scale
        q_idx = i * Q_BLOCK + jnp.arange(Q_BLOCK)
        sc = jnp.where(key_idx[None, :] <= q_idx[:, None], sc, -jnp.inf)
        p = jax.nn.softmax(sc, axis=-1).astype(v.dtype)
        return jnp.einsum('bhqk,bkhd->bqhd', p, v)

    o = lax.map(one_block, (q_blocks, jnp.arange(n_blk)))
    return o.transpose(1, 0, 2, 3, 4).reshape(b, s, h, v.shape[-1])


def linear_recurrence(a, b):
    def combine(left, right):
        a_l, b_l = left
        a_r, b_r = right
        return a_l * a_r, a_r * b_l + b_r
    _, h = lax.associative_scan(combine, (a, b), axis=1)
    return h


def hybrid_mixer(h, cos, sin, w_in, q_norm_g, w_uq, kv_norm_g, w_ukv, qk_q_g, qk_k_g, w_up_attn,
                 conv_w, conv_b, w_rg, b_rg, w_ig, b_ig, lru_lambda, w_up_lru, w_o):
    b, s, _ = h.shape
    proj = h @ w_in
    c_q, c_kv, k_r, x_lru, g_lru, gate_a, gate_b = jnp.split(
        proj, list(np.cumsum(IN_SPLITS)[:-1]), axis=-1)

    q = (rms_norm(c_q, q_norm_g) @ w_uq).reshape(b, s, MLA_HEADS, QK_HEAD_DIM)
    kv = (rms_norm(c_kv, kv_norm_g) @ w_ukv).reshape(b, s, MLA_HEADS, QK_NOPE_DIM + V_HEAD_DIM)
    k_nope, v = jnp.split(kv, [QK_NOPE_DIM], axis=-1)
    k_rope = jnp.broadcast_to(k_r[:, :, None, :], (b, s, MLA_HEADS, QK_ROPE_DIM))
    k = jnp.concatenate([k_nope, k_rope], axis=-1)
    q = rms_norm(q, qk_q_g)
    k = rms_norm(k, qk_k_g)
    q = jnp.concatenate([q[..., :QK_NOPE_DIM], apply_rope(q[..., QK_NOPE_DIM:], cos, sin)], axis=-1)
    k = jnp.concatenate([k[..., :QK_NOPE_DIM], apply_rope(k[..., QK_NOPE_DIM:], cos, sin)], axis=-1)
    o_attn = causal_block_attention(q, k, v).reshape(b, s, MLA_HEADS * V_HEAD_DIM)
    u_a = o_attn @ w_up_attn

    xc = lax.conv_general_dilated(
        x_lru, conv_w[:, None, :], window_strides=(1,), padding=[(CONV_WIDTH - 1, 0)],
        dimension_numbers=('NWC', 'WIO', 'NWC'), feature_group_count=LRU_WIDTH) + conv_b
    xh = xc.reshape(b, s, LRU_HEADS, LRU_HEAD_DIM)
    r = jax.nn.sigmoid((jnp.einsum('bshi,hij->bshj', xh, w_rg).reshape(b, s, LRU_WIDTH) + b_rg).astype(jnp.float32))
    i = jax.nn.sigmoid((jnp.einsum('bshi,hij->bshj', xh, w_ig).reshape(b, s, LRU_WIDTH) + b_ig).astype(jnp.float32))
    log_a = -LRU_C * r * jax.nn.softplus(-lru_lambda.astype(jnp.float32))
    a = jnp.exp(log_a)
    mult = jnp.sqrt(-jnp.expm1(2.0 * log_a))
    hs = linear_recurrence(a, mult * i * xc.astype(jnp.float32))
    y_lru = (hs * jax.nn.gelu(g_lru.astype(jnp.float32), approximate=True)).astype(h.dtype)
    u_b = y_lru @ w_up_lru

    merged = jax.nn.sigmoid(gate_a) * u_a + jax.nn.sigmoid(gate_b) * u_b
    return merged @ w_o


def swiglu(h, wg, wu, wd):
    return (jax.nn.silu(h @ wg) * (h @ wu)) @ wd


def moe_swiglu(h2, router, wg, wu, wd):
    n_tok, d = h2.shape
    n_assign = n_tok * TOP_K
    logits = jnp.matmul(h2, router, preferred_element_type=jnp.float32)
    top_logit, top_e = lax.top_k(logits, TOP_K)
    top_w = jax.nn.softmax(top_logit, axis=-1)
    flat_e = top_e.reshape(-1).astype(jnp.int32)
    flat_tok = jnp.repeat(jnp.arange(n_tok, dtype=jnp.int32), TOP_K)
    flat_w = top_w.reshape(-1)
    se, stok, sw = lax.sort((flat_e, flat_tok, flat_w), num_keys=1, is_stable=True)
    counts = jnp.bincount(flat_e, length=N_EXPERTS)
    padded = (counts + MOE_BLOCK - 1) // MOE_BLOCK * MOE_BLOCK
    start = jnp.cumsum(counts) - counts
    pstart = jnp.cumsum(padded) - padded
    pend = pstart + padded
    dest = pstart[se] + jnp.arange(n_assign, dtype=jnp.int32) - start[se]
    n_blocks = -(-(n_assign + N_EXPERTS * (MOE_BLOCK - 1)) // MOE_BLOCK)
    n_rows = n_blocks * MOE_BLOCK
    tok_buf = jnp.full((n_rows,), n_tok, jnp.int32).at[dest].set(stok)
    w_buf = jnp.zeros((n_rows,), jnp.float32).at[dest].set(sw)
    block_rows = jnp.arange(n_blocks, dtype=jnp.int32) * MOE_BLOCK
    block_e = jnp.minimum(jnp.sum(block_rows[:, None] >= pend[None, :], axis=1), N_EXPERTS - 1)
    xg = jnp.take(h2, tok_buf, axis=0, mode='fill', fill_value=0)

    def expert_block(args):
        xb, e = args
        return swiglu(xb, wg[e], wu[e], wd[e])

    yb = lax.map(expert_block, (xg.reshape(n_blocks, MOE_BLOCK, d), block_e)).reshape(n_rows, d)
    return jnp.zeros_like(h2).at[tok_buf].add(yb * w_buf[:, None].astype(h2.dtype), mode='drop')


def setup_inputs(seed: int = 0) -> dict:
    key = jax.random.key(seed)
    ks = jax.random.split(key, 32)
    f32 = jnp.float32
    nrm = lambda k, shape, fan_in: jax.random.normal(k, shape, f32) * (fan_in ** -0.5)
    gain = lambda k, shape: 1.0 + 0.01 * jax.random.normal(k, shape, f32)
    small = lambda k, shape: 0.01 * jax.random.normal(k, shape, f32)
    res_scale = (2 * DEPTH) ** -0.5
    x = jax.random.normal(ks[0], (BATCH, SEQ, D_MODEL), f32)
    offsets = jax.random.randint(ks[1], (BATCH, 1), 0, 1024, dtype=jnp.int32)
    positions = offsets + jnp.arange(SEQ, dtype=jnp.int32)[None, :]
    u = jax.random.uniform(ks[2], (DEPTH, LRU_WIDTH), f32, minval=0.9, maxval=0.999)
    a0 = u ** (1.0 / LRU_C)
    lru_lambda = jnp.log(a0) - jnp.log1p(-a0)
    return {
        'x': x,
        'positions': positions,
        'norm1_g': gain(ks[3], (DEPTH, D_MODEL)),
        'w_in': nrm(ks[4], (DEPTH, D_MODEL, D_IN), D_MODEL),
        'q_norm_g': gain(ks[5], (DEPTH, Q_LORA_RANK)),
        'w_uq': nrm(ks[6], (DEPTH, Q_LORA_RANK, MLA_HEADS * QK_HEAD_DIM), Q_LORA_RANK),
        'kv_norm_g': gain(ks[7], (DEPTH, KV_LORA_RANK)),
        'w_ukv': nrm(ks[8], (DEPTH, KV_LORA_RANK, MLA_HEADS * (QK_NOPE_DIM + V_HEAD_DIM)), KV_LORA_RANK),
        'qk_q_g': gain(ks[9], (DEPTH, QK_HEAD_DIM)),
        'qk_k_g': gain(ks[10], (DEPTH, QK_HEAD_DIM)),
        'w_up_attn': nrm(ks[11], (DEPTH, MLA_HEADS * V_HEAD_DIM, D_MODEL), MLA_HEADS * V_HEAD_DIM),
        'conv_w': nrm(ks[12], (DEPTH, CONV_WIDTH, LRU_WIDTH), CONV_WIDTH),
        'conv_b': small(ks[13], (DEPTH, LRU_WIDTH)),
        'w_rg': nrm(ks[14], (DEPTH, LRU_HEADS, LRU_HEAD_DIM, LRU_HEAD_DIM), LRU_HEAD_DIM),
        'b_rg': small(ks[15], (DEPTH, LRU_WIDTH)),
        'w_ig': nrm(ks[16], (DEPTH, LRU_HEADS, LRU_HEAD_DIM, LRU_HEAD_DIM), LRU_HEAD_DIM),
        'b_ig': small(ks[17], (DEPTH, LRU_WIDTH)),
        'lru_lambda': lru_lambda,
        'w_up_lru': nrm(ks[18], (DEPTH, LRU_WIDTH, D_MODEL), LRU_WIDTH),
        'w_o': nrm(ks[19], (DEPTH, D_MODEL, D_MODEL), D_MODEL) * res_scale,
        'norm2_g': gain(ks[20], (DEPTH, D_MODEL)),
        'ffn_w_gate': nrm(ks[21], (N_DENSE, D_MODEL, D_FF), D_MODEL),
        'ffn_w_up': nrm(ks[22], (N_DENSE, D_MODEL, D_FF), D_MODEL),
        'ffn_w_down': nrm(ks[23], (N_DENSE, D_FF, D_MODEL), D_FF) * res_scale,
        'moe_router': nrm(ks[24], (N_MOE, D_MODEL, N_EXPERTS), D_MODEL),
        'moe_w_gate': nrm(ks[25], (N_MOE, N_EXPERTS, D_MODEL, D_FF_EXPERT), D_MODEL),
        'moe_w_up': nrm(ks[26], (N_MOE, N_EXPERTS, D_MODEL, D_FF_EXPERT), D_MODEL),
        'moe_w_down': nrm(ks[27], (N_MOE, N_EXPERTS, D_FF_EXPERT, D_MODEL), D_FF_EXPERT) * res_scale,
    }


def reference(x, positions, norm1_g, w_in, q_norm_g, w_uq, kv_norm_g, w_ukv, qk_q_g, qk_k_g,
              w_up_attn, conv_w, conv_b, w_rg, b_rg, w_ig, b_ig, lru_lambda, w_up_lru, w_o,
              norm2_g, ffn_w_gate, ffn_w_up, ffn_w_down, moe_router, moe_w_gate, moe_w_up,
              moe_w_down):
    cos, sin = rope_tables(positions)
    for layer in range(DEPTH):
        h = rms_norm(x, norm1_g[layer])
        x = x + hybrid_mixer(
            h, cos, sin, w_in[layer], q_norm_g[layer], w_uq[layer], kv_norm_g[layer], w_ukv[layer],
            qk_q_g[layer], qk_k_g[layer], w_up_attn[layer], conv_w[layer], conv_b[layer],
            w_rg[layer], b_rg[layer], w_ig[layer], b_ig[layer], lru_lambda[layer],
            w_up_lru[layer], w_o[layer])
        h = rms_norm(x, norm2_g[layer])
        j = layer // 2
        if layer % 2 == 0:
            x = x + swiglu(h, ffn_w_gate[j], ffn_w_up[j], ffn_w_down[j])
        else:
            y = moe_swiglu(h.reshape(-1, D_MODEL), moe_router[j], moe_w_gate[j], moe_w_up[j], moe_w_down[j])
            x = x + y.reshape(x.shape)
    return x
```

```python
import numpy as np
from contextlib import ExitStack
import concourse.bass as bass
import concourse.mybir as mybir
from concourse.bass_utils import run_bass_kernel_spmd

F32 = mybir.dt.float32
BF16 = mybir.dt.bfloat16
I32 = mybir.dt.int32
ALU = mybir.AluOpType
AF = mybir.ActivationFunctionType
AX = mybir.AxisListType


class Eng:
    def __init__(self, name, handle, sem, same_sync):
        self.name = name
        self.h = handle
        self.sem = sem
        self.count = 0
        self.seen = {}
        self.ops = []
        self.same_sync = same_sync
        self.dma_sems = []
        self.dma_tgt = []
        self.dma_k = 0


class Buf:
    def __init__(self, t, name=""):
        self.t = t
        self.name = name
        self.w = None
        self.r = {}

    def __getitem__(self, idx):
        return self.t[idx]

    def sub(self, key):
        if not hasattr(self, "_subs"):
            self._subs = {}
        if key not in self._subs:
            self._subs[key] = Buf(self.t, f"{self.name}.{key}")
        return self._subs[key]

    def all(self):
        return list(getattr(self, "_subs", {}).values())


class K:
    def __init__(self, nc, es, n_dma_sems=8):
        self.nc = nc
        self.es = es
        mk = lambda nm: es.enter_context(nc.semaphore(nm))
        self.pe = Eng("pe", nc.tensor, mk("s_pe"), False)
        self.dve = Eng("dve", nc.vector, mk("s_dve"), True)
        self.act = Eng("act", nc.scalar, mk("s_act"), True)
        self.pool = Eng("pool", nc.gpsimd, mk("s_pool"), True)
        self.sp = Eng("sp", nc.sync, mk("s_sp"), False)
        self.engs = [self.pe, self.dve, self.act, self.pool, self.sp]
        for q in (self.sp, self.act, self.pool):
            for i in range(n_dma_sems):
                q.dma_sems.append(mk(f"d_{q.name}{i}"))
                q.dma_tgt.append(0)
        self.nbuf = 0

    def sb(self, shape, dtype, name=None, stack=None):
        self.nbuf += 1
        name = name or f"sb{self.nbuf}"
        t = (stack or self.es).enter_context(self.nc.sbuf_tensor(f"{name}_{self.nbuf}", list(shape), dtype))
        return Buf(t, name)

    def ps(self, shape, dtype, name=None, stack=None):
        self.nbuf += 1
        name = name or f"ps{self.nbuf}"
        t = (stack or self.es).enter_context(self.nc.psum_tensor(f"{name}_{self.nbuf}", list(shape), dtype))
        return Buf(t, name)

    def dram(self, name, shape, dtype, kind="Internal"):
        t = self.nc.dram_tensor(name, list(shape), dtype, kind=kind)
        return Buf(t.ap(), name)

    def _collect(self, eng, reads, writes):
        need = {}

        def add(ev):
            if ev is None:
                return
            sem, val, key = ev
            if key == id(eng.sem) and not eng.same_sync:
                return
            if eng.seen.get(key, 0) >= val:
                return
            if key not in need or need[key][1] < val:
                need[key] = ev

        for b in reads:
            add(b.w)
        for b in writes:
            add(b.w)
            for ev in b.r.values():
                add(ev)
        for key, (sem, val, _) in need.items():
            eng.ops.append(("wait", sem, val))
            eng.seen[key] = val

    def _mark(self, ev, reads, writes):
        key = ev[2]
        for b in reads:
            old = b.r.get(key)
            if old is None or old[1] < ev[1]:
                b.r[key] = ev
        for b in writes:
            b.w = ev
            b.r = {}

    def op(self, eng, fn, reads=(), writes=(), inc=True):
        self._collect(eng, reads, writes)
        eng.ops.append(("op", fn, inc))
        if inc:
            eng.count += 1
            ev = (eng.sem, eng.count, id(eng.sem))
        else:
            ev = (eng.sem, eng.count + 1, id(eng.sem))
        self._mark(ev, reads, writes)

    def dma(self, q, out, in_, reads=(), writes=()):
        self._collect(q, reads, writes)
        i = q.dma_k % len(q.dma_sems)
        q.dma_k += 1
        sem = q.dma_sems[i]
        prev = q.dma_tgt[i]
        if prev > 0 and q.seen.get(id(sem), 0) < prev:
            q.ops.append(("wait", sem, prev))
            q.seen[id(sem)] = prev
        tgt = prev + 16
        q.dma_tgt[i] = tgt
        q.ops.append(("dma", out, in_, sem))
        ev = (sem, tgt, id(sem))
        self._mark(ev, reads, writes)

    def wait_all_dma(self, eng):
        for q in (self.sp, self.act, self.pool):
            for sem, tgt in zip(q.dma_sems, q.dma_tgt):
                if tgt > 0 and eng.seen.get(id(sem), 0) < tgt:
                    eng.ops.append(("wait", sem, tgt))
                    eng.seen[id(sem)] = tgt

    def simulate(self):
        names = {}
        for e in self.engs:
            names[id(e.sem)] = e.name
            for i, d in enumerate(e.dma_sems):
                names[id(d)] = f"dma_{e.name}{i}"
        if not hasattr(self, "_simval"):
            self._simval = {}
        val = self._simval
        pos = {e.name: 0 for e in self.engs}
        progress = True
        while progress:
            progress = False
            for e in self.engs:
                while pos[e.name] < len(e.ops):
                    o = e.ops[pos[e.name]]
                    if o[0] == "wait":
                        if val.get(id(o[1]), 0) >= o[2]:
                            pos[e.name] += 1; progress = True
                        else:
                            break
                    elif o[0] == "op":
                        if o[2]:
                            val[id(e.sem)] = val.get(id(e.sem), 0) + 1
                        pos[e.name] += 1; progress = True
                    else:
                        val[id(o[3])] = val.get(id(o[3]), 0) + 16
                        pos[e.name] += 1; progress = True
        stuck = [e for e in self.engs if pos[e.name] < len(e.ops)]
        if stuck:
            msg = []
            for e in stuck:
                o = e.ops[pos[e.name]]
                nxt = next((x for x in e.ops[pos[e.name]:] if x[0] != "wait"), None)
                line = nxt[1].__code__.co_firstlineno if nxt is not None and nxt[0] == "op" else "dma"
                msg.append(f"{e.name} blocked at op#{pos[e.name]} waiting {names.get(id(o[1]))}>={o[2]} (now {val.get(id(o[1]), 0)}), next op from source line {line}")
            raise RuntimeError("DEADLOCK in recorded program:\n" + "\n".join(msg))

    def emit(self):
        nc = self.nc
        with nc.Block() as block:
            def replay(eng):
                def body(h):
                    for o in eng.ops:
                        if o[0] == "wait":
                            h.wait_ge(o[1], o[2])
                        elif o[0] == "op":
                            ins = o[1](h)
                            if o[2]:
                                ins.then_inc(eng.sem, 1)
                        else:
                            h.dma_start(out=o[1], in_=o[2]).then_inc(o[3], 16)
                    eng.ops = []
                return body
            block.tensor(replay(self.pe))
            block.vector(replay(self.dve))
            block.scalar(replay(self.act))
            block.gpsimd(replay(self.pool))
            block.sync(replay(self.sp))


S = 4096; D = 1024; NT = 32; NCH = 8
EPS = 1e-6
TWO_PI = 6.283185307179586
C1 = 6.28125
C2 = TWO_PI - C1
PI_SAFE = 3.1415925
MAGIC = 12582912.0
D_FF = 2816; D_FFE = 3584; NE = 8


class PsPool:
    def __init__(self, bufs):
        self.bufs = bufs; self.i = 0
    def next(self):
        b = self.bufs[self.i % len(self.bufs)]; self.i += 1
        return b


def bc(ap, shape, axis):
    return ap.unsqueeze(axis).to_broadcast(list(shape))


def build(debug=None, stop_after=None):
    nc = bass.Bass("TRN2", target_bir_lowering=False)
    def din(name, shape, dt=F32):
        return nc.dram_tensor(name, list(shape), dt, kind="ExternalInput").ap()
    I = {}
    I["x"] = din("x", [S, D]); I["pos"] = din("pos", [128, NT], I32)
    I["ident"] = din("ident", [128, 128]); I["tri"] = din("tri", [128, 128]); I["invf"] = din("invf", [128, 16])
    for L in range(2):
        I[f"w_in{L}"] = din(f"w_in{L}", [1024, 3488]); I[f"g1{L}"] = din(f"g1{L}", [128, 8])
        I[f"w_uq{L}"] = din(f"w_uq{L}", [256, 768]); I[f"qng{L}"] = din(f"qng{L}", [128, 2])
        I[f"w_ukv{L}"] = din(f"w_ukv{L}", [128, 1024]); I[f"kvng{L}"] = din(f"kvng{L}", [128, 1])
        I[f"qkg{L}"] = din(f"qkg{L}", [128, 192]); I[f"w_upa{L}"] = din(f"w_upa{L}", [512, 1024])
        I[f"convw{L}"] = din(f"convw{L}", [128, 4, 4]); I[f"lruv{L}"] = din(f"lruv{L}", [128, 4, 4])
        I[f"w_rg{L}"] = din(f"w_rg{L}", [128, 4, 128]); I[f"w_ig{L}"] = din(f"w_ig{L}", [128, 4, 128])
        I[f"w_upl{L}"] = din(f"w_upl{L}", [512, 1024]); I[f"w_o{L}"] = din(f"w_o{L}", [1024, 1024])
        I[f"g2{L}"] = din(f"g2{L}", [128, 8])
    I["wg"] = din("wg", [1024, D_FF]); I["wu"] = din("wu", [1024, D_FF]); I["wd"] = din("wd", [D_FF, 1024])
    I["mwg"] = din("mwg", [NE, 1024, D_FFE]); I["mwu"] = din("mwu", [NE, 1024, D_FFE]); I["mwd"] = din("mwd", [NE, D_FFE, 1024])
    I["rtw"] = din("rtw", [128, 8, NE])
    y_out = nc.dram_tensor("y", [S, D], F32, kind="ExternalOutput").ap()
    dbg_out = {}
    if debug:
        for nm, (shape, dt) in debug.items():
            dbg_out[nm] = nc.dram_tensor("dbg_" + nm, list(shape), dt, kind="ExternalOutput").ap()

    with ExitStack() as es:
        k = K(nc, es)
        pe, dve, act, pool, sp = k.pe, k.dve, k.act, k.pool, k.sp

        def chunked(name, shape, dt, view_fn):
            t = nc.dram_tensor(name, list(shape), dt, kind="Internal").ap()
            return t, [Buf(view_fn(t, c), f"{name}{c}") for c in range(NCH)]
        tokview = lambda t, c: t.rearrange("(j p) d -> p j d", p=128)[:, 4 * c:4 * c + 4, :]
        Xin_c = [Buf(tokview(I["x"], c), f"x{c}") for c in range(NCH)]
        Y_c = [Buf(tokview(y_out, c), f"y{c}") for c in range(NCH)]
        fmview = lambda t, c: t[:, :, c * 512:(c + 1) * 512]
        H1T_t, H1T = chunked("h1t", [128, 8, S], BF16, fmview)
        H2T_t, H2T = chunked("h2t", [128, 8, S], BF16, fmview)
        QT_t, QT = chunked("qt", [96, 8, S], BF16, fmview)
        KT_t, KT = chunked("kt", [96, 8, S], BF16, fmview)
        V_t, Vd = chunked("vv", [128, NT, 768], BF16, lambda t, c: t[:, 4 * c:4 * c + 4, :])
        SGA_t, SGA = chunked("sga", [128, 8, S], BF16, fmview)
        MB_t, MB = chunked("mb", [128, 8, S], BF16, fmview)

        identb = k.sb([128, 128], BF16, "identb")
        identf = k.sb([128, 128], F32, "identf")
        trib = k.sb([128, 128], BF16, "trib")
        cost = k.sb([128, NT, 16], F32, "cost")
        sint = k.sb([128, NT, 16], F32, "sint")
        epsb = k.sb([128, 1], F32, "epsb")
        wr = k.sb([128, NT, NE], F32, "wr")

        phase_no = [0]

        def end_phase():
            k.wait_all_dma(sp)
            phase_no[0] += 1
            k.simulate()
            with nc.named_scope(f"ph{phase_no[0]:02d}"):
                k.emit()

        with ExitStack() as st:
            idf = identf; trf = k.sb([128, 128], F32, "trf", st)
            posi = k.sb([128, NT], I32, "posi", st); posf = k.sb([128, NT], F32, "posf", st)
            invf = k.sb([128, 16], F32, "invf", st)
            ang = k.sb([128, NT, 16], F32, "ang", st)
            u = k.sb([128, NT, 16], F32, "u", st); nn = k.sb([128, NT, 16], F32, "nn", st)
            k.dma(sp, idf[:], I["ident"], writes=[idf]); k.dma(sp, trf[:], I["tri"], writes=[trf])
            k.dma(sp, posi[:], I["pos"], writes=[posi]); k.dma(sp, invf[:], I["invf"], writes=[invf])
            k.op(pool, lambda h: h.memset(epsb[:], EPS), writes=[epsb])
            k.op(pool, lambda h: h.tensor_copy(out=identb[:], in_=idf[:]), reads=[idf], writes=[identb])
            k.op(pool, lambda h: h.tensor_copy(out=trib[:], in_=trf[:]), reads=[trf], writes=[trib])
            k.op(dve, lambda h: h.tensor_copy(out=posf[:], in_=posi[:]), reads=[posi], writes=[posf])
            k.op(dve, lambda h: h.tensor_tensor(out=ang[:], in0=bc(posf[:], [128, NT, 16], 2), in1=bc(invf[:], [128, NT, 16], 1), op=ALU.mult),
                 reads=[posf, invf], writes=[ang])
            for tab, shift in ((sint, 0.0), (cost, TWO_PI / 4)):
                k.op(dve, lambda h, shift=shift: h.tensor_scalar(out=u[:], in0=ang[:], scalar1=shift, scalar2=None, op0=ALU.add), reads=[ang], writes=[u])
                k.op(dve, lambda h: h.tensor_scalar(out=nn[:], in0=u[:], scalar1=1.0 / TWO_PI, scalar2=MAGIC, op0=ALU.mult, op1=ALU.add), reads=[u], writes=[nn])
                k.op(dve, lambda h: h.tensor_scalar(out=nn[:], in0=nn[:], scalar1=MAGIC, scalar2=None, op0=ALU.subtract), reads=[nn], writes=[nn])
                k.op(dve, lambda h: h.scalar_tensor_tensor(out=u[:], in0=nn[:], scalar=-C1, in1=u[:], op0=ALU.mult, op1=ALU.add), reads=[nn, u], writes=[u])
                k.op(dve, lambda h: h.scalar_tensor_tensor(out=u[:], in0=nn[:], scalar=-C2, in1=u[:], op0=ALU.mult, op1=ALU.add), reads=[nn, u], writes=[u])
                k.op(dve, lambda h: h.tensor_scalar(out=u[:], in0=u[:], scalar1=-PI_SAFE, scalar2=PI_SAFE, op0=ALU.max, op1=ALU.min), reads=[u], writes=[u])
                k.op(act, lambda h, tab=tab: h.activation(out=tab[:], in_=u[:], func=AF.Sin), reads=[u], writes=[tab])
            end_phase()

        cast_i = [0]

        def cast(out_ap, in_ap, scale_ap, reads, writes, eng=None):
            if eng is None:
                eng = act if cast_i[0] % 2 == 0 else dve
                cast_i[0] += 1
            if eng is act:
                if scale_ap is None:
                    k.op(act, lambda h: h.activation(out=out_ap, in_=in_ap, func=AF.Copy), reads=reads, writes=writes)
                else:
                    k.op(act, lambda h: h.activation(out=out_ap, in_=in_ap, func=AF.Copy, scale=scale_ap), reads=reads, writes=writes)
            else:
                if scale_ap is None:
                    k.op(eng, lambda h: h.tensor_copy(out=out_ap, in_=in_ap), reads=reads, writes=writes)
                else:
                    k.op(eng, lambda h: h.tensor_scalar(out=out_ap, in0=in_ap, scalar1=scale_ap, scalar2=None, op0=ALU.mult), reads=reads, writes=writes)

        def load_w(st_bufs, dst, dst_fn, src, nk, n, scale=None, c0=0, q=None, eng=None):
            q = q or sp
            for kc in range(nk):
                sg = st_bufs.next()
                k.dma(q, sg[:, 0:n], src[kc * 128:(kc + 1) * 128, c0:c0 + n], writes=[sg])
                if scale is None:
                    cast(dst_fn(kc), sg[:, 0:n], None, [sg], [dst], eng)
                else:
                    cast(dst_fn(kc), sg[:, 0:n], scale[:, kc:kc + 1], [sg, scale], [dst], eng)

        def rms_T(xin, ssq, rstd, hb, hT, pTs, junk):
            for j in range(4):
                k.op(act, lambda h, j=j: h.activation(out=junk[:], in_=xin[:, j, :], func=AF.Square, scale=1.0 / 32.0, accum_out=ssq[:, j:j + 1]),
                     reads=[xin.sub(j)], writes=[ssq])
            k.op(act, lambda h: h.activation(out=ssq[:], in_=ssq[:], func=AF.Sqrt, bias=epsb[:, 0:1], scale=1.0), reads=[ssq, epsb], writes=[ssq])
            k.op(dve, lambda h: h.reciprocal(out=rstd[:], in_=ssq[:]), reads=[ssq], writes=[rstd])
            for j in range(4):
                if j % 2 == 0:
                    k.op(act, lambda h, j=j: h.activation(out=hb[:, j, :], in_=xin[:, j, :], func=AF.Copy, scale=rstd[:, j:j + 1]), reads=[xin.sub(j), rstd], writes=[hb.sub(j)])
                else:
                    k.op(dve, lambda h, j=j: h.tensor_scalar(out=hb[:, j, :], in0=xin[:, j, :], scalar1=rstd[:, j:j + 1], scalar2=None, op0=ALU.mult),
                         reads=[xin.sub(j), rstd], writes=[hb.sub(j)])
            for kc in range(8):
                pT = pTs.next()
                for j in range(4):
                    k.op(pe, lambda h, j=j, kc=kc, pT=pT: h.transpose(out=pT[:, j * 128:(j + 1) * 128], in_=hb[:, j, kc * 128:(kc + 1) * 128], identity=identb[:]),
                         reads=[hb.sub(j), identb], writes=[pT], inc=(j == 3))
                e = act if kc % 2 == 0 else dve
                if e is act:
                    k.op(act, lambda h, kc=kc, pT=pT: h.activation(out=hT[:, kc, :], in_=pT[:, 0:512], func=AF.Copy), reads=[pT], writes=[hT.sub(kc)])
                else:
                    k.op(dve, lambda h, kc=kc, pT=pT: h.tensor_copy(out=hT[:, kc, :], in_=pT[:, 0:512]), reads=[pT], writes=[hT.sub(kc)])

        def rope(x1, x2, cs, sn, d1, d2, tmps, rd, wr_, tb, eng=None):
            eng = eng or dve
            t1, t2 = tmps
            b1, b2 = tb
            k.op(eng, lambda h: h.tensor_tensor(out=t1, in0=x1, in1=cs, op=ALU.mult), reads=rd, writes=[b1])
            yield
            k.op(eng, lambda h: h.tensor_tensor(out=t2, in0=x2, in1=sn, op=ALU.mult), reads=rd, writes=[b2])
            yield
            k.op(eng, lambda h: h.tensor_tensor(out=d1, in0=t1, in1=t2, op=ALU.subtract), reads=[b1, b2], writes=wr_)
            yield
            k.op(eng, lambda h: h.tensor_tensor(out=t1, in0=x2, in1=cs, op=ALU.mult), reads=rd, writes=[b1])
            yield
            k.op(eng, lambda h: h.tensor_tensor(out=t2, in0=x1, in1=sn, op=ALU.mult), reads=rd, writes=[b2])
            yield
            k.op(eng, lambda h: h.tensor_tensor(out=d2, in0=t1, in1=t2, op=ALU.add), reads=[b1, b2], writes=wr_)
            yield

        dbg_dump = []
        for L in range(2):
            Xc = Xin_c if L == 0 else Y_c
            with ExitStack() as st:
                stg = PsPool([k.sb([128, 1024], F32, f"stg{i}", st) for i in range(2)])
                Wsm = k.sb([128, 8, 416], BF16, "Wsm", st); Wuq = k.sb([128, 2, 768], BF16, "Wuq", st); Wukv = k.sb([128, 1024], BF16, "Wukv", st)
                g1 = k.sb([128, 8], F32, "g1", st); qng = k.sb([128, 2], F32, "qng", st); kvng = k.sb([128, 1], F32, "kvng", st)
                qkg = k.sb([128, 192], F32, "qkg", st)
                for b_, nm in ((g1, "g1"), (qng, "qng"), (kvng, "kvng"), (qkg, "qkg")):
                    k.dma(sp, b_[:], I[f"{nm}{L}"], writes=[b_])
                load_w(stg, Wsm, lambda kc: Wsm[:, kc, :], I[f"w_in{L}"], 8, 416, g1)
                load_w(stg, Wuq, lambda kc: Wuq[:, kc, :], I[f"w_uq{L}"], 2, 768, qng)
                load_w(stg, Wukv, lambda kc: Wukv[:, :], I[f"w_ukv{L}"], 1, 1024, kvng)
                xins = [k.sb([128, 4, 1024], F32, f"xin{i}", st) for i in range(2)]
                for xb_ in xins:
                    for j in range(4):
                        xb_.sub(j)
                hb = k.sb([128, 4, 1024], BF16, "hb", st)
                hTs = [k.sb([128, 8, 512], BF16, f"hT{i}", st) for i in range(2)]
                junk = k.sb([128, 1024], BF16, "junk", st)
                chunkB = []
                for i in range(2):
                    chunkB.append((k.sb([128, 4, 416], F32, f"csm{i}", st), k.sb([128, 4, 4], F32, f"st4{i}", st), k.sb([128, 4, 2], F32, f"rs4{i}", st),
                                   k.sb([128, 4, 384], BF16, f"cqn{i}", st), k.sb([128, 3, 512], BF16, f"cT{i}", st),
                                   k.sb([128, 4], F32, f"ssq{i}", st), k.sb([128, 4], F32, f"rstd{i}", st)))
                tileB = []
                for i in range(2):
                    tileB.append((k.sb([128, 8, 96], F32, f"q_s{i}", st), k.sb([128, 8, 128], F32, f"kv_s{i}", st),
                                  k.sb([128, 8, 96], F32, f"sqt{i}", st), k.sb([128, 8, 64], F32, f"sqk{i}", st),
                                  k.sb([128, 16], F32, f"ss{i}", st), k.sb([128, 16], F32, f"rs{i}", st),
                                  k.sb([128, 8, 96], F32, f"qf{i}", st), k.sb([128, 8, 64], F32, f"kf{i}", st),
                                  k.sb([128, 8, 96], BF16, f"qb{i}", st), k.sb([128, 8, 96], BF16, f"kb{i}", st),
                                  k.sb([128, 32], F32, f"krg{i}", st), k.sb([128, 32], F32, f"krr{i}", st),
                                  k.sb([128, 2, 8, 16], F32, f"tmpB{i}", st), k.sb([128, 2, 16], F32, f"tmpK{i}", st),
                                  (Buf(None, "tq1"), Buf(None, "tq2")), (Buf(None, "tk1"), Buf(None, "tk2"))))
                QTs = k.sb([128, 8, 512], BF16, "QTs", st); KTs = k.sb([128, 8, 512], BF16, "KTs", st)
                Vs = k.sb([128, 4, 768], BF16, "Vs", st)
                pTs = PsPool([k.ps([128, 1024], BF16, f"pT{i}", st) for i in range(2)])
                psms = PsPool([k.ps([128, 512], F32, f"psm{i}", st) for i in range(2)])
                pq = k.ps([128, 2, 512], F32, "pq", st); pkv = k.ps([128, 2, 512], F32, "pkv", st)
                for j in range(4):
                    Vs.sub(j); QTs.sub(j); KTs.sub(j)
                k.op(pool, lambda h: h.memset(Vs[:], 1.0), writes=Vs.all())
                gq = qkg[:, 0:96]; gk = qkg[:, 96:192]
                def tile_gen(c, j, B, CB):
                    t = 4 * c + j
                    q_s, kv_s, sqt, sqk, ss, rs, qf, kf, qb, kb, krg, krr, tmpB, tmpK, tbq, tbk = B
                    csm, st4, rs4, cqn, cT, ssq, rstd = CB
                    if True:
                        for hh in range(2):
                            for kc in range(2):
                                k.op(pe, lambda h, j=j, hh=hh, kc=kc: h.matmul(pq[:, hh, 0:384], lhsT=cT[:, kc, j * 128:(j + 1) * 128], rhs=Wuq[:, kc, hh * 384:(hh + 1) * 384], start=(kc == 0), stop=(kc == 1)),
                                     reads=[cT.sub(kc), Wuq], writes=[pq], inc=(kc == 1))
                        for hh in range(2):
                            k.op(pe, lambda h, j=j, hh=hh: h.matmul(pkv[:, hh, :], lhsT=cT[:, 2, j * 128:(j + 1) * 128], rhs=Wukv[:, hh * 512:(hh + 1) * 512], start=True, stop=True),
                                 reads=[cT.sub(2), Wukv], writes=[pkv])
                        for hh in range(2):
                            k.op(act, lambda h, hh=hh: h.activation(out=q_s[:, 4 * hh:4 * hh + 4, :], in_=pq[:, hh, 0:384].rearrange("p (a d) -> p a d", a=4), func=AF.Copy), reads=[pq], writes=[q_s])
                            k.op(act, lambda h, hh=hh: h.activation(out=kv_s[:, 4 * hh:4 * hh + 4, :], in_=pkv[:, hh, :].rearrange("p (a d) -> p a d", a=4), func=AF.Copy), reads=[pkv], writes=[kv_s])
                        k.op(act, lambda h: h.activation(out=sqt[:], in_=q_s[:], func=AF.Square), reads=[q_s], writes=[sqt])
                        yield
                        k.op(act, lambda h: h.activation(out=sqk[:], in_=kv_s[:, :, 0:64], func=AF.Square), reads=[kv_s], writes=[sqk])
                        yield
                    if True:
                        k.op(dve, lambda h: h.tensor_reduce(out=ss[:, 0:8], in_=sqt[:], axis=AX.X, op=ALU.add), reads=[sqt], writes=[ss])
                        yield
                        k.op(dve, lambda h: h.tensor_reduce(out=ss[:, 8:16], in_=sqk[:], axis=AX.X, op=ALU.add), reads=[sqk], writes=[ss])
                        yield
                        k.op(dve, lambda h, j=j: h.tensor_scalar(out=ss[:, 8:16], in0=ss[:, 8:16], scalar1=st4[:, j, 2:3], scalar2=None, op0=ALU.add), reads=[ss, st4.sub(j)], writes=[ss])
                        yield
                        k.op(act, lambda h: h.activation(out=ss[:], in_=ss[:], func=AF.Sqrt, bias=epsb[:, 0:1], scale=1.0 / 96.0), reads=[ss, epsb], writes=[ss])
                        yield
                        k.op(dve, lambda h: h.reciprocal(out=rs[:], in_=ss[:]), reads=[ss], writes=[rs])
                        yield
                        k.op(dve, lambda h: h.tensor_scalar(out=rs[:, 0:8], in0=rs[:, 0:8], scalar1=96.0 ** -0.5, scalar2=None, op0=ALU.mult), reads=[rs], writes=[rs])
                        yield
                    if True:
                        k.op(dve, lambda h: h.tensor_tensor(out=qf[:], in0=q_s[:], in1=bc(rs[:, 0:8], [128, 8, 96], 2), op=ALU.mult), reads=[q_s, rs], writes=[qf])
                        yield
                        k.op(dve, lambda h: h.tensor_tensor(out=qf[:], in0=qf[:], in1=bc(gq, [128, 8, 96], 1), op=ALU.mult), reads=[qf, qkg], writes=[qf])
                        yield
                        k.op(act, lambda h: h.activation(out=qb[:, :, 0:64], in_=qf[:, :, 0:64], func=AF.Copy), reads=[qf], writes=[qb])
                        yield
                        cs8 = bc(cost[:, t, :], [128, 8, 16], 1); sn8 = bc(sint[:, t, :], [128, 8, 16], 1)
                        yield from rope(qf[:, :, 64:80], qf[:, :, 80:96], cs8, sn8, qb[:, :, 64:80], qb[:, :, 80:96], (tmpB[:, 0, :, :], tmpB[:, 1, :, :]), [qf, cost, sint], [qb], tbq)
                        k.op(dve, lambda h: h.tensor_tensor(out=kf[:], in0=kv_s[:, :, 0:64], in1=bc(rs[:, 8:16], [128, 8, 64], 2), op=ALU.mult), reads=[kv_s, rs], writes=[kf])
                        yield
                        k.op(dve, lambda h: h.tensor_tensor(out=kb[:, :, 0:64], in0=kf[:], in1=bc(qkg[:, 96:160], [128, 8, 64], 1), op=ALU.mult), reads=[kf, qkg], writes=[kb])
                        yield
                        k.op(pool, lambda h, j=j: h.tensor_tensor(out=krg[:], in0=csm[:, j, 384:416], in1=qkg[:, 160:192], op=ALU.mult), reads=[csm.sub(j), qkg], writes=[krg])
                        yield
                        yield from rope(krg[:, 0:16], krg[:, 16:32], cost[:, t, :], sint[:, t, :], krr[:, 0:16], krr[:, 16:32], (tmpK[:, 0, :], tmpK[:, 1, :]), [krg, cost, sint], [krr], tbk, pool)
                        k.op(pool, lambda h: h.tensor_tensor(out=kb[:, :, 64:96], in0=bc(krr[:], [128, 8, 32], 1), in1=bc(rs[:, 8:16], [128, 8, 32], 2), op=ALU.mult), reads=[krr, rs], writes=[kb])
                        yield
                        kv4 = kv_s[:].rearrange("p (a b) d -> p a b d", b=2)
                        Vv = Vs[:, j, :].rearrange("p (a c) -> p a c", c=192)
                        k.op(act, lambda h, kv4=kv4, Vv=Vv: h.activation(out=Vv[:, :, 0:64], in_=kv4[:, :, 0, 64:128], func=AF.Copy), reads=[kv_s], writes=[Vs.sub(j)])
                        yield
                        k.op(act, lambda h, kv4=kv4, Vv=Vv: h.activation(out=Vv[:, :, 128:192], in_=kv4[:, :, 1, 64:128], func=AF.Copy), reads=[kv_s], writes=[Vs.sub(j)])
                        yield
                    if True:
                        for src, dstT in ((qb, QTs), (kb, KTs)):
                            pT = pTs.next()
                            for hd in range(8):
                                k.op(pe, lambda h, hd=hd, pT=pT, src=src: h.transpose(out=pT[0:96, hd * 128:(hd + 1) * 128], in_=src[:, hd, :], identity=identb[:]),
                                     reads=[src, identb], writes=[pT], inc=(hd == 7))
                            k.op(dve, lambda h, j=j, pT=pT, dstT=dstT: h.tensor_copy(out=dstT[0:96, :, j * 128:(j + 1) * 128], in_=pT[0:96, :].rearrange("p (a d) -> p a d", a=8)), reads=[pT], writes=[dstT.sub(j)])

                k.dma(sp, xins[0][:], Xc[0].t, reads=[Xc[0]], writes=xins[0].all())

                def p1a_chunk(c, CB):
                    csm, st4, rs4, cqn, cT, ssq, rstd = CB
                    xin = xins[c % 2]; hT = hTs[c % 2]
                    if c + 1 < NCH:
                        k.dma(sp, xins[(c + 1) % 2][:], Xc[c + 1].t, reads=[Xc[c + 1]], writes=xins[(c + 1) % 2].all())
                    rms_T(xin, ssq, rstd, hb, hT, pTs, junk)
                    k.dma(sp, H1T[c].t, hT[:], reads=hT.all(), writes=[H1T[c]])
                    for j in range(4):
                        psm = psms.next()
                        for kc in range(8):
                            k.op(pe, lambda h, j=j, kc=kc, psm=psm, hT=hT: h.matmul(psm[:, 0:416], lhsT=hT[:, kc, j * 128:(j + 1) * 128], rhs=Wsm[:, kc, :], start=(kc == 0), stop=(kc == 7)),
                                 reads=[hT.sub(kc), Wsm], writes=[psm], inc=(kc == 7))
                        k.op(act, lambda h, j=j, psm=psm: h.activation(out=csm[:, j, :], in_=psm[:, 0:416], func=AF.Copy), reads=[psm], writes=[csm.sub(j)])
                        k.op(act, lambda h, j=j: h.activation(out=junk[:, 0:256], in_=csm[:, j, 0:256], func=AF.Square, scale=1.0 / 16.0, accum_out=st4[:, j, 0:1]), reads=[csm.sub(j)], writes=[st4.sub(j)])
                        k.op(act, lambda h, j=j: h.activation(out=junk[:, 0:128], in_=csm[:, j, 256:384], func=AF.Square, scale=128.0 ** -0.5, accum_out=st4[:, j, 1:2]), reads=[csm.sub(j)], writes=[st4.sub(j)])
                        k.op(act, lambda h, j=j: h.activation(out=junk[:, 0:32], in_=csm[:, j, 384:416], func=AF.Square, accum_out=st4[:, j, 2:3]), reads=[csm.sub(j)], writes=[st4.sub(j)])
                    k.op(act, lambda h: h.activation(out=rs4[:], in_=st4[:, :, 0:2], func=AF.Sqrt, bias=epsb[:, 0:1], scale=1.0), reads=st4.all() + [epsb], writes=[rs4])
                    k.op(dve, lambda h: h.reciprocal(out=rs4[:], in_=rs4[:]), reads=[rs4], writes=[rs4])
                    for j in range(4):
                        k.op(act, lambda h, j=j: h.activation(out=cqn[:, j, 0:256], in_=csm[:, j, 0:256], func=AF.Copy, scale=rs4[:, j, 0:1]), reads=[csm.sub(j), rs4], writes=[cqn.sub(j)])
                        k.op(act, lambda h, j=j: h.activation(out=cqn[:, j, 256:384], in_=csm[:, j, 256:384], func=AF.Copy, scale=rs4[:, j, 1:2]), reads=[csm.sub(j), rs4], writes=[cqn.sub(j)])
                    for kk in range(3):
                        pT = pTs.next()
                        for j in range(4):
                            k.op(pe, lambda h, j=j, kk=kk, pT=pT: h.transpose(out=pT[:, j * 128:(j + 1) * 128], in_=cqn[:, j, kk * 128:(kk + 1) * 128], identity=identb[:]),
                                 reads=[cqn.sub(j), identb], writes=[pT], inc=(j == 3))
                        k.op(act, lambda h, kk=kk, pT=pT: h.activation(out=cT[:, kk, :], in_=pT[:, 0:512], func=AF.Copy), reads=[pT], writes=[cT.sub(kk)])
                    gens = [tile_gen(c, j, tileB[j % 2], CB) for j in range(4)]
                    active = [gens[0], gens[1]]; nxt = 2
                    while active:
                        for g in list(active):
                            try:
                                next(g)
                            except StopIteration:
                                active.remove(g)
                                if nxt < 4:
                                    active.append(gens[nxt]); nxt += 1
                    k.dma(sp, QT[c].t, QTs[0:96, :, :], reads=QTs.all(), writes=[QT[c]])
                    k.dma(sp, KT[c].t, KTs[0:96, :, :], reads=KTs.all(), writes=[KT[c]])
                    k.dma(sp, Vd[c].t, Vs[:], reads=Vs.all(), writes=[Vd[c]])

                for c in range(NCH):
                    p1a_chunk(c, chunkB[c % 2])
                end_phase()
            if stop_after == f"P1a{L}":
                break

            with ExitStack() as st:
                stg = PsPool([k.sb([128, 1024], F32, f"stg{i}", st) for i in range(2)])
                Wbig = k.sb([128, 8, 3072], BF16, "Wbig", st)
                Wrg = k.sb([128, 4, 128], BF16, "Wrg", st); Wig = k.sb([128, 4, 128], BF16, "Wig", st)
                Wupl = k.sb([128, 4, 1024], BF16, "Wupl", st)
                g1 = k.sb([128, 8], F32, "g1", st); cw = k.sb([128, 4, 4], F32, "cw", st); lv = k.sb([128, 4, 4], F32, "lv", st)
                cA = k.sb([128, 4], F32, "cA", st); cA2 = k.sb([128, 4], F32, "cA2", st)
                for b_, nm in ((g1, "g1"), (cw, "convw"), (lv, "lruv")):
                    k.dma(sp, b_[:], I[f"{nm}{L}"], writes=[b_])
                for pc in range(3):
                    load_w(stg, Wbig, lambda kc, pc=pc: Wbig[:, kc, pc * 1024:(pc + 1) * 1024], I[f"w_in{L}"], 8, 1024, g1, c0=416 + pc * 1024)
                sgx = stg.next()
                k.dma(sp, sgx[:, 0:512], I[f"w_rg{L}"].rearrange("p a b -> p (a b)"), writes=[sgx])
                cast(Wrg[:].rearrange("p a b -> p (a b)"), sgx[:, 0:512], None, [sgx], [Wrg])
                sgx2 = stg.next()
                k.dma(sp, sgx2[:, 0:512], I[f"w_ig{L}"].rearrange("p a b -> p (a b)"), writes=[sgx2])
                cast(Wig[:].rearrange("p a b -> p (a b)"), sgx2[:, 0:512], None, [sgx2], [Wig])
                load_w(stg, Wupl, lambda kc: Wupl[:, kc, :], I[f"w_upl{L}"], 4, 1024)
                k.op(act, lambda h: h.activation(out=cA[:], in_=lv[:, :, 3], func=AF.Exp, scale=-1.0), reads=[lv], writes=[cA])
                k.op(dve, lambda h: h.tensor_scalar(out=cA[:], in0=cA[:], scalar1=1.0, scalar2=None, op0=ALU.add), reads=[cA], writes=[cA])
                k.op(act, lambda h: h.activation(out=cA[:], in_=cA[:], func=AF.Ln), reads=[cA], writes=[cA])
                k.op(dve, lambda h: h.tensor_scalar(out=cA2[:], in0=cA[:], scalar1=-16.0, scalar2=None, op0=ALU.mult), reads=[cA], writes=[cA2])
                k.op(dve, lambda h: h.tensor_scalar(out=cA[:], in0=cA[:], scalar1=-8.0, scalar2=None, op0=ALU.mult), reads=[cA], writes=[cA])
                hTs = [k.sb([128, 8, 512], BF16, f"hT{i}", st) for i in range(1)]
                xl = k.sb([128, 4, 515], F32, "xl", st)
                hprev = k.sb([128, 4], F32, "hprev", st)
                tA = [k.sb([128, 512], F32, f"tA{i}", st) for i in range(4)]
                xcb = [k.sb([128, 512], BF16, f"xcb{i}", st) for i in range(4)]
                tR = [k.sb([128, 512], F32, f"tR{i}", st) for i in range(4)]
                tI = [k.sb([128, 512], F32, f"tI{i}", st) for i in range(4)]
                tM = [k.sb([128, 512], F32, f"tM{i}", st) for i in range(4)]
                tH = [k.sb([128, 512], F32, f"tH{i}", st) for i in range(4)]
                tG = [k.sb([128, 512], F32, f"tG{i}", st) for i in range(4)]
                yl = k.sb([128, 4, 512], BF16, "yl", st)
                sga_s = k.sb([128, 8, 512], BF16, "sgas", st)
                sgb_s = k.sb([128, 8, 512], BF16, "sgbs", st)
                mb_s = k.sb([128, 8, 512], BF16, "mbs", st)
                pp = PsPool([k.ps([128, 512], F32, f"pp{i}", st) for i in range(8)])
                for fc in range(4):
                    xl.sub(fc)
                k.op(pool, lambda h: h.memset(xl[:], 0.0), writes=xl.all())
                k.op(pool, lambda h: h.memset(hprev[:], 0.0), writes=[hprev])

                def proj(hT, col):
                    p_ = pp.next()
                    for kc in range(8):
                        k.op(pe, lambda h, kc=kc, p_=p_: h.matmul(p_[:], lhsT=Wbig[:, kc, col:col + 128], rhs=hT[:, kc, :], start=(kc == 0), stop=(kc == 7)),
                             reads=[hT, Wbig], writes=[p_], inc=(kc == 7))
                    return p_

                k.dma(sp, hTs[0][:], H1T[0].t, reads=[H1T[0]], writes=[hTs[0]])
                for c in range(NCH):
                    hT = hTs[0]
                    for fc in range(4):
                        px = proj(hT, fc * 128)
                        k.op(act, lambda h, fc=fc, px=px: h.activation(out=xl[:, fc, 3:515], in_=px[:], func=AF.Copy), reads=[px], writes=[xl.sub(fc)])
                    for fc in range(4):
                        xc = tA[fc]
                        k.op(dve, lambda h, fc=fc, xc=xc: h.tensor_scalar(out=xc[:], in0=xl[:, fc, 0:512], scalar1=cw[:, fc, 0:1], scalar2=lv[:, fc, 0:1], op0=ALU.mult, op1=ALU.add),
                             reads=[xl.sub(fc), cw, lv], writes=[xc])
                        for tp in range(1, 4):
                            k.op(dve, lambda h, fc=fc, tp=tp, xc=xc: h.scalar_tensor_tensor(out=xc[:], in0=xl[:, fc, tp:tp + 512], scalar=cw[:, fc, tp:tp + 1], in1=xc[:], op0=ALU.mult, op1=ALU.add),
                                 reads=[xl.sub(fc), cw, xc], writes=[xc])
                        k.op(act, lambda h, fc=fc, xc=xc: h.activation(out=xcb[fc][:], in_=xc[:], func=AF.Copy), reads=[xc], writes=[xcb[fc]])
                    for fc in range(4):
                        k.op(pool, lambda h, fc=fc: h.tensor_copy(out=xl[:, fc, 0:3], in_=xl[:, fc, 512:515]), reads=[xl.sub(fc)], writes=[xl.sub(fc)])
                    for fc in range(4):
                        pg = proj(hT, 512 + fc * 128)
                        k.op(act, lambda h, fc=fc, pg=pg: h.activation(out=tG[fc][:], in_=pg[:], func=AF.Copy), reads=[pg], writes=[tG[fc]])
                    for dc in range(8):
                        pga = proj(hT, 1024 + dc * 128)
                        k.op(act, lambda h, dc=dc, pga=pga: h.activation(out=sga_s[:, dc, :], in_=pga[:], func=AF.Sigmoid), reads=[pga], writes=[sga_s.sub(dc)])
                    for fc in range(4):
                        pr_ = pp.next(); pi_ = pp.next()
                        k.op(pe, lambda h, fc=fc, pr_=pr_: h.matmul(pr_[:], lhsT=Wrg[:, fc, :], rhs=xcb[fc][:], start=True, stop=True), reads=[Wrg, xcb[fc]], writes=[pr_])
                        k.op(pe, lambda h, fc=fc, pi_=pi_: h.matmul(pi_[:], lhsT=Wig[:, fc, :], rhs=xcb[fc][:], start=True, stop=True), reads=[Wig, xcb[fc]], writes=[pi_])
                        k.op(act, lambda h, fc=fc, pr_=pr_: h.activation(out=tR[fc][:], in_=pr_[:], func=AF.Sigmoid, bias=lv[:, fc, 1:2], scale=1.0), reads=[pr_, lv], writes=[tR[fc]])
                        k.op(act, lambda h, fc=fc, pi_=pi_: h.activation(out=tI[fc][:], in_=pi_[:], func=AF.Sigmoid, bias=lv[:, fc, 2:3], scale=1.0), reads=[pi_, lv], writes=[tI[fc]])
                    pgbs = []
                    for dc in range(8):
                        pgb = proj(hT, 2048 + dc * 128)
                        pgbs.append((dc, pgb))
                    if c + 1 < NCH:
                        k.dma(sp, hT[:], H1T[c + 1].t, reads=[H1T[c + 1]], writes=[hT])
                    for fc in range(4):
                        k.op(act, lambda h, fc=fc: h.activation(out=tM[fc][:], in_=tR[fc][:], func=AF.Exp, scale=cA2[:, fc:fc + 1]), reads=[tR[fc], cA2], writes=[tM[fc]])
                        k.op(act, lambda h, fc=fc: h.activation(out=tR[fc][:], in_=tR[fc][:], func=AF.Exp, scale=cA[:, fc:fc + 1]), reads=[tR[fc], cA], writes=[tR[fc]])
                        k.op(dve, lambda h, fc=fc: h.tensor_scalar(out=tM[fc][:], in0=tM[fc][:], scalar1=-1.0, scalar2=1.0, op0=ALU.mult, op1=ALU.add), reads=[tM[fc]], writes=[tM[fc]])
                        k.op(dve, lambda h, fc=fc: h.tensor_tensor(out=tI[fc][:], in0=tI[fc][:], in1=tA[fc][:], op=ALU.mult), reads=[tI[fc], tA[fc]], writes=[tI[fc]])
                    for fc in range(4):
                        k.op(act, lambda h, fc=fc: h.activation(out=tM[fc][:], in_=tM[fc][:], func=AF.Sqrt), reads=[tM[fc]], writes=[tM[fc]])
                        k.op(dve, lambda h, fc=fc: h.tensor_tensor(out=tI[fc][:], in0=tI[fc][:], in1=tM[fc][:], op=ALU.mult), reads=[tI[fc], tM[fc]], writes=[tI[fc]])
                        k.op(dve, lambda h, fc=fc: h.tensor_tensor_scan(out=tH[fc][:], data0=tR[fc][:], data1=tI[fc][:], initial=hprev[:, fc:fc + 1], op0=ALU.mult, op1=ALU.add),
                             reads=[tR[fc], tI[fc], hprev], writes=[tH[fc]])
                        k.op(dve, lambda h, fc=fc: h.tensor_copy(out=hprev[:, fc:fc + 1], in_=tH[fc][:, 511:512]), reads=[tH[fc]], writes=[hprev])
                    for fc in range(4):
                        k.op(act, lambda h, fc=fc: h.activation(out=tG[fc][:], in_=tG[fc][:], func=AF.Gelu_apprx_tanh), reads=[tG[fc]], writes=[tG[fc]])
                        k.op(dve, lambda h, fc=fc: h.tensor_tensor(out=yl[:, fc, :], in0=tH[fc][:], in1=tG[fc][:], op=ALU.mult), reads=[tH[fc], tG[fc]], writes=[yl.sub(fc)])
                    for (dc, pgb) in pgbs:
                        k.op(act, lambda h, dc=dc, pgb=pgb: h.activation(out=sgb_s[:, dc, :], in_=pgb[:], func=AF.Sigmoid), reads=[pgb], writes=[sgb_s.sub(dc)])
                    for dc in range(8):
                        pu = pp.next()
                        for fc in range(4):
                            k.op(pe, lambda h, fc=fc, dc=dc, pu=pu: h.matmul(pu[:], lhsT=Wupl[:, fc, dc * 128:(dc + 1) * 128], rhs=yl[:, fc, :], start=(fc == 0), stop=(fc == 3)),
                                 reads=[Wupl, yl.sub(fc)], writes=[pu], inc=(fc == 3))
                        k.op(dve, lambda h, dc=dc, pu=pu: h.tensor_tensor(out=mb_s[:, dc, :], in0=pu[:], in1=sgb_s[:, dc, :], op=ALU.mult), reads=[pu, sgb_s.sub(dc)], writes=[mb_s.sub(dc)])
                    k.dma(sp, SGA[c].t, sga_s[:], reads=sga_s.all(), writes=[SGA[c]])
                    k.dma(sp, MB[c].t, mb_s[:], reads=mb_s.all(), writes=[MB[c]])
                end_phase()
            if stop_after == f"P1b{L}":
                break

            st23 = ExitStack()
            OT = k.sb([128, 4, S], BF16, f"OT{L}", st23)
            Wupa = k.sb([128, 4, 1024], BF16, "Wupa", st23); Wo = k.sb([128, 8, 1024], BF16, "Wo", st23)
            with ExitStack() as st:
                stgw = PsPool([k.sb([128, 1024], F32, f"stgw{i}", st) for i in range(2)])
                Vp = [k.sb([128, NT, 192], BF16, f"Vp{i}", st) for i in range(2)]
                KTh = [k.sb([128, S], BF16, f"KTh{i}", st) for i in range(2)]
                QTh = [k.sb([128, S], BF16, f"QTh{i}", st) for i in range(2)]
                pts = PsPool([k.sb([128, 512], BF16, f"pt{i}", st) for i in range(6)])
                rcs = PsPool([k.sb([128, 512], F32, f"rc{i}", st) for i in range(2)])
                pss = PsPool([k.ps([128, 512], F32, f"ps{i}", st) for i in range(5)])
                pos_ = PsPool([k.ps([128, 512], F32, f"po{i}", st) for i in range(3)])

                def load_head(hd):
                    i2 = hd % 2
                    if hd % 2 == 0:
                        pr = hd // 2
                        k.dma(sp, Vp[pr % 2][:], V_t[:, :, pr * 192:(pr + 1) * 192], reads=Vd, writes=[Vp[pr % 2]])
                    k.dma(sp, KTh[i2][0:96, :], KT_t[:, hd, :], reads=KT, writes=[KTh[i2]])
                    k.dma(sp, QTh[i2][0:96, :], QT_t[:, hd, :], reads=QT, writes=[QTh[i2]])
                load_head(0)

                def wprefetch():
                    for (W_, src, nk) in ((Wupa, I[f"w_upa{L}"], 4), (Wo, I[f"w_o{L}"], 8)):
                        for kc in range(nk):
                            sg = stgw.next()
                            k.dma(sp, sg[:, 0:1024], src[kc * 128:(kc + 1) * 128, 0:1024], writes=[sg])
                            yield
                            cast(W_[:, kc, :], sg[:, 0:1024], None, [sg], [W_], dve)
                            yield
                wgen = wprefetch(); wcnt = [0]
                LA = 3
                items = []
                for hd in range(8):
                    for c in range(NCH):
                        nk = 4 * c + 4
                        for kt in range(nk):
                            items.append((hd, c, kt, nk))
                state = {}

                def issue_S(it):
                    hd, c, kt, nk = it
                    if c == 0 and kt == 0 and hd + 1 < 8:
                        load_head(hd + 1)
                    if hd >= 2:
                        wcnt[0] += 1
                        if wcnt[0] % 3 == 0:
                            next(wgen, None)
                    Kh = KTh[hd % 2]; Qh = QTh[hd % 2]
                    dd = kt - 4 * c
                    q0 = dd * 128 if dd > 0 else 0
                    ps_ = pss.next(); pt = pts.next()
                    k.op(pe, lambda h: h.matmul(ps_[:, q0:512], lhsT=Kh[0:96, kt * 128:(kt + 1) * 128], rhs=Qh[0:96, c * 512 + q0:(c + 1) * 512], start=True, stop=True),
                         reads=[Kh, Qh], writes=[ps_])
                    k.op(act, lambda h: h.activation(out=pt[:, q0:512], in_=ps_[:, q0:512], func=AF.Exp), reads=[ps_], writes=[pt])
                    if dd >= 0:
                        k.op(dve, lambda h: h.tensor_tensor(out=pt[:, q0:q0 + 128], in0=pt[:, q0:q0 + 128], in1=trib[:], op=ALU.mult), reads=[pt, trib], writes=[pt])
                    state[it] = (pt, q0)

                def issue_PV(it):
                    hd, c, kt, nk = it
                    pt, q0 = state.pop(it)
                    pr = hd // 2; odd = hd % 2
                    Vh = Vp[pr % 2]; voff = 64 if odd else 0
                    if kt == 0:
                        state[("po", hd, c)] = pos_.next()
                    po = state[("po", hd, c)]
                    k.op(pe, lambda h: h.matmul(po[:, q0:512], lhsT=Vh[:, kt, voff:voff + 128], rhs=pt[:, q0:512], start=(kt == 0), stop=(kt == nk - 1)),
                         reads=[Vh, pt], writes=[po], inc=(kt == nk - 1))
                    if kt == nk - 1:
                        del state[("po", hd, c)]
                        rc = rcs.next()
                        if not odd:
                            k.op(dve, lambda h: h.reciprocal(out=rc[64:128, :], in_=po[64:128, :]), reads=[po], writes=[rc])
                            k.op(dve, lambda h: h.tensor_tensor(out=OT[0:64, pr, c * 512:(c + 1) * 512], in0=po[0:64, :], in1=rc[64:128, :], op=ALU.mult), reads=[po, rc], writes=[OT])
                        else:
                            k.op(dve, lambda h: h.reciprocal(out=rc[0:64, :], in_=po[0:64, :]), reads=[po], writes=[rc])
                            k.op(dve, lambda h: h.tensor_tensor(out=OT[64:128, pr, c * 512:(c + 1) * 512], in0=po[64:128, :], in1=rc[0:64, :], op=ALU.mult), reads=[po, rc], writes=[OT])

                for i in range(len(items) + LA):
                    if i < len(items):
                        issue_S(items[i])
                    if i - LA >= 0:
                        issue_PV(items[i - LA])
                for _ in wgen:
                    pass
                if debug and "ot" in debug and L == 0:
                    k.dma(sp, dbg_out["ot"], OT[:], reads=[OT])
                end_phase()
            if stop_after == f"P2{L}":
                st23.close()
                break

            with ExitStack() as st:
                xins = [k.sb([128, 4, 1024], F32, f"xin{i}", st) for i in range(2)]
                for xb_ in xins:
                    for j in range(4):
                        xb_.sub(j)
                sgl = [k.sb([128, 8, 512], BF16, f"sgl{i}", st) for i in range(2)]
                mbl = [k.sb([128, 8, 512], BF16, f"mbl{i}", st) for i in range(2)]
                mg = k.sb([128, 8, 512], BF16, "mg", st)
                tmpf = [k.sb([128, 512], F32, f"tmpf{i}", st) for i in range(2)]
                hb = k.sb([128, 4, 1024], BF16, "hb", st); hT = k.sb([128, 8, 512], BF16, "hT", st)
                junk = k.sb([128, 1024], BF16, "junk", st)
                ssq = k.sb([128, 4], F32, "ssq", st); rstd = k.sb([128, 4], F32, "rstd", st)
                pTs = PsPool([k.ps([128, 1024], BF16, f"pT{i}", st) for i in range(2)])
                pp = PsPool([k.ps([128, 512], F32, f"pp{i}", st) for i in range(6 if L == 0 else 3)])
                if L == 1:
                    pTfs = PsPool([k.ps([128, 512], F32, f"pTf{i}", st) for i in range(2)])
                    plg = k.ps([128, 512], F32, "plg", st)
                    xTf = k.sb([128, 8, 128], F32, "xTf", st)
                    rtw = k.sb([128, 8, NE], F32, "rtw", st); g2r = k.sb([128, 8], F32, "g2r", st)
                    k.dma(sp, rtw[:], I["rtw"], writes=[rtw]); k.dma(sp, g2r[:], I["g21"], writes=[g2r])
                    k.op(dve, lambda h: h.tensor_tensor(out=rtw[:], in0=rtw[:], in1=bc(g2r[:], [128, 8, NE], 2), op=ALU.mult), reads=[rtw, g2r], writes=[rtw])
                    lg = k.sb([128, 4, NE], F32, "lg", st); m1 = k.sb([128, 4], F32, "m1", st); m2 = k.sb([128, 4], F32, "m2", st)
                    msk = k.sb([128, 4, NE], F32, "msk", st); lg2 = k.sb([128, 4, NE], F32, "lg2", st); ex = k.sb([128, 4, NE], F32, "ex", st)
                    den = k.sb([128, 4], F32, "den", st)

                def route(c, xin):
                    for j in range(4):
                        for half in range(2):
                            pTf = pTfs.next()
                            for q4 in range(4):
                                kc = half * 4 + q4
                                k.op(pe, lambda h, j=j, kc=kc, q4=q4, pTf=pTf: h.transpose(out=pTf[:, q4 * 128:(q4 + 1) * 128], in_=xin[:, j, kc * 128:(kc + 1) * 128], identity=identf[:]),
                                     reads=[xin.sub(j), identf], writes=[pTf], inc=(q4 == 3))
                            k.op(act, lambda h, half=half, pTf=pTf: h.activation(out=xTf[:, half * 4:(half + 1) * 4, :].rearrange("p a b -> p (a b)"), in_=pTf[:], func=AF.Copy), reads=[pTf], writes=[xTf])
                        for kc in range(8):
                            k.op(pe, lambda h, kc=kc: h.matmul(plg[:, 0:NE], lhsT=xTf[:, kc, :], rhs=rtw[:, kc, :], start=(kc == 0), stop=(kc == 7)), reads=[xTf, rtw], writes=[plg], inc=(kc == 7))
                        k.op(dve, lambda h, j=j: h.tensor_scalar(out=lg[:, j, :], in0=plg[:, 0:NE], scalar1=rstd[:, j:j + 1], scalar2=None, op0=ALU.mult), reads=[plg, rstd], writes=[lg])
                    sh = [128, 4, NE]
                    k.op(dve, lambda h: h.tensor_reduce(out=m1[:], in_=lg[:], axis=AX.X, op=ALU.max), reads=[lg], writes=[m1])
                    k.op(dve, lambda h: h.tensor_tensor(out=msk[:], in0=lg[:], in1=bc(m1[:], sh, 2), op=ALU.is_equal), reads=[lg, m1], writes=[msk])
                    k.op(dve, lambda h: h.scalar_tensor_tensor(out=lg2[:], in0=msk[:], scalar=-1e30, in1=lg[:], op0=ALU.mult, op1=ALU.add), reads=[msk, lg], writes=[lg2])
                    k.op(dve, lambda h: h.tensor_reduce(out=m2[:], in_=lg2[:], axis=AX.X, op=ALU.max), reads=[lg2], writes=[m2])
                    k.op(dve, lambda h: h.tensor_tensor(out=msk[:], in0=lg[:], in1=bc(m2[:], sh, 2), op=ALU.is_ge), reads=[lg, m2], writes=[msk])
                    k.op(dve, lambda h: h.tensor_tensor(out=ex[:], in0=lg[:], in1=bc(m1[:], sh, 2), op=ALU.subtract), reads=[lg, m1], writes=[ex])
                    k.op(dve, lambda h: h.tensor_scalar(out=ex[:], in0=ex[:], scalar1=-80.0, scalar2=None, op0=ALU.max), reads=[ex], writes=[ex])
                    k.op(act, lambda h: h.activation(out=ex[:], in_=ex[:], func=AF.Exp), reads=[ex], writes=[ex])
                    k.op(dve, lambda h: h.tensor_tensor(out=ex[:], in0=ex[:], in1=msk[:], op=ALU.mult), reads=[ex, msk], writes=[ex])
                    k.op(dve, lambda h: h.tensor_reduce(out=den[:], in_=ex[:], axis=AX.X, op=ALU.add), reads=[ex], writes=[den])
                    k.op(dve, lambda h: h.reciprocal(out=den[:], in_=den[:]), reads=[den], writes=[den])
                    k.op(dve, lambda h: h.tensor_tensor(out=wr[:, 4 * c:4 * c + 4, :], in0=ex[:], in1=bc(den[:], sh, 2), op=ALU.mult), reads=[ex, den], writes=[wr])

                def loads(c):
                    i2 = c % 2
                    k.dma(sp, xins[i2][:], Xc[c].t, reads=[Xc[c]], writes=xins[i2].all())
                    k.dma(sp, sgl[i2][:], SGA[c].t, reads=[SGA[c]], writes=[sgl[i2]])
                    k.dma(sp, mbl[i2][:], MB[c].t, reads=[MB[c]], writes=[mbl[i2]])
                def ua_part(c):
                    sg_ = sgl[c % 2]; mb_ = mbl[c % 2]
                    for dc in range(8):
                        pu = pp.next(); tf = tmpf[dc % 2]
                        for pr in range(4):
                            k.op(pe, lambda h, pr=pr, dc=dc, pu=pu: h.matmul(pu[:], lhsT=Wupa[:, pr, dc * 128:(dc + 1) * 128], rhs=OT[:, pr, c * 512:(c + 1) * 512], start=(pr == 0), stop=(pr == 3)),
                                 reads=[Wupa, OT], writes=[pu], inc=(pr == 3))
                        k.op(dve, lambda h, dc=dc, pu=pu, tf=tf: h.tensor_tensor(out=tf[:], in0=pu[:], in1=sg_[:, dc, :], op=ALU.mult), reads=[pu, sg_], writes=[tf])
                        k.op(dve, lambda h, dc=dc, tf=tf: h.tensor_tensor(out=mg[:, dc, :], in0=tf[:], in1=mb_[:, dc, :], op=ALU.add), reads=[tf, mb_], writes=[mg.sub(dc)])

                def wo_part(c):
                    xin = xins[c % 2]
                    for j in range(4):
                        for nh in range(2):
                            py = pp.next()
                            for dc in range(8):
                                k.op(pe, lambda h, j=j, nh=nh, dc=dc, py=py: h.matmul(py[:], lhsT=mg[:, dc, j * 128:(j + 1) * 128], rhs=Wo[:, dc, nh * 512:(nh + 1) * 512], start=(dc == 0), stop=(dc == 7)),
                                     reads=[mg.sub(dc), Wo], writes=[py], inc=(dc == 7))
                            k.op(dve, lambda h, j=j, nh=nh, py=py: h.tensor_tensor(out=xin[:, j, nh * 512:(nh + 1) * 512], in0=py[:], in1=xin[:, j, nh * 512:(nh + 1) * 512], op=ALU.add), reads=[py, xin.sub(j)], writes=[xin.sub(j)])

                def tail_part(c):
                    xin = xins[c % 2]
                    k.dma(sp, Y_c[c].t, xin[:], reads=xin.all(), writes=[Y_c[c]])
                    rms_T(xin, ssq, rstd, hb, hT, pTs, junk)
                    k.dma(sp, H2T[c].t, hT[:], reads=hT.all(), writes=[H2T[c]])
                    if L == 1:
                        route(c, xin)

                loads(0)
                ua_part(0)
                for c in range(NCH):
                    if c + 1 < NCH:
                        loads(c + 1)
                    wo_part(c)
                    if c + 1 < NCH:
                        ua_part(c + 1)
                    tail_part(c)
                end_phase()
            st23.close()
            if stop_after == f"P3{L}":
                break

            if L == 0:
                units = [dict(wg=I["wg"], wu=I["wu"], wd=I["wd"], f0=f0, nf=nf, e=None) for (f0, nf) in ((0, 8), (8, 7), (15, 7))]
            else:
                units = [dict(wg=I["mwg"][e], wu=I["mwu"][e], wd=I["mwd"][e], f0=q4 * 7, nf=7, e=e) for e in range(NE) for q4 in range(4)]
            NF = 8
            with ExitStack() as st:
                stg = PsPool([k.sb([128, 1024], F32, f"stg{i}", st) for i in range(4)])
                g2 = k.sb([128, 8], F32, "g2", st)
                k.dma(sp, g2[:], I[f"g2{L}"], writes=[g2])
                Wsets = []
                for i in range(2):
                    Wg_t = k.sb([128, 8, NF * 128], BF16, f"Wg{i}", st); Wu_t = k.sb([128, 8, NF * 128], BF16, f"Wu{i}", st)
                    Wd_t = k.sb([128, NF, 1024], BF16, f"Wd{i}", st)
                    Wsets.append((Wg_t, Wu_t, Wd_t))
                xin = k.sb([128, 4, 1024], F32, "xin", st)
                hTs = [k.sb([128, 8, 512], BF16, f"hT{i}", st) for i in range(2)]
                acts = [k.sb([128, NF, 512], BF16, f"actb{i}", st) for i in range(2)]
                sgt = [k.sb([128, 512], F32, f"sgt{i}", st) for i in range(2)]
                pp = PsPool([k.ps([128, 512], F32, f"pp{i}", st) for i in range(8)])

                def loads(u, c):
                    i2 = (u * NCH + c) % 2
                    k.dma(sp, hTs[i2][:], H2T[c].t, reads=[H2T[c]], writes=[hTs[i2]])

                def loadx(c):
                    k.dma(sp, xin[:], Y_c[c].t, reads=[Y_c[c]], writes=[xin])

                def unit_pieces(u):
                    un = units[u]; nf = un["nf"]; c0 = un["f0"] * 128
                    Wg_t, Wu_t, Wd_t = Wsets[u % 2]
                    for (src, Wt) in ((un["wg"], Wg_t), (un["wu"], Wu_t)):
                        for kc in range(8):
                            sg = stg.next()
                            k.dma(pool, sg[:, 0:nf * 128], src[kc * 128:(kc + 1) * 128, c0:c0 + nf * 128], writes=[sg])
                            cast(Wt[:, kc, 0:nf * 128], sg[:, 0:nf * 128], g2[:, kc:kc + 1], [sg, g2], [Wt])
                            yield
                    for f in range(nf):
                        sg = stg.next()
                        k.dma(pool, sg[:, 0:1024], un["wd"][c0 + f * 128:c0 + (f + 1) * 128, :], writes=[sg])
                        cast(Wd_t[:, f, :], sg[:, 0:1024], None, [sg], [Wd_t])
                        yield

                for _ in unit_pieces(0):
                    pass
                loads(0, 0)
                loadx(0)
                for u, un in enumerate(units):
                    nf = un["nf"]; e = un["e"]
                    Wg_t, Wu_t, Wd_t = Wsets[u % 2]
                    nxt = unit_pieces(u + 1) if u + 1 < len(units) else iter(())
                    for c in range(NCH):
                        i2 = (u * NCH + c) % 2
                        nu, ncn = (u, c + 1) if c + 1 < NCH else (u + 1, 0)
                        if nu < len(units):
                            loads(nu, ncn)
                        hT = hTs[i2]; ab = acts[i2]
                        for f in range(nf):
                            pg = pp.next(); pu = pp.next(); sg_ = sgt[f % 2]
                            for (p_, Wt) in ((pg, Wg_t), (pu, Wu_t)):
                                for kc in range(8):
                                    k.op(pe, lambda h, kc=kc, f=f, p_=p_, Wt=Wt, hT=hT: h.matmul(p_[:], lhsT=Wt[:, kc, f * 128:(f + 1) * 128], rhs=hT[:, kc, :], start=(kc == 0), stop=(kc == 7)),
                                         reads=[Wt, hT], writes=[p_], inc=(kc == 7))
                            k.op(act, lambda h, pg=pg, sg_=sg_: h.activation(out=sg_[:], in_=pg[:], func=AF.Silu), reads=[pg], writes=[sg_])
                            k.op(dve, lambda h, f=f, pu=pu, sg_=sg_, ab=ab: h.tensor_tensor(out=ab[:, f, :], in0=pu[:], in1=sg_[:], op=ALU.mult), reads=[pu, sg_], writes=[ab])
                            if f % 2 == 1:
                                next(nxt, None)
                        for j in range(4):
                            for nh in range(2):
                                py = pp.next()
                                for f in range(nf):
                                    k.op(pe, lambda h, j=j, nh=nh, f=f, py=py, ab=ab, nf=nf, Wd_t=Wd_t: h.matmul(py[:], lhsT=ab[:, f, j * 128:(j + 1) * 128], rhs=Wd_t[:, f, nh * 512:(nh + 1) * 512], start=(f == 0), stop=(f == nf - 1)),
                                         reads=[ab, Wd_t], writes=[py], inc=(f == nf - 1))
                                xs_ = xin[:, j, nh * 512:(nh + 1) * 512]
                                if e is None:
                                    k.op(dve, lambda h, py=py, xs_=xs_: h.tensor_tensor(out=xs_, in0=py[:], in1=xs_, op=ALU.add), reads=[py, xin], writes=[xin])
                                else:
                                    t = 4 * c + j
                                    k.op(dve, lambda h, py=py, xs_=xs_, t=t, e=e: h.scalar_tensor_tensor(out=xs_, in0=py[:], scalar=wr[:, t, e:e + 1], in1=xs_, op0=ALU.mult, op1=ALU.add),
                                         reads=[py, xin, wr], writes=[xin])
                        k.dma(sp, Y_c[c].t, xin[:], reads=[xin], writes=[Y_c[c]])
                        if nu < len(units):
                            loadx(ncn)
                    for _ in nxt:
                        pass
                end_phase()
            if stop_after == f"P5{L}":
                break

        if debug:
            srcs = dict(h1t=H1T_t, h2t=H2T_t, qt=QT_t, kt=KT_t, vv=V_t, sga=SGA_t, mb=MB_t)
            with ExitStack() as st:
                for nm in debug:
                    if nm in srcs:
                        k.dma(sp, dbg_out[nm], srcs[nm], reads=H1T + H2T + QT + KT + Vd + SGA + MB)
                    elif nm == "wr":
                        k.dma(sp, dbg_out[nm], wr[:], reads=[wr])
                    elif nm == "rope":
                        k.dma(sp, dbg_out[nm][:, 0], cost[:], reads=[cost]); k.dma(sp, dbg_out[nm][:, 1], sint[:], reads=[sint])
                end_phase()
    return nc


def host_inputs(inp):
    f32 = np.float32
    A = lambda a: np.ascontiguousarray(np.asarray(a))
    com = {}
    com["ident"] = np.eye(128, dtype=f32)
    com["tri"] = np.triu(np.ones((128, 128), dtype=f32))
    invf = (np.float32(10000.0) ** (-np.arange(0, 32, 2, dtype=f32) / np.float32(32))).astype(f32)
    com["invf"] = A(np.broadcast_to(invf[None, :], (128, 16)))
    pk = lambda v, n: A(np.asarray(v, dtype=f32).reshape(n, 128).T)
    for L in range(2):
        com[f"w_in{L}"] = A(inp["w_in"][L]); com[f"g1{L}"] = pk(inp["norm1_g"][L], 8)
        com[f"w_uq{L}"] = A(inp["w_uq"][L]); com[f"qng{L}"] = pk(inp["q_norm_g"][L], 2)
        com[f"w_ukv{L}"] = A(inp["w_ukv"][L]); com[f"kvng{L}"] = pk(inp["kv_norm_g"][L], 1)
        com[f"qkg{L}"] = A(np.broadcast_to(np.concatenate([inp["qk_q_g"][L], inp["qk_k_g"][L]])[None, :], (128, 192)))
        com[f"w_upa{L}"] = A(inp["w_up_attn"][L])
        com[f"convw{L}"] = A(np.asarray(inp["conv_w"][L]).reshape(4, 4, 128).transpose(2, 1, 0))
        lv = np.stack([inp["conv_b"][L], inp["b_rg"][L], inp["b_ig"][L], inp["lru_lambda"][L]], axis=-1)
        com[f"lruv{L}"] = A(lv.reshape(4, 128, 4).transpose(1, 0, 2))
        for nm, key in ((f"w_rg{L}", "w_rg"), (f"w_ig{L}", "w_ig")):
            w = np.asarray(inp[key][L])
            bd = np.zeros((128, 4, 128), dtype=f32)
            for fc in range(4):
                bd[0:64, fc, 0:64] = w[2 * fc]; bd[64:128, fc, 64:128] = w[2 * fc + 1]
            com[nm] = bd
        com[f"w_upl{L}"] = A(inp["w_up_lru"][L]); com[f"w_o{L}"] = A(inp["w_o"][L]); com[f"g2{L}"] = pk(inp["norm2_g"][L], 8)
    com["wg"] = A(inp["ffn_w_gate"][0]); com["wu"] = A(inp["ffn_w_up"][0]); com["wd"] = A(inp["ffn_w_down"][0])
    com["mwg"] = A(inp["moe_w_gate"][0]); com["mwu"] = A(inp["moe_w_up"][0]); com["mwd"] = A(inp["moe_w_down"][0])
    com["rtw"] = A(np.asarray(inp["moe_router"][0], dtype=f32).reshape(8, 128, NE).transpose(1, 0, 2))
    maps = []
    for b in range(8):
        m = dict(com)
        m["x"] = A(inp["x"][b]); m["pos"] = A(np.asarray(inp["positions"][b], dtype=np.int32).reshape(NT, 128).T)
        maps.append(m)
    return maps


def kernel(**inputs):
    nc = build()
    maps = host_inputs(inputs)
    res = run_bass_kernel_spmd(nc, maps, core_ids=list(range(8)))
    return np.stack([np.asarray(r["y"], dtype=np.float32).reshape(S, D) for r in res.results], axis=0)
```

```python
import numpy as np
from contextlib import ExitStack
import concourse.bass as bass
import concourse.mybir as mybir
from concourse.bass_utils import run_bass_kernel_spmd

F32 = mybir.dt.float32
BF16 = mybir.dt.bfloat16
I32 = mybir.dt.int32
ALU = mybir.AluOpType
AF = mybir.ActivationFunctionType
AX = mybir.AxisListType


class Eng:
    def __init__(self, name, handle, sem, same_sync):
        self.name = name
        self.h = handle
        self.sem = sem
        self.count = 0
        self.seen = {}
        self.ops = []
        self.same_sync = same_sync
        self.dma_sems = []
        self.dma_tgt = []
        self.dma_k = 0


class Buf:
    def __init__(self, t, name=""):
        self.t = t
        self.name = name
        self.w = None
        self.r = {}

    def __getitem__(self, idx):
        return self.t[idx]

    def sub(self, key):
        if not hasattr(self, "_subs"):
            self._subs = {}
        if key not in self._subs:
            self._subs[key] = Buf(self.t, f"{self.name}.{key}")
        return self._subs[key]

    def all(self):
        return list(getattr(self, "_subs", {}).values())


class K:
    def __init__(self, nc, es, n_dma_sems=8):
        self.nc = nc
        self.es = es
        mk = lambda nm: es.enter_context(nc.semaphore(nm))
        self.pe = Eng("pe", nc.tensor, mk("s_pe"), False)
        self.dve = Eng("dve", nc.vector, mk("s_dve"), True)
        self.act = Eng("act", nc.scalar, mk("s_act"), True)
        self.pool = Eng("pool", nc.gpsimd, mk("s_pool"), True)
        self.sp = Eng("sp", nc.sync, mk("s_sp"), False)
        self.engs = [self.pe, self.dve, self.act, self.pool, self.sp]
        for q in (self.sp, self.act, self.pool):
            for i in range(n_dma_sems):
                q.dma_sems.append(mk(f"d_{q.name}{i}"))
                q.dma_tgt.append(0)
        self.nbuf = 0

    def sb(self, shape, dtype, name=None, stack=None):
        self.nbuf += 1
        name = name or f"sb{self.nbuf}"
        t = (stack or self.es).enter_context(self.nc.sbuf_tensor(f"{name}_{self.nbuf}", list(shape), dtype))
        return Buf(t, name)

    def ps(self, shape, dtype, name=None, stack=None):
        self.nbuf += 1
        name = name or f"ps{self.nbuf}"
        t = (stack or self.es).enter_context(self.nc.psum_tensor(f"{name}_{self.nbuf}", list(shape), dtype))
        return Buf(t, name)

    def dram(self, name, shape, dtype, kind="Internal"):
        t = self.nc.dram_tensor(name, list(shape), dtype, kind=kind)
        return Buf(t.ap(), name)

    def _collect(self, eng, reads, writes):
        need = {}

        def add(ev):
            if ev is None:
                return
            sem, val, key = ev
            if key == id(eng.sem) and not eng.same_sync:
                return
            if eng.seen.get(key, 0) >= val:
                return
            if key not in need or need[key][1] < val:
                need[key] = ev

        for b in reads:
            add(b.w)
        for b in writes:
            add(b.w)
            for ev in b.r.values():
                add(ev)
        for key, (sem, val, _) in need.items():
            eng.ops.append(("wait", sem, val))
            eng.seen[key] = val

    def _mark(self, ev, reads, writes):
        key = ev[2]
        for b in reads:
            old = b.r.get(key)
            if old is None or old[1] < ev[1]:
                b.r[key] = ev
        for b in writes:
            b.w = ev
            b.r = {}

    def op(self, eng, fn, reads=(), writes=(), inc=True):
        self._collect(eng, reads, writes)
        eng.ops.append(("op", fn, inc))
        if inc:
            eng.count += 1
            ev = (eng.sem, eng.count, id(eng.sem))
        else:
            ev = (eng.sem, eng.count + 1, id(eng.sem))
        self._mark(ev, reads, writes)

    def dma(self, q, out, in_, reads=(), writes=()):
        self._collect(q, reads, writes)
        i = q.dma_k % len(q.dma_sems)
        q.dma_k += 1
        sem = q.dma_sems[i]
        prev = q.dma_tgt[i]
        if prev > 0 and q.seen.get(id(sem), 0) < prev:
            q.ops.append(("wait", sem, prev))
            q.seen[id(sem)] = prev
        tgt = prev + 16
        q.dma_tgt[i] = tgt
        q.ops.append(("dma", out, in_, sem))
        ev = (sem, tgt, id(sem))
        self._mark(ev, reads, writes)

    def wait_all_dma(self, eng):
        for q in (self.sp, self.act, self.pool):
            for sem, tgt in zip(q.dma_sems, q.dma_tgt):
                if tgt > 0 and eng.seen.get(id(sem), 0) < tgt:
                    eng.ops.append(("wait", sem, tgt))
                    eng.seen[id(sem)] = tgt

    def simulate(self):
        names = {}
        for e in self.engs:
            names[id(e.sem)] = e.name
            for i, d in enumerate(e.dma_sems):
                names[id(d)] = f"dma_{e.name}{i}"
        if not hasattr(self, "_simval"):
            self._simval = {}
        val = self._simval
        pos = {e.name: 0 for e in self.engs}
        progress = True
        while progress:
            progress = False
            for e in self.engs:
                while pos[e.name] < len(e.ops):
                    o = e.ops[pos[e.name]]
                    if o[0] == "wait":
                        if val.get(id(o[1]), 0) >= o[2]:
                            pos[e.name] += 1; progress = True
                        else:
                            break
                    elif o[0] == "op":
                        if o[2]:
                            val[id(e.sem)] = val.get(id(e.sem), 0) + 1
                        pos[e.name] += 1; progress = True
                    else:
                        val[id(o[3])] = val.get(id(o[3]), 0) + 16
                        pos[e.name] += 1; progress = True
        stuck = [e for e in self.engs if pos[e.name] < len(e.ops)]
        if stuck:
            msg = []
            for e in stuck:
                o = e.ops[pos[e.name]]
                nxt = next((x for x in e.ops[pos[e.name]:] if x[0] != "wait"), None)
                line = nxt[1].__code__.co_firstlineno if nxt is not None and nxt[0] == "op" else "dma"
                msg.append(f"{e.name} blocked at op#{pos[e.name]} waiting {names.get(id(o[1]))}>={o[2]} (now {val.get(id(o[1]), 0)}), next op from source line {line}")
            raise RuntimeError("DEADLOCK in recorded program:\n" + "\n".join(msg))

    def emit(self):
        nc = self.nc
        with nc.Block() as block:
            def replay(eng):
                def body(h):
                    for o in eng.ops:
                        if o[0] == "wait":
                            h.wait_ge(o[1], o[2])
                        elif o[0] == "op":
                            ins = o[1](h)
                            if o[2]:
                                ins.then_inc(eng.sem, 1)
                        else:
                            h.dma_start(out=o[1], in_=o[2]).then_inc(o[3], 16)
                    eng.ops = []
                return body
            block.tensor(replay(self.pe))
            block.vector(replay(self.dve))
            block.scalar(replay(self.act))
            block.gpsimd(replay(self.pool))
            block.sync(replay(self.sp))


S = 4096; D = 1024; NT = 32; NCH = 8
EPS = 1e-6
TWO_PI = 6.283185307179586
C1 = 6.28125
C2 = TWO_PI - C1
PI_SAFE = 3.1415925
MAGIC = 12582912.0
D_FF = 2816; D_FFE = 3584; NE = 8


class PsPool:
    def __init__(self, bufs):
        self.bufs = bufs; self.i = 0
    def next(self):
        b = self.bufs[self.i % len(self.bufs)]; self.i += 1
        return b


def bc(ap, shape, axis):
    return ap.unsqueeze(axis).to_broadcast(list(shape))


def build(debug=None, stop_after=None):
    nc = bass.Bass("TRN2", target_bir_lowering=False)
    def din(name, shape, dt=F32):
        return nc.dram_tensor(name, list(shape), dt, kind="ExternalInput").ap()
    I = {}
    I["x"] = din("x", [S, D]); I["pos"] = din("pos", [128, NT], I32)
    I["ident"] = din("ident", [128, 128]); I["tri"] = din("tri", [128, 128]); I["invf"] = din("invf", [128, 16])
    for L in range(2):
        I[f"w_in{L}"] = din(f"w_in{L}", [1024, 3488]); I[f"g1{L}"] = din(f"g1{L}", [128, 8])
        I[f"w_uq{L}"] = din(f"w_uq{L}", [256, 768]); I[f"qng{L}"] = din(f"qng{L}", [128, 2])
        I[f"w_ukv{L}"] = din(f"w_ukv{L}", [128, 1024]); I[f"kvng{L}"] = din(f"kvng{L}", [128, 1])
        I[f"qkg{L}"] = din(f"qkg{L}", [128, 192]); I[f"w_upa{L}"] = din(f"w_upa{L}", [512, 1024])
        I[f"convw{L}"] = din(f"convw{L}", [128, 4, 4]); I[f"lruv{L}"] = din(f"lruv{L}", [128, 4, 4])
        I[f"w_rg{L}"] = din(f"w_rg{L}", [128, 4, 128]); I[f"w_ig{L}"] = din(f"w_ig{L}", [128, 4, 128])
        I[f"w_upl{L}"] = din(f"w_upl{L}", [512, 1024]); I[f"w_o{L}"] = din(f"w_o{L}", [1024, 1024])
        I[f"g2{L}"] = din(f"g2{L}", [128, 8])
    I["wg"] = din("wg", [1024, D_FF]); I["wu"] = din("wu", [1024, D_FF]); I["wd"] = din("wd", [D_FF, 1024])
    I["mwg"] = din("mwg", [NE, 1024, D_FFE]); I["mwu"] = din("mwu", [NE, 1024, D_FFE]); I["mwd"] = din("mwd", [NE, D_FFE, 1024])
    I["rtw"] = din("rtw", [128, 8, NE])
    y_out = nc.dram_tensor("y", [S, D], F32, kind="ExternalOutput").ap()
    dbg_out = {}
    if debug:
        for nm, (shape, dt) in debug.items():
            dbg_out[nm] = nc.dram_tensor("dbg_" + nm, list(shape), dt, kind="ExternalOutput").ap()

    with ExitStack() as es:
        k = K(nc, es)
        pe, dve, act, pool, sp = k.pe, k.dve, k.act, k.pool, k.sp

        def chunked(name, shape, dt, view_fn):
            t = nc.dram_tensor(name, list(shape), dt, kind="Internal").ap()
            return t, [Buf(view_fn(t, c), f"{name}{c}") for c in range(NCH)]
        tokview = lambda t, c: t.rearrange("(j p) d -> p j d", p=128)[:, 4 * c:4 * c + 4, :]
        Xin_c = [Buf(tokview(I["x"], c), f"x{c}") for c in range(NCH)]
        Y_c = [Buf(tokview(y_out, c), f"y{c}") for c in range(NCH)]
        fmview = lambda t, c: t[:, :, c * 512:(c + 1) * 512]
        H1T_t, H1T = chunked("h1t", [128, 8, S], BF16, fmview)
        H2T_t, H2T = chunked("h2t", [128, 8, S], BF16, fmview)
        QT_t, QT = chunked("qt", [96, 8, S], BF16, fmview)
        KT_t, KT = chunked("kt", [96, 8, S], BF16, fmview)
        V_t, Vd = chunked("vv", [128, NT, 768], BF16, lambda t, c: t[:, 4 * c:4 * c + 4, :])
        SGA_t, SGA = chunked("sga", [128, 8, S], BF16, fmview)
        MB_t, MB = chunked("mb", [128, 8, S], BF16, fmview)

        identb = k.sb([128, 128], BF16, "identb")
        identf = k.sb([128, 128], F32, "identf")
        trib = k.sb([128, 128], BF16, "trib")
        cost = k.sb([128, NT, 16], F32, "cost")
        sint = k.sb([128, NT, 16], F32, "sint")
        epsb = k.sb([128, 1], F32, "epsb")
        wr = k.sb([128, NT, NE], F32, "wr")

        phase_no = [0]

        def end_phase():
            k.wait_all_dma(sp)
            phase_no[0] += 1
            k.simulate()
            with nc.named_scope(f"ph{phase_no[0]:02d}"):
                k.emit()

        with ExitStack() as st:
            idf = identf; trf = k.sb([128, 128], F32, "trf", st)
            posi = k.sb([128, NT], I32, "posi", st); posf = k.sb([128, NT], F32, "posf", st)
            invf = k.sb([128, 16], F32, "invf", st)
            ang = k.sb([128, NT, 16], F32, "ang", st)
            u = k.sb([128, NT, 16], F32, "u", st); nn = k.sb([128, NT, 16], F32, "nn", st)
            k.dma(sp, idf[:], I["ident"], writes=[idf]); k.dma(sp, trf[:], I["tri"], writes=[trf])
            k.dma(sp, posi[:], I["pos"], writes=[posi]); k.dma(sp, invf[:], I["invf"], writes=[invf])
            k.op(pool, lambda h: h.memset(epsb[:], EPS), writes=[epsb])
            k.op(pool, lambda h: h.tensor_copy(out=identb[:], in_=idf[:]), reads=[idf], writes=[identb])
            k.op(pool, lambda h: h.tensor_copy(out=trib[:], in_=trf[:]), reads=[trf], writes=[trib])
            k.op(dve, lambda h: h.tensor_copy(out=posf[:], in_=posi[:]), reads=[posi], writes=[posf])
            k.op(dve, lambda h: h.tensor_tensor(out=ang[:], in0=bc(posf[:], [128, NT, 16], 2), in1=bc(invf[:], [128, NT, 16], 1), op=ALU.mult),
                 reads=[posf, invf], writes=[ang])
            for tab, shift in ((sint, 0.0), (cost, TWO_PI / 4)):
                k.op(dve, lambda h, shift=shift: h.tensor_scalar(out=u[:], in0=ang[:], scalar1=shift, scalar2=None, op0=ALU.add), reads=[ang], writes=[u])
                k.op(dve, lambda h: h.tensor_scalar(out=nn[:], in0=u[:], scalar1=1.0 / TWO_PI, scalar2=MAGIC, op0=ALU.mult, op1=ALU.add), reads=[u], writes=[nn])
                k.op(dve, lambda h: h.tensor_scalar(out=nn[:], in0=nn[:], scalar1=MAGIC, scalar2=None, op0=ALU.subtract), reads=[nn], writes=[nn])
                k.op(dve, lambda h: h.scalar_tensor_tensor(out=u[:], in0=nn[:], scalar=-C1, in1=u[:], op0=ALU.mult, op1=ALU.add), reads=[nn, u], writes=[u])
                k.op(dve, lambda h: h.scalar_tensor_tensor(out=u[:], in0=nn[:], scalar=-C2, in1=u[:], op0=ALU.mult, op1=ALU.add), reads=[nn, u], writes=[u])
                k.op(dve, lambda h: h.tensor_scalar(out=u[:], in0=u[:], scalar1=-PI_SAFE, scalar2=PI_SAFE, op0=ALU.max, op1=ALU.min), reads=[u], writes=[u])
                k.op(act, lambda h, tab=tab: h.activation(out=tab[:], in_=u[:], func=AF.Sin), reads=[u], writes=[tab])
            end_phase()

        cast_i = [0]

        def cast(out_ap, in_ap, scale_ap, reads, writes, eng=None):
            if eng is None:
                eng = act if cast_i[0] % 2 == 0 else dve
                cast_i[0] += 1
            if eng is act:
                if scale_ap is None:
                    k.op(act, lambda h: h.activation(out=out_ap, in_=in_ap, func=AF.Copy), reads=reads, writes=writes)
                else:
                    k.op(act, lambda h: h.activation(out=out_ap, in_=in_ap, func=AF.Copy, scale=scale_ap), reads=reads, writes=writes)
            else:
                if scale_ap is None:
                    k.op(eng, lambda h: h.tensor_copy(out=out_ap, in_=in_ap), reads=reads, writes=writes)
                else:
                    k.op(eng, lambda h: h.tensor_scalar(out=out_ap, in0=in_ap, scalar1=scale_ap, scalar2=None, op0=ALU.mult), reads=reads, writes=writes)

        def load_w(st_bufs, dst, dst_fn, src, nk, n, scale=None, c0=0, q=None, eng=None):
            q = q or sp
            for kc in range(nk):
                sg = st_bufs.next()
                k.dma(q, sg[:, 0:n], src[kc * 128:(kc + 1) * 128, c0:c0 + n], writes=[sg])
                if scale is None:
                    cast(dst_fn(kc), sg[:, 0:n], None, [sg], [dst], eng)
                else:
                    cast(dst_fn(kc), sg[:, 0:n], scale[:, kc:kc + 1], [sg, scale], [dst], eng)

        def rms_T(xin, ssq, rstd, hb, hT, pTs, junk):
            for j in range(4):
                k.op(act, lambda h, j=j: h.activation(out=junk[:], in_=xin[:, j, :], func=AF.Square, scale=1.0 / 32.0, accum_out=ssq[:, j:j + 1]),
                     reads=[xin.sub(j)], writes=[ssq])
            k.op(act, lambda h: h.activation(out=ssq[:], in_=ssq[:], func=AF.Sqrt, bias=epsb[:, 0:1], scale=1.0), reads=[ssq, epsb], writes=[ssq])
            k.op(dve, lambda h: h.reciprocal(out=rstd[:], in_=ssq[:]), reads=[ssq], writes=[rstd])
            for j in range(4):
                if j % 2 == 0:
                    k.op(act, lambda h, j=j: h.activation(out=hb[:, j, :], in_=xin[:, j, :], func=AF.Copy, scale=rstd[:, j:j + 1]), reads=[xin.sub(j), rstd], writes=[hb.sub(j)])
                else:
                    k.op(dve, lambda h, j=j: h.tensor_scalar(out=hb[:, j, :], in0=xin[:, j, :], scalar1=rstd[:, j:j + 1], scalar2=None, op0=ALU.mult),
                         reads=[xin.sub(j), rstd], writes=[hb.sub(j)])
            for kc in range(8):
                pT = pTs.next()
                for j in range(4):
                    k.op(pe, lambda h, j=j, kc=kc, pT=pT: h.transpose(out=pT[:, j * 128:(j + 1) * 128], in_=hb[:, j, kc * 128:(kc + 1) * 128], identity=identb[:]),
                         reads=[hb.sub(j), identb], writes=[pT], inc=(j == 3))
                e = act if kc % 2 == 0 else dve
                if e is act:
                    k.op(act, lambda h, kc=kc, pT=pT: h.activation(out=hT[:, kc, :], in_=pT[:, 0:512], func=AF.Copy), reads=[pT], writes=[hT.sub(kc)])
                else:
                    k.op(dve, lambda h, kc=kc, pT=pT: h.tensor_copy(out=hT[:, kc, :], in_=pT[:, 0:512]), reads=[pT], writes=[hT.sub(kc)])

        def rope(x1, x2, cs, sn, d1, d2, tmps, rd, wr_, tb, eng=None):
            eng = eng or dve
            t1, t2 = tmps
            b1, b2 = tb
            k.op(eng, lambda h: h.tensor_tensor(out=t1, in0=x1, in1=cs, op=ALU.mult), reads=rd, writes=[b1])
            yield
            k.op(eng, lambda h: h.tensor_tensor(out=t2, in0=x2, in1=sn, op=ALU.mult), reads=rd, writes=[b2])
            yield
            k.op(eng, lambda h: h.tensor_tensor(out=d1, in0=t1, in1=t2, op=ALU.subtract), reads=[b1, b2], writes=wr_)
            yield
            k.op(eng, lambda h: h.tensor_tensor(out=t1, in0=x2, in1=cs, op=ALU.mult), reads=rd, writes=[b1])
            yield
            k.op(eng, lambda h: h.tensor_tensor(out=t2, in0=x1, in1=sn, op=ALU.mult), reads=rd, writes=[b2])
            yield
            k.op(eng, lambda h: h.tensor_tensor(out=d2, in0=t1, in1=t2, op=ALU.add), reads=[b1, b2], writes=wr_)
            yield

        dbg_dump = []
        for L in range(2):
            Xc = Xin_c if L == 0 else Y_c
            with ExitStack() as st:
                stg = PsPool([k.sb([128, 1024], F32, f"stg{i}", st) for i in range(2)])
                Wsm = k.sb([128, 8, 416], BF16, "Wsm", st); Wuq = k.sb([128, 2, 768], BF16, "Wuq", st); Wukv = k.sb([128, 1024], BF16, "Wukv", st)
                g1 = k.sb([128, 8], F32, "g1", st); qng = k.sb([128, 2], F32, "qng", st); kvng = k.sb([128, 1], F32, "kvng", st)
                qkg = k.sb([128, 192], F32, "qkg", st)
                for b_, nm in ((g1, "g1"), (qng, "qng"), (kvng, "kvng"), (qkg, "qkg")):
                    k.dma(sp, b_[:], I[f"{nm}{L}"], writes=[b_])
                load_w(stg, Wsm, lambda kc: Wsm[:, kc, :], I[f"w_in{L}"], 8, 416, g1)
                load_w(stg, Wuq, lambda kc: Wuq[:, kc, :], I[f"w_uq{L}"], 2, 768, qng)
                load_w(stg, Wukv, lambda kc: Wukv[:, :], I[f"w_ukv{L}"], 1, 1024, kvng)
                xins = [k.sb([128, 4, 1024], F32, f"xin{i}", st) for i in range(2)]
                for xb_ in xins:
                    for j in range(4):
                        xb_.sub(j)
                hb = k.sb([128, 4, 1024], BF16, "hb", st)
                hTs = [k.sb([128, 8, 512], BF16, f"hT{i}", st) for i in range(2)]
                junk = k.sb([128, 1024], BF16, "junk", st)
                chunkB = []
                for i in range(2):
                    chunkB.append((k.sb([128, 4, 416], F32, f"csm{i}", st), k.sb([128, 4, 4], F32, f"st4{i}", st), k.sb([128, 4, 2], F32, f"rs4{i}", st),
                                   k.sb([128, 4, 384], BF16, f"cqn{i}", st), k.sb([128, 3, 512], BF16, f"cT{i}", st),
                                   k.sb([128, 4], F32, f"ssq{i}", st), k.sb([128, 4], F32, f"rstd{i}", st)))
                tileB = []
                for i in range(2):
                    tileB.append((k.sb([128, 8, 96], F32, f"q_s{i}", st), k.sb([128, 8, 128], F32, f"kv_s{i}", st),
                                  k.sb([128, 8, 96], F32, f"sqt{i}", st), k.sb([128, 8, 64], F32, f"sqk{i}", st),
                                  k.sb([128, 16], F32, f"ss{i}", st), k.sb([128, 16], F32, f"rs{i}", st),
                                  k.sb([128, 8, 96], F32, f"qf{i}", st), k.sb([128, 8, 64], F32, f"kf{i}", st),
                                  k.sb([128, 8, 96], BF16, f"qb{i}", st), k.sb([128, 8, 96], BF16, f"kb{i}", st),
                                  k.sb([128, 32], F32, f"krg{i}", st), k.sb([128, 32], F32, f"krr{i}", st),
                                  k.sb([128, 2, 8, 16], F32, f"tmpB{i}", st), k.sb([128, 2, 16], F32, f"tmpK{i}", st),
                                  (Buf(None, "tq1"), Buf(None, "tq2")), (Buf(None, "tk1"), Buf(None, "tk2"))))
                QTs = k.sb([128, 8, 512], BF16, "QTs", st); KTs = k.sb([128, 8, 512], BF16, "KTs", st)
                Vs = k.sb([128, 4, 768], BF16, "Vs", st)
                pTs = PsPool([k.ps([128, 1024], BF16, f"pT{i}", st) for i in range(2)])
                psms = PsPool([k.ps([128, 512], F32, f"psm{i}", st) for i in range(2)])
                pq = k.ps([128, 2, 512], F32, "pq", st); pkv = k.ps([128, 2, 512], F32, "pkv", st)
                for j in range(4):
                    Vs.sub(j); QTs.sub(j); KTs.sub(j)
                k.op(pool, lambda h: h.memset(Vs[:], 1.0), writes=Vs.all())
                gq = qkg[:, 0:96]; gk = qkg[:, 96:192]
                def tile_gen(c, j, B, CB):
                    t = 4 * c + j
                    q_s, kv_s, sqt, sqk, ss, rs, qf, kf, qb, kb, krg, krr, tmpB, tmpK, tbq, tbk = B
                    csm, st4, rs4, cqn, cT, ssq, rstd = CB
                    if True:
                        for hh in range(2):
                            for kc in range(2):
                                k.op(pe, lambda h, j=j, hh=hh, kc=kc: h.matmul(pq[:, hh, 0:384], lhsT=cT[:, kc, j * 128:(j + 1) * 128], rhs=Wuq[:, kc, hh * 384:(hh + 1) * 384], start=(kc == 0), stop=(kc == 1)),
                                     reads=[cT.sub(kc), Wuq], writes=[pq], inc=(kc == 1))
                        for hh in range(2):
                            k.op(pe, lambda h, j=j, hh=hh: h.matmul(pkv[:, hh, :], lhsT=cT[:, 2, j * 128:(j + 1) * 128], rhs=Wukv[:, hh * 512:(hh + 1) * 512], start=True, stop=True),
                                 reads=[cT.sub(2), Wukv], writes=[pkv])
                        for hh in range(2):
                            k.op(act, lambda h, hh=hh: h.activation(out=q_s[:, 4 * hh:4 * hh + 4, :], in_=pq[:, hh, 0:384].rearrange("p (a d) -> p a d", a=4), func=AF.Copy), reads=[pq], writes=[q_s])
                            k.op(act, lambda h, hh=hh: h.activation(out=kv_s[:, 4 * hh:4 * hh + 4, :], in_=pkv[:, hh, :].rearrange("p (a d) -> p a d", a=4), func=AF.Copy), reads=[pkv], writes=[kv_s])
                        k.op(act, lambda h: h.activation(out=sqt[:], in_=q_s[:], func=AF.Square), reads=[q_s], writes=[sqt])
                        yield
                        k.op(act, lambda h: h.activation(out=sqk[:], in_=kv_s[:, :, 0:64], func=AF.Square), reads=[kv_s], writes=[sqk])
                        yield
                    if True:
                        k.op(dve, lambda h: h.tensor_reduce(out=ss[:, 0:8], in_=sqt[:], axis=AX.X, op=ALU.add), reads=[sqt], writes=[ss])
                        yield
                        k.op(dve, lambda h: h.tensor_reduce(out=ss[:, 8:16], in_=sqk[:], axis=AX.X, op=ALU.add), reads=[sqk], writes=[ss])
                        yield
                        k.op(dve, lambda h, j=j: h.tensor_scalar(out=ss[:, 8:16], in0=ss[:, 8:16], scalar1=st4[:, j, 2:3], scalar2=None, op0=ALU.add), reads=[ss, st4.sub(j)], writes=[ss])
                        yield
                        k.op(act, lambda h: h.activation(out=ss[:], in_=ss[:], func=AF.Sqrt, bias=epsb[:, 0:1], scale=1.0 / 96.0), reads=[ss, epsb], writes=[ss])
                        yield
                        k.op(dve, lambda h: h.reciprocal(out=rs[:], in_=ss[:]), reads=[ss], writes=[rs])
                        yield
                        k.op(dve, lambda h: h.tensor_scalar(out=rs[:, 0:8], in0=rs[:, 0:8], scalar1=96.0 ** -0.5, scalar2=None, op0=ALU.mult), reads=[rs], writes=[rs])
                        yield
                    if True:
                        k.op(dve, lambda h: h.tensor_tensor(out=qf[:], in0=q_s[:], in1=bc(rs[:, 0:8], [128, 8, 96], 2), op=ALU.mult), reads=[q_s, rs], writes=[qf])
                        yield
                        k.op(dve, lambda h: h.tensor_tensor(out=qf[:], in0=qf[:], in1=bc(gq, [128, 8, 96], 1), op=ALU.mult), reads=[qf, qkg], writes=[qf])
                        yield
                        k.op(act, lambda h: h.activation(out=qb[:, :, 0:64], in_=qf[:, :, 0:64], func=AF.Copy), reads=[qf], writes=[qb])
                        yield
                        cs8 = bc(cost[:, t, :], [128, 8, 16], 1); sn8 = bc(sint[:, t, :], [128, 8, 16], 1)
                        yield from rope(qf[:, :, 64:80], qf[:, :, 80:96], cs8, sn8, qb[:, :, 64:80], qb[:, :, 80:96], (tmpB[:, 0, :, :], tmpB[:, 1, :, :]), [qf, cost, sint], [qb], tbq)
                        k.op(dve, lambda h: h.tensor_tensor(out=kf[:], in0=kv_s[:, :, 0:64], in1=bc(rs[:, 8:16], [128, 8, 64], 2), op=ALU.mult), reads=[kv_s, rs], writes=[kf])
                        yield
                        k.op(dve, lambda h: h.tensor_tensor(out=kb[:, :, 0:64], in0=kf[:], in1=bc(qkg[:, 96:160], [128, 8, 64], 1), op=ALU.mult), reads=[kf, qkg], writes=[kb])
                        yield
                        k.op(pool, lambda h, j=j: h.tensor_tensor(out=krg[:], in0=csm[:, j, 384:416], in1=qkg[:, 160:192], op=ALU.mult), reads=[csm.sub(j), qkg], writes=[krg])
                        yield
                        yield from rope(krg[:, 0:16], krg[:, 16:32], cost[:, t, :], sint[:, t, :], krr[:, 0:16], krr[:, 16:32], (tmpK[:, 0, :], tmpK[:, 1, :]), [krg, cost, sint], [krr], tbk, pool)
                        k.op(pool, lambda h: h.tensor_tensor(out=kb[:, :, 64:96], in0=bc(krr[:], [128, 8, 32], 1), in1=bc(rs[:, 8:16], [128, 8, 32], 2), op=ALU.mult), reads=[krr, rs], writes=[kb])
                        yield
                        kv4 = kv_s[:].rearrange("p (a b) d -> p a b d", b=2)
                        Vv = Vs[:, j, :].rearrange("p (a c) -> p a c", c=192)
                        k.op(act, lambda h, kv4=kv4, Vv=Vv: h.activation(out=Vv[:, :, 0:64], in_=kv4[:, :, 0, 64:128], func=AF.Copy), reads=[kv_s], writes=[Vs.sub(j)])
                        yield
                        k.op(act, lambda h, kv4=kv4, Vv=Vv: h.activation(out=Vv[:, :, 128:192], in_=kv4[:, :, 1, 64:128], func=AF.Copy), reads=[kv_s], writes=[Vs.sub(j)])
                        yield
                    if True:
                        for src, dstT in ((qb, QTs), (kb, KTs)):
                            pT = pTs.next()
                            for hd in range(8):
                                k.op(pe, lambda h, hd=hd, pT=pT, src=src: h.transpose(out=pT[0:96, hd * 128:(hd + 1) * 128], in_=src[:, hd, :], identity=identb[:]),
                                     reads=[src, identb], writes=[pT], inc=(hd == 7))
                            k.op(dve, lambda h, j=j, pT=pT, dstT=dstT: h.tensor_copy(out=dstT[0:96, :, j * 128:(j + 1) * 128], in_=pT[0:96, :].rearrange("p (a d) -> p a d", a=8)), reads=[pT], writes=[dstT.sub(j)])

                k.dma(sp, xins[0][:], Xc[0].t, reads=[Xc[0]], writes=xins[0].all())

                def p1a_chunk(c, CB):
                    csm, st4, rs4, cqn, cT, ssq, rstd = CB
                    xin = xins[c % 2]; hT = hTs[c % 2]
                    if c + 1 < NCH:
                        k.dma(sp, xins[(c + 1) % 2][:], Xc[c + 1].t, reads=[Xc[c + 1]], writes=xins[(c + 1) % 2].all())
                    rms_T(xin, ssq, rstd, hb, hT, pTs, junk)
                    k.dma(sp, H1T[c].t, hT[:], reads=hT.all(), writes=[H1T[c]])
                    for j in range(4):
                        psm = psms.next()
                        for kc in range(8):
                            k.op(pe, lambda h, j=j, kc=kc, psm=psm, hT=hT: h.matmul(psm[:, 0:416], lhsT=hT[:, kc, j * 128:(j + 1) * 128], rhs=Wsm[:, kc, :], start=(kc == 0), stop=(kc == 7)),
                                 reads=[hT.sub(kc), Wsm], writes=[psm], inc=(kc == 7))
                        k.op(act, lambda h, j=j, psm=psm: h.activation(out=csm[:, j, :], in_=psm[:, 0:416], func=AF.Copy), reads=[psm], writes=[csm.sub(j)])
                        k.op(act, lambda h, j=j: h.activation(out=junk[:, 0:256], in_=csm[:, j, 0:256], func=AF.Square, scale=1.0 / 16.0, accum_out=st4[:, j, 0:1]), reads=[csm.sub(j)], writes=[st4.sub(j)])
                        k.op(act, lambda h, j=j: h.activation(out=junk[:, 0:128], in_=csm[:, j, 256:384], func=AF.Square, scale=128.0 ** -0.5, accum_out=st4[:, j, 1:2]), reads=[csm.sub(j)], writes=[st4.sub(j)])
                        k.op(act, lambda h, j=j: h.activation(out=junk[:, 0:32], in_=csm[:, j, 384:416], func=AF.Square, accum_out=st4[:, j, 2:3]), reads=[csm.sub(j)], writes=[st4.sub(j)])
                    k.op(act, lambda h: h.activation(out=rs4[:], in_=st4[:, :, 0:2], func=AF.Sqrt, bias=epsb[:, 0:1], scale=1.0), reads=st4.all() + [epsb], writes=[rs4])
                    k.op(dve, lambda h: h.reciprocal(out=rs4[:], in_=rs4[:]), reads=[rs4], writes=[rs4])
                    for j in range(4):
                        k.op(act, lambda h, j=j: h.activation(out=cqn[:, j, 0:256], in_=csm[:, j, 0:256], func=AF.Copy, scale=rs4[:, j, 0:1]), reads=[csm.sub(j), rs4], writes=[cqn.sub(j)])
                        k.op(act, lambda h, j=j: h.activation(out=cqn[:, j, 256:384], in_=csm[:, j, 256:384], func=AF.Copy, scale=rs4[:, j, 1:2]), reads=[csm.sub(j), rs4], writes=[cqn.sub(j)])
                    for kk in range(3):
                        pT = pTs.next()
                        for j in range(4):
                            k.op(pe, lambda h, j=j, kk=kk, pT=pT: h.transpose(out=pT[:, j * 128:(j + 1) * 128], in_=cqn[:, j, kk * 128:(kk + 1) * 128], identity=identb[:]),
                                 reads=[cqn.sub(j), identb], writes=[pT], inc=(j == 3))
                        k.op(act, lambda h, kk=kk, pT=pT: h.activation(out=cT[:, kk, :], in_=pT[:, 0:512], func=AF.Copy), reads=[pT], writes=[cT.sub(kk)])
                    gens = [tile_gen(c, j, tileB[j % 2], CB) for j in range(4)]
                    active = [gens[0], gens[1]]; nxt = 2
                    while active:
                        for g in list(active):
                            try:
                                next(g)
                            except StopIteration:
                                active.remove(g)
                                if nxt < 4:
                                    active.append(gens[nxt]); nxt += 1
                    k.dma(sp, QT[c].t, QTs[0:96, :, :], reads=QTs.all(), writes=[QT[c]])
                    k.dma(sp, KT[c].t, KTs[0:96, :, :], reads=KTs.all(), writes=[KT[c]])
                    k.dma(sp, Vd[c].t, Vs[:], reads=Vs.all(), writes=[Vd[c]])

                for c in range(NCH):
                    p1a_chunk(c, chunkB[c % 2])
                end_phase()
            if stop_after == f"P1a{L}":
                break

            with ExitStack() as st:
                stg = PsPool([k.sb([128, 1024], F32, f"stg{i}", st) for i in range(2)])
                Wbig = k.sb([128, 8, 3072], BF16, "Wbig", st)
                Wrg = k.sb([128, 4, 128], BF16, "Wrg", st); Wig = k.sb([128, 4, 128], BF16, "Wig", st)
                Wupl = k.sb([128, 4, 1024], BF16, "Wupl", st)
                g1 = k.sb([128, 8], F32, "g1", st); cw = k.sb([128, 4, 4], F32, "cw", st); lv = k.sb([128, 4, 4], F32, "lv", st)
                cA = k.sb([128, 4], F32, "cA", st); cA2 = k.sb([128, 4], F32, "cA2", st)
                for b_, nm in ((g1, "g1"), (cw, "convw"), (lv, "lruv")):
                    k.dma(sp, b_[:], I[f"{nm}{L}"], writes=[b_])
                for pc in range(3):
                    load_w(stg, Wbig, lambda kc, pc=pc: Wbig[:, kc, pc * 1024:(pc + 1) * 1024], I[f"w_in{L}"], 8, 1024, g1, c0=416 + pc * 1024)
                sgx = stg.next()
                k.dma(sp, sgx[:, 0:512], I[f"w_rg{L}"].rearrange("p a b -> p (a b)"), writes=[sgx])
                cast(Wrg[:].rearrange("p a b -> p (a b)"), sgx[:, 0:512], None, [sgx], [Wrg])
                sgx2 = stg.next()
                k.dma(sp, sgx2[:, 0:512], I[f"w_ig{L}"].rearrange("p a b -> p (a b)"), writes=[sgx2])
                cast(Wig[:].rearrange("p a b -> p (a b)"), sgx2[:, 0:512], None, [sgx2], [Wig])
                load_w(stg, Wupl, lambda kc: Wupl[:, kc, :], I[f"w_upl{L}"], 4, 1024)
                k.op(act, lambda h: h.activation(out=cA[:], in_=lv[:, :, 3], func=AF.Exp, scale=-1.0), reads=[lv], writes=[cA])
                k.op(dve, lambda h: h.tensor_scalar(out=cA[:], in0=cA[:], scalar1=1.0, scalar2=None, op0=ALU.add), reads=[cA], writes=[cA])
                k.op(act, lambda h: h.activation(out=cA[:], in_=cA[:], func=AF.Ln), reads=[cA], writes=[cA])
                k.op(dve, lambda h: h.tensor_scalar(out=cA2[:], in0=cA[:], scalar1=-16.0, scalar2=None, op0=ALU.mult), reads=[cA], writes=[cA2])
                k.op(dve, lambda h: h.tensor_scalar(out=cA[:], in0=cA[:], scalar1=-8.0, scalar2=None, op0=ALU.mult), reads=[cA], writes=[cA])
                hTs = [k.sb([128, 8, 512], BF16, f"hT{i}", st) for i in range(1)]
                xl = k.sb([128, 4, 515], F32, "xl", st)
                hprev = k.sb([128, 4], F32, "hprev", st)
                tA = [k.sb([128, 512], F32, f"tA{i}", st) for i in range(4)]
                xcb = [k.sb([128, 512], BF16, f"xcb{i}", st) for i in range(4)]
                tR = [k.sb([128, 512], F32, f"tR{i}", st) for i in range(4)]
                tI = [k.sb([128, 512], F32, f"tI{i}", st) for i in range(4)]
                tM = [k.sb([128, 512], F32, f"tM{i}", st) for i in range(4)]
                tH = [k.sb([128, 512], F32, f"tH{i}", st) for i in range(4)]
                tG = [k.sb([128, 512], F32, f"tG{i}", st) for i in range(4)]
                yl = k.sb([128, 4, 512], BF16, "yl", st)
                sga_s = k.sb([128, 8, 512], BF16, "sgas", st)
                sgb_s = k.sb([128, 8, 512], BF16, "sgbs", st)
                mb_s = k.sb([128, 8, 512], BF16, "mbs", st)
                pp = PsPool([k.ps([128, 512], F32, f"pp{i}", st) for i in range(8)])
                for fc in range(4):
                    xl.sub(fc)
                k.op(pool, lambda h: h.memset(xl[:], 0.0), writes=xl.all())
                k.op(pool, lambda h: h.memset(hprev[:], 0.0), writes=[hprev])

                def proj(hT, col):
                    p_ = pp.next()
                    for kc in range(8):
                        k.op(pe, lambda h, kc=kc, p_=p_: h.matmul(p_[:], lhsT=Wbig[:, kc, col:col + 128], rhs=hT[:, kc, :], start=(kc == 0), stop=(kc == 7)),
                             reads=[hT, Wbig], writes=[p_], inc=(kc == 7))
                    return p_

                k.dma(sp, hTs[0][:], H1T[0].t, reads=[H1T[0]], writes=[hTs[0]])
                for c in range(NCH):
                    hT = hTs[0]
                    for fc in range(4):
                        px = proj(hT, fc * 128)
                        k.op(act, lambda h, fc=fc, px=px: h.activation(out=xl[:, fc, 3:515], in_=px[:], func=AF.Copy), reads=[px], writes=[xl.sub(fc)])
                    for fc in range(4):
                        xc = tA[fc]
                        k.op(dve, lambda h, fc=fc, xc=xc: h.tensor_scalar(out=xc[:], in0=xl[:, fc, 0:512], scalar1=cw[:, fc, 0:1], scalar2=lv[:, fc, 0:1], op0=ALU.mult, op1=ALU.add),
                             reads=[xl.sub(fc), cw, lv], writes=[xc])
                        for tp in range(1, 4):
                            k.op(dve, lambda h, fc=fc, tp=tp, xc=xc: h.scalar_tensor_tensor(out=xc[:], in0=xl[:, fc, tp:tp + 512], scalar=cw[:, fc, tp:tp + 1], in1=xc[:], op0=ALU.mult, op1=ALU.add),
                                 reads=[xl.sub(fc), cw, xc], writes=[xc])
                        k.op(act, lambda h, fc=fc, xc=xc: h.activation(out=xcb[fc][:], in_=xc[:], func=AF.Copy), reads=[xc], writes=[xcb[fc]])
                    for fc in range(4):
                        k.op(pool, lambda h, fc=fc: h.tensor_copy(out=xl[:, fc, 0:3], in_=xl[:, fc, 512:515]), reads=[xl.sub(fc)], writes=[xl.sub(fc)])
                    for fc in range(4):
                        pg = proj(hT, 512 + fc * 128)
                        k.op(act, lambda h, fc=fc, pg=pg: h.activation(out=tG[fc][:], in_=pg[:], func=AF.Copy), reads=[pg], writes=[tG[fc]])
                    for dc in range(8):
                        pga = proj(hT, 1024 + dc * 128)
                        k.op(act, lambda h, dc=dc, pga=pga: h.activation(out=sga_s[:, dc, :], in_=pga[:], func=AF.Sigmoid), reads=[pga], writes=[sga_s.sub(dc)])
                    for fc in range(4):
                        pr_ = pp.next(); pi_ = pp.next()
                        k.op(pe, lambda h, fc=fc, pr_=pr_: h.matmul(pr_[:], lhsT=Wrg[:, fc, :], rhs=xcb[fc][:], start=True, stop=True), reads=[Wrg, xcb[fc]], writes=[pr_])
                        k.op(pe, lambda h, fc=fc, pi_=pi_: h.matmul(pi_[:], lhsT=Wig[:, fc, :], rhs=xcb[fc][:], start=True, stop=True), reads=[Wig, xcb[fc]], writes=[pi_])
                        k.op(act, lambda h, fc=fc, pr_=pr_: h.activation(out=tR[fc][:], in_=pr_[:], func=AF.Sigmoid, bias=lv[:, fc, 1:2], scale=1.0), reads=[pr_, lv], writes=[tR[fc]])
                        k.op(act, lambda h, fc=fc, pi_=pi_: h.activation(out=tI[fc][:], in_=pi_[:], func=AF.Sigmoid, bias=lv[:, fc, 2:3], scale=1.0), reads=[pi_, lv], writes=[tI[fc]])
                    pgbs = []
                    for dc in range(8):
                        pgb = proj(hT, 2048 + dc * 128)
                        pgbs.append((dc, pgb))
                    if c + 1 < NCH:
                        k.dma(sp, hT[:], H1T[c + 1].t, reads=[H1T[c + 1]], writes=[hT])
                    for fc in range(4):
                        k.op(act, lambda h, fc=fc: h.activation(out=tM[fc][:], in_=tR[fc][:], func=AF.Exp, scale=cA2[:, fc:fc + 1]), reads=[tR[fc], cA2], writes=[tM[fc]])
                        k.op(act, lambda h, fc=fc: h.activation(out=tR[fc][:], in_=tR[fc][:], func=AF.Exp, scale=cA[:, fc:fc + 1]), reads=[tR[fc], cA], writes=[tR[fc]])
                        k.op(dve, lambda h, fc=fc: h.tensor_scalar(out=tM[fc][:], in0=tM[fc][:], scalar1=-1.0, scalar2=1.0, op0=ALU.mult, op1=ALU.add), reads=[tM[fc]], writes=[tM[fc]])
                        k.op(dve, lambda h, fc=fc: h.tensor_tensor(out=tI[fc][:], in0=tI[fc][:], in1=tA[fc][:], op=ALU.mult), reads=[tI[fc], tA[fc]], writes=[tI[fc]])
                    for fc in range(4):
                        k.op(act, lambda h, fc=fc: h.activation(out=tM[fc][:], in_=tM[fc][:], func=AF.Sqrt), reads=[tM[fc]], writes=[tM[fc]])
                        k.op(dve, lambda h, fc=fc: h.tensor_tensor(out=tI[fc][:], in0=tI[fc][:], in1=tM[fc][:], op=ALU.mult), reads=[tI[fc], tM[fc]], writes=[tI[fc]])
                        k.op(dve, lambda h, fc=fc: h.tensor_tensor_scan(out=tH[fc][:], data0=tR[fc][:], data1=tI[fc][:], initial=hprev[:, fc:fc + 1], op0=ALU.mult, op1=ALU.add),
                             reads=[tR[fc], tI[fc], hprev], writes=[tH[fc]])
                        k.op(dve, lambda h, fc=fc: h.tensor_copy(out=hprev[:, fc:fc + 1], in_=tH[fc][:, 511:512]), reads=[tH[fc]], writes=[hprev])
                    for fc in range(4):
                        k.op(act, lambda h, fc=fc: h.activation(out=tG[fc][:], in_=tG[fc][:], func=AF.Gelu_apprx_tanh), reads=[tG[fc]], writes=[tG[fc]])
                        k.op(dve, lambda h, fc=fc: h.tensor_tensor(out=yl[:, fc, :], in0=tH[fc][:], in1=tG[fc][:], op=ALU.mult), reads=[tH[fc], tG[fc]], writes=[yl.sub(fc)])
                    for (dc, pgb) in pgbs:
                        k.op(act, lambda h, dc=dc, pgb=pgb: h.activation(out=sgb_s[:, dc, :], in_=pgb[:], func=AF.Sigmoid), reads=[pgb], writes=[sgb_s.sub(dc)])
                    for dc in range(8):
                        pu = pp.next()
                        for fc in range(4):
                            k.op(pe, lambda h, fc=fc, dc=dc, pu=pu: h.matmul(pu[:], lhsT=Wupl[:, fc, dc * 128:(dc + 1) * 128], rhs=yl[:, fc, :], start=(fc == 0), stop=(fc == 3)),
                                 reads=[Wupl, yl.sub(fc)], writes=[pu], inc=(fc == 3))
                        k.op(dve, lambda h, dc=dc, pu=pu: h.tensor_tensor(out=mb_s[:, dc, :], in0=pu[:], in1=sgb_s[:, dc, :], op=ALU.mult), reads=[pu, sgb_s.sub(dc)], writes=[mb_s.sub(dc)])
                    k.dma(sp, SGA[c].t, sga_s[:], reads=sga_s.all(), writes=[SGA[c]])
                    k.dma(sp, MB[c].t, mb_s[:], reads=mb_s.all(), writes=[MB[c]])
                end_phase()
            if stop_after == f"P1b{L}":
                break

            st23 = ExitStack()
            OT = k.sb([128, 4, S], BF16, f"OT{L}", st23)
            Wupa = k.sb([128, 4, 1024], BF16, "Wupa", st23); Wo = k.sb([128, 8, 1024], BF16, "Wo", st23)
            with ExitStack() as st:
                stgw = PsPool([k.sb([128, 1024], F32, f"stgw{i}", st) for i in range(2)])
                Vp = [k.sb([128, NT, 192], BF16, f"Vp{i}", st) for i in range(2)]
                KTh = [k.sb([128, S], BF16, f"KTh{i}", st) for i in range(2)]
                QTh = [k.sb([128, S], BF16, f"QTh{i}", st) for i in range(2)]
                pts = PsPool([k.sb([128, 512], BF16, f"pt{i}", st) for i in range(6)])
                rcs = PsPool([k.sb([128, 512], F32, f"rc{i}", st) for i in range(2)])
                pss = PsPool([k.ps([128, 512], F32, f"ps{i}", st) for i in range(5)])
                pos_ = PsPool([k.ps([128, 512], F32, f"po{i}", st) for i in range(3)])

                def load_head(hd):
                    i2 = hd % 2
                    if hd % 2 == 0:
                        pr = hd // 2
                        k.dma(sp, Vp[pr % 2][:], V_t[:, :, pr * 192:(pr + 1) * 192], reads=Vd, writes=[Vp[pr % 2]])
                    k.dma(sp, KTh[i2][0:96, :], KT_t[:, hd, :], reads=KT, writes=[KTh[i2]])
                    k.dma(sp, QTh[i2][0:96, :], QT_t[:, hd, :], reads=QT, writes=[QTh[i2]])
                load_head(0)

                def wprefetch():
                    for (W_, src, nk) in ((Wupa, I[f"w_upa{L}"], 4), (Wo, I[f"w_o{L}"], 8)):
                        for kc in range(nk):
                            sg = stgw.next()
                            k.dma(sp, sg[:, 0:1024], src[kc * 128:(kc + 1) * 128, 0:1024], writes=[sg])
                            yield
                            cast(W_[:, kc, :], sg[:, 0:1024], None, [sg], [W_], dve)
                            yield
                wgen = wprefetch(); wcnt = [0]
                LA = 3
                items = []
                for hd in range(8):
                    for c in range(NCH):
                        nk = 4 * c + 4
                        for kt in range(nk):
                            items.append((hd, c, kt, nk))
                state = {}

                def issue_S(it):
                    hd, c, kt, nk = it
                    if c == 0 and kt == 0 and hd + 1 < 8:
                        load_head(hd + 1)
                    if hd >= 2:
                        wcnt[0] += 1
                        if wcnt[0] % 3 == 0:
                            next(wgen, None)
                    Kh = KTh[hd % 2]; Qh = QTh[hd % 2]
                    dd = kt - 4 * c
                    q0 = dd * 128 if dd > 0 else 0
                    ps_ = pss.next(); pt = pts.next()
                    k.op(pe, lambda h: h.matmul(ps_[:, q0:512], lhsT=Kh[0:96, kt * 128:(kt + 1) * 128], rhs=Qh[0:96, c * 512 + q0:(c + 1) * 512], start=True, stop=True),
                         reads=[Kh, Qh], writes=[ps_])
                    k.op(act, lambda h: h.activation(out=pt[:, q0:512], in_=ps_[:, q0:512], func=AF.Exp), reads=[ps_], writes=[pt])
                    if dd >= 0:
                        k.op(dve, lambda h: h.tensor_tensor(out=pt[:, q0:q0 + 128], in0=pt[:, q0:q0 + 128], in1=trib[:], op=ALU.mult), reads=[pt, trib], writes=[pt])
                    state[it] = (pt, q0)

                def issue_PV(it):
                    hd, c, kt, nk = it
                    pt, q0 = state.pop(it)
                    pr = hd // 2; odd = hd % 2
                    Vh = Vp[pr % 2]; voff = 64 if odd else 0
                    if kt == 0:
                        state[("po", hd, c)] = pos_.next()
                    po = state[("po", hd, c)]
                    k.op(pe, lambda h: h.matmul(po[:, q0:512], lhsT=Vh[:, kt, voff:voff + 128], rhs=pt[:, q0:512], start=(kt == 0), stop=(kt == nk - 1)),
                         reads=[Vh, pt], writes=[po], inc=(kt == nk - 1))
                    if kt == nk - 1:
                        del state[("po", hd, c)]
                        rc = rcs.next()
                        if not odd:
                            k.op(dve, lambda h: h.reciprocal(out=rc[64:128, :], in_=po[64:128, :]), reads=[po], writes=[rc])
                            k.op(dve, lambda h: h.tensor_tensor(out=OT[0:64, pr, c * 512:(c + 1) * 512], in0=po[0:64, :], in1=rc[64:128, :], op=ALU.mult), reads=[po, rc], writes=[OT])
                        else:
                            k.op(dve, lambda h: h.reciprocal(out=rc[0:64, :], in_=po[0:64, :]), reads=[po], writes=[rc])
                            k.op(dve, lambda h: h.tensor_tensor(out=OT[64:128, pr, c * 512:(c + 1) * 512], in0=po[64:128, :], in1=rc[0:64, :], op=ALU.mult), reads=[po, rc], writes=[OT])

                for i in range(len(items) + LA):
                    if i < len(items):
                        issue_S(items[i])
                    if i - LA >= 0:
                        issue_PV(items[i - LA])
                for _ in wgen:
                    pass
                if debug and "ot" in debug and L == 0:
                    k.dma(sp, dbg_out["ot"], OT[:], reads=[OT])
                end_phase()
            if stop_after == f"P2{L}":
                st23.close()
                break

            with ExitStack() as st:
                xins = [k.sb([128, 4, 1024], F32, f"xin{i}", st) for i in range(2)]
                for xb_ in xins:
                    for j in range(4):
                        xb_.sub(j)
                sgl = [k.sb([128, 8, 512], BF16, f"sgl{i}", st) for i in range(2)]
                mbl = [k.sb([128, 8, 512], BF16, f"mbl{i}", st) for i in range(2)]
                mg = k.sb([128, 8, 512], BF16, "mg", st)
                tmpf = [k.sb([128, 512], F32, f"tmpf{i}", st) for i in range(2)]
                hb = k.sb([128, 4, 1024], BF16, "hb", st); hT = k.sb([128, 8, 512], BF16, "hT", st)
                junk = k.sb([128, 1024], BF16, "junk", st)
                ssq = k.sb([128, 4], F32, "ssq", st); rstd = k.sb([128, 4], F32, "rstd", st)
                pTs = PsPool([k.ps([128, 1024], BF16, f"pT{i}", st) for i in range(2)])
                pp = PsPool([k.ps([128, 512], F32, f"pp{i}", st) for i in range(6 if L == 0 else 3)])
                if L == 1:
                    pTfs = PsPool([k.ps([128, 512], F32, f"pTf{i}", st) for i in range(2)])
                    plg = k.ps([128, 512], F32, "plg", st)
                    xTf = k.sb([128, 8, 128], F32, "xTf", st)
                    rtw = k.sb([128, 8, NE], F32, "rtw", st); g2r = k.sb([128, 8], F32, "g2r", st)
                    k.dma(sp, rtw[:], I["rtw"], writes=[rtw]); k.dma(sp, g2r[:], I["g21"], writes=[g2r])
                    k.op(dve, lambda h: h.tensor_tensor(out=rtw[:], in0=rtw[:], in1=bc(g2r[:], [128, 8, NE], 2), op=ALU.mult), reads=[rtw, g2r], writes=[rtw])
                    lg = k.sb([128, 4, NE], F32, "lg", st); m1 = k.sb([128, 4], F32, "m1", st); m2 = k.sb([128, 4], F32, "m2", st)
                    msk = k.sb([128, 4, NE], F32, "msk", st); lg2 = k.sb([128, 4, NE], F32, "lg2", st); ex = k.sb([128, 4, NE], F32, "ex", st)
                    den = k.sb([128, 4], F32, "den", st)

                def route(c, xin):
                    for j in range(4):
                        for half in range(2):
                            pTf = pTfs.next()
                            for q4 in range(4):
                                kc = half * 4 + q4
                                k.op(pe, lambda h, j=j, kc=kc, q4=q4, pTf=pTf: h.transpose(out=pTf[:, q4 * 128:(q4 + 1) * 128], in_=xin[:, j, kc * 128:(kc + 1) * 128], identity=identf[:]),
                                     reads=[xin.sub(j), identf], writes=[pTf], inc=(q4 == 3))
                            k.op(act, lambda h, half=half, pTf=pTf: h.activation(out=xTf[:, half * 4:(half + 1) * 4, :].rearrange("p a b -> p (a b)"), in_=pTf[:], func=AF.Copy), reads=[pTf], writes=[xTf])
                        for kc in range(8):
                            k.op(pe, lambda h, kc=kc: h.matmul(plg[:, 0:NE], lhsT=xTf[:, kc, :], rhs=rtw[:, kc, :], start=(kc == 0), stop=(kc == 7)), reads=[xTf, rtw], writes=[plg], inc=(kc == 7))
                        k.op(dve, lambda h, j=j: h.tensor_scalar(out=lg[:, j, :], in0=plg[:, 0:NE], scalar1=rstd[:, j:j + 1], scalar2=None, op0=ALU.mult), reads=[plg, rstd], writes=[lg])
                    sh = [128, 4, NE]
                    k.op(dve, lambda h: h.tensor_reduce(out=m1[:], in_=lg[:], axis=AX.X, op=ALU.max), reads=[lg], writes=[m1])
                    k.op(dve, lambda h: h.tensor_tensor(out=msk[:], in0=lg[:], in1=bc(m1[:], sh, 2), op=ALU.is_equal), reads=[lg, m1], writes=[msk])
                    k.op(dve, lambda h: h.scalar_tensor_tensor(out=lg2[:], in0=msk[:], scalar=-1e30, in1=lg[:], op0=ALU.mult, op1=ALU.add), reads=[msk, lg], writes=[lg2])
                    k.op(dve, lambda h: h.tensor_reduce(out=m2[:], in_=lg2[:], axis=AX.X, op=ALU.max), reads=[lg2], writes=[m2])
                    k.op(dve, lambda h: h.tensor_tensor(out=msk[:], in0=lg[:], in1=bc(m2[:], sh, 2), op=ALU.is_ge), reads=[lg, m2], writes=[msk])
                    k.op(dve, lambda h: h.tensor_tensor(out=ex[:], in0=lg[:], in1=bc(m1[:], sh, 2), op=ALU.subtract), reads=[lg, m1], writes=[ex])
                    k.op(dve, lambda h: h.tensor_scalar(out=ex[:], in0=ex[:], scalar1=-80.0, scalar2=None, op0=ALU.max), reads=[ex], writes=[ex])
                    k.op(act, lambda h: h.activation(out=ex[:], in_=ex[:], func=AF.Exp), reads=[ex], writes=[ex])
                    k.op(dve, lambda h: h.tensor_tensor(out=ex[:], in0=ex[:], in1=msk[:], op=ALU.mult), reads=[ex, msk], writes=[ex])
                    k.op(dve, lambda h: h.tensor_reduce(out=den[:], in_=ex[:], axis=AX.X, op=ALU.add), reads=[ex], writes=[den])
                    k.op(dve, lambda h: h.reciprocal(out=den[:], in_=den[:]), reads=[den], writes=[den])
                    k.op(dve, lambda h: h.tensor_tensor(out=wr[:, 4 * c:4 * c + 4, :], in0=ex[:], in1=bc(den[:], sh, 2), op=ALU.mult), reads=[ex, den], writes=[wr])

                def loads(c):
                    i2 = c % 2
                    k.dma(sp, xins[i2][:], Xc[c].t, reads=[Xc[c]], writes=xins[i2].all())
                    k.dma(sp, sgl[i2][:], SGA[c].t, reads=[SGA[c]], writes=[sgl[i2]])
                    k.dma(sp, mbl[i2][:], MB[c].t, reads=[MB[c]], writes=[mbl[i2]])
                def ua_part(c):
                    sg_ = sgl[c % 2]; mb_ = mbl[c % 2]
                    for dc in range(8):
                        pu = pp.next(); tf = tmpf[dc % 2]
                        for pr in range(4):
                            k.op(pe, lambda h, pr=pr, dc=dc, pu=pu: h.matmul(pu[:], lhsT=Wupa[:, pr, dc * 128:(dc + 1) * 128], rhs=OT[:, pr, c * 512:(c + 1) * 512], start=(pr == 0), stop=(pr == 3)),
                                 reads=[Wupa, OT], writes=[pu], inc=(pr == 3))
                        k.op(dve, lambda h, dc=dc, pu=pu, tf=tf: h.tensor_tensor(out=tf[:], in0=pu[:], in1=sg_[:, dc, :], op=ALU.mult), reads=[pu, sg_], writes=[tf])
                        k.op(dve, lambda h, dc=dc, tf=tf: h.tensor_tensor(out=mg[:, dc, :], in0=tf[:], in1=mb_[:, dc, :], op=ALU.add), reads=[tf, mb_], writes=[mg.sub(dc)])

                def wo_part(c):
                    xin = xins[c % 2]
                    for j in range(4):
                        for nh in range(2):
                            py = pp.next()
                            for dc in range(8):
                                k.op(pe, lambda h, j=j, nh=nh, dc=dc, py=py: h.matmul(py[:], lhsT=mg[:, dc, j * 128:(j + 1) * 128], rhs=Wo[:, dc, nh * 512:(nh + 1) * 512], start=(dc == 0), stop=(dc == 7)),
                                     reads=[mg.sub(dc), Wo], writes=[py], inc=(dc == 7))
                            k.op(dve, lambda h, j=j, nh=nh, py=py: h.tensor_tensor(out=xin[:, j, nh * 512:(nh + 1) * 512], in0=py[:], in1=xin[:, j, nh * 512:(nh + 1) * 512], op=ALU.add), reads=[py, xin.sub(j)], writes=[xin.sub(j)])

                def tail_part(c):
                    xin = xins[c % 2]
                    k.dma(sp, Y_c[c].t, xin[:], reads=xin.all(), writes=[Y_c[c]])
                    rms_T(xin, ssq, rstd, hb, hT, pTs, junk)
                    k.dma(sp, H2T[c].t, hT[:], reads=hT.all(), writes=[H2T[c]])
                    if L == 1:
                        route(c, xin)

                loads(0)
                ua_part(0)
                for c in range(NCH):
                    if c + 1 < NCH:
                        loads(c + 1)
                    wo_part(c)
                    if c + 1 < NCH:
                        ua_part(c + 1)
                    tail_part(c)
                end_phase()
            st23.close()
            if stop_after == f"P3{L}":
                break

            if L == 0:
                units = [dict(wg=I["wg"], wu=I["wu"], wd=I["wd"], f0=f0, nf=nf, e=None) for (f0, nf) in ((0, 8), (8, 7), (15, 7))]
            else:
                units = [dict(wg=I["mwg"][e], wu=I["mwu"][e], wd=I["mwd"][e], f0=q4 * 7, nf=7, e=e) for e in range(NE) for q4 in range(4)]
            NF = 8
            with ExitStack() as st:
                stg = PsPool([k.sb([128, 1024], F32, f"stg{i}", st) for i in range(4)])
                g2 = k.sb([128, 8], F32, "g2", st)
                k.dma(sp, g2[:], I[f"g2{L}"], writes=[g2])
                Wsets = []
                for i in range(2):
                    Wg_t = k.sb([128, 8, NF * 128], BF16, f"Wg{i}", st); Wu_t = k.sb([128, 8, NF * 128], BF16, f"Wu{i}", st)
                    Wd_t = k.sb([128, NF, 1024], BF16, f"Wd{i}", st)
                    Wsets.append((Wg_t, Wu_t, Wd_t))
                xin = k.sb([128, 4, 1024], F32, "xin", st)
                hTs = [k.sb([128, 8, 512], BF16, f"hT{i}", st) for i in range(2)]
                acts = [k.sb([128, NF, 512], BF16, f"actb{i}", st) for i in range(2)]
                sgt = [k.sb([128, 512], F32, f"sgt{i}", st) for i in range(2)]
                pp = PsPool([k.ps([128, 512], F32, f"pp{i}", st) for i in range(8)])

                def loads(u, c):
                    i2 = (u * NCH + c) % 2
                    k.dma(sp, hTs[i2][:], H2T[c].t, reads=[H2T[c]], writes=[hTs[i2]])

                def loadx(c):
                    k.dma(sp, xin[:], Y_c[c].t, reads=[Y_c[c]], writes=[xin])

                def unit_pieces(u):
                    un = units[u]; nf = un["nf"]; c0 = un["f0"] * 128
                    Wg_t, Wu_t, Wd_t = Wsets[u % 2]
                    for (src, Wt) in ((un["wg"], Wg_t), (un["wu"], Wu_t)):
                        for kc in range(8):
                            sg = stg.next()
                            k.dma(pool, sg[:, 0:nf * 128], src[kc * 128:(kc + 1) * 128, c0:c0 + nf * 128], writes=[sg])
                            cast(Wt[:, kc, 0:nf * 128], sg[:, 0:nf * 128], g2[:, kc:kc + 1], [sg, g2], [Wt])
                            yield
                    for f in range(nf):
                        sg = stg.next()
                        k.dma(pool, sg[:, 0:1024], un["wd"][c0 + f * 128:c0 + (f + 1) * 128, :], writes=[sg])
                        cast(Wd_t[:, f, :], sg[:, 0:1024], None, [sg], [Wd_t])
                        yield

                for _ in unit_pieces(0):
                    pass
                loads(0, 0)
                loadx(0)
                for u, un in enumerate(units):
                    nf = un["nf"]; e = un["e"]
                    Wg_t, Wu_t, Wd_t = Wsets[u % 2]
                    nxt = unit_pieces(u + 1) if u + 1 < len(units) else iter(())
                    for c in range(NCH):
                        i2 = (u * NCH + c) % 2
                        nu, ncn = (u, c + 1) if c + 1 < NCH else (u + 1, 0)
                        if nu < len(units):
                            loads(nu, ncn)
                        hT = hTs[i2]; ab = acts[i2]
                        for f in range(nf):
                            pg = pp.next(); pu = pp.next(); sg_ = sgt[f % 2]
                            for (p_, Wt) in ((pg, Wg_t), (pu, Wu_t)):
                                for kc in range(8):
                                    k.op(pe, lambda h, kc=kc, f=f, p_=p_, Wt=Wt, hT=hT: h.matmul(p_[:], lhsT=Wt[:, kc, f * 128:(f + 1) * 128], rhs=hT[:, kc, :], start=(kc == 0), stop=(kc == 7)),
                                         reads=[Wt, hT], writes=[p_], inc=(kc == 7))
                            k.op(act, lambda h, pg=pg, sg_=sg_: h.activation(out=sg_[:], in_=pg[:], func=AF.Silu), reads=[pg], writes=[sg_])
                            k.op(dve, lambda h, f=f, pu=pu, sg_=sg_, ab=ab: h.tensor_tensor(out=ab[:, f, :], in0=pu[:], in1=sg_[:], op=ALU.mult), reads=[pu, sg_], writes=[ab.sub(f)])
                            if f % 2 == 1:
                                next(nxt, None)
                        for j in range(4):
                            for nh in range(2):
                                py = pp.next()
                                for f in range(nf):
                                    k.op(pe, lambda h, j=j, nh=nh, f=f, py=py, ab=ab, nf=nf, Wd_t=Wd_t: h.matmul(py[:], lhsT=ab[:, f, j * 128:(j + 1) * 128], rhs=Wd_t[:, f, nh * 512:(nh + 1) * 512], start=(f == 0), stop=(f == nf - 1)),
                                         reads=[ab.sub(f), Wd_t], writes=[py], inc=(f == nf - 1))
                                xs_ = xin[:, j, nh * 512:(nh + 1) * 512]
                                if e is None:
                                    k.op(dve, lambda h, py=py, xs_=xs_: h.tensor_tensor(out=xs_, in0=py[:], in1=xs_, op=ALU.add), reads=[py, xin], writes=[xin])
                                else:
                                    t = 4 * c + j
                                    k.op(dve, lambda h, py=py, xs_=xs_, t=t, e=e: h.scalar_tensor_tensor(out=xs_, in0=py[:], scalar=wr[:, t, e:e + 1], in1=xs_, op0=ALU.mult, op1=ALU.add),
                                         reads=[py, xin, wr], writes=[xin])
                        k.dma(sp, Y_c[c].t, xin[:], reads=[xin], writes=[Y_c[c]])
                        if nu < len(units):
                            loadx(ncn)
                    for _ in nxt:
                        pass
                end_phase()
            if stop_after == f"P5{L}":
                break

        if debug:
            srcs = dict(h1t=H1T_t, h2t=H2T_t, qt=QT_t, kt=KT_t, vv=V_t, sga=SGA_t, mb=MB_t)
            with ExitStack() as st:
                for nm in debug:
                    if nm in srcs:
                        k.dma(sp, dbg_out[nm], srcs[nm], reads=H1T + H2T + QT + KT + Vd + SGA + MB)
                    elif nm == "wr":
                        k.dma(sp, dbg_out[nm], wr[:], reads=[wr])
                    elif nm == "rope":
                        k.dma(sp, dbg_out[nm][:, 0], cost[:], reads=[cost]); k.dma(sp, dbg_out[nm][:, 1], sint[:], reads=[sint])
                end_phase()
    return nc


def host_inputs(inp):
    f32 = np.float32
    A = lambda a: np.ascontiguousarray(np.asarray(a))
    com = {}
    com["ident"] = np.eye(128, dtype=f32)
    com["tri"] = np.triu(np.ones((128, 128), dtype=f32))
    invf = (np.float32(10000.0) ** (-np.arange(0, 32, 2, dtype=f32) / np.float32(32))).astype(f32)
    com["invf"] = A(np.broadcast_to(invf[None, :], (128, 16)))
    pk = lambda v, n: A(np.asarray(v, dtype=f32).reshape(n, 128).T)
    for L in range(2):
        com[f"w_in{L}"] = A(inp["w_in"][L]); com[f"g1{L}"] = pk(inp["norm1_g"][L], 8)
        com[f"w_uq{L}"] = A(inp["w_uq"][L]); com[f"qng{L}"] = pk(inp["q_norm_g"][L], 2)
        com[f"w_ukv{L}"] = A(inp["w_ukv"][L]); com[f"kvng{L}"] = pk(inp["kv_norm_g"][L], 1)
        com[f"qkg{L}"] = A(np.broadcast_to(np.concatenate([inp["qk_q_g"][L], inp["qk_k_g"][L]])[None, :], (128, 192)))
        com[f"w_upa{L}"] = A(inp["w_up_attn"][L])
        com[f"convw{L}"] = A(np.asarray(inp["conv_w"][L]).reshape(4, 4, 128).transpose(2, 1, 0))
        lv = np.stack([inp["conv_b"][L], inp["b_rg"][L], inp["b_ig"][L], inp["lru_lambda"][L]], axis=-1)
        com[f"lruv{L}"] = A(lv.reshape(4, 128, 4).transpose(1, 0, 2))
        for nm, key in ((f"w_rg{L}", "w_rg"), (f"w_ig{L}", "w_ig")):
            w = np.asarray(inp[key][L])
            bd = np.zeros((128, 4, 128), dtype=f32)
            for fc in range(4):
                bd[0:64, fc, 0:64] = w[2 * fc]; bd[64:128, fc, 64:128] = w[2 * fc + 1]
            com[nm] = bd
        com[f"w_upl{L}"] = A(inp["w_up_lru"][L]); com[f"w_o{L}"] = A(inp["w_o"][L]); com[f"g2{L}"] = pk(inp["norm2_g"][L], 8)
    com["wg"] = A(inp["ffn_w_gate"][0]); com["wu"] = A(inp["ffn_w_up"][0]); com["wd"] = A(inp["ffn_w_down"][0])
    com["mwg"] = A(inp["moe_w_gate"][0]); com["mwu"] = A(inp["moe_w_up"][0]); com["mwd"] = A(inp["moe_w_down"][0])
    com["rtw"] = A(np.asarray(inp["moe_router"][0], dtype=f32).reshape(8, 128, NE).transpose(1, 0, 2))
    maps = []
    for b in range(8):
        m = dict(com)
        m["x"] = A(inp["x"][b]); m["pos"] = A(np.asarray(inp["positions"][b], dtype=np.int32).reshape(NT, 128).T)
        maps.append(m)
    return maps


def kernel(**inputs):
    nc = build()
    maps = host_inputs(inputs)
    res = run_bass_kernel_spmd(nc, maps, core_ids=list(range(8)))
    return np.stack([np.asarray(r["y"], dtype=np.float32).reshape(S, D) for r in res.results], axis=0)
```

```python
import numpy as np
from contextlib import ExitStack
import concourse.bass as bass
import concourse.mybir as mybir
from concourse.bass_utils import run_bass_kernel_spmd

F32 = mybir.dt.float32
BF16 = mybir.dt.bfloat16
I32 = mybir.dt.int32
ALU = mybir.AluOpType
AF = mybir.ActivationFunctionType
AX = mybir.AxisListType


class Eng:
    def __init__(self, name, handle, sem, same_sync):
        self.name = name
        self.h = handle
        self.sem = sem
        self.count = 0
        self.seen = {}
        self.ops = []
        self.same_sync = same_sync
        self.dma_sems = []
        self.dma_tgt = []
        self.dma_k = 0


class Buf:
    def __init__(self, t, name=""):
        self.t = t
        self.name = name
        self.w = None
        self.r = {}

    def __getitem__(self, idx):
        return self.t[idx]

    def sub(self, key):
        if not hasattr(self, "_subs"):
            self._subs = {}
        if key not in self._subs:
            self._subs[key] = Buf(self.t, f"{self.name}.{key}")
        return self._subs[key]

    def all(self):
        return list(getattr(self, "_subs", {}).values())


class K:
    def __init__(self, nc, es, n_dma_sems=8):
        self.nc = nc
        self.es = es
        mk = lambda nm: es.enter_context(nc.semaphore(nm))
        self.pe = Eng("pe", nc.tensor, mk("s_pe"), False)
        self.dve = Eng("dve", nc.vector, mk("s_dve"), True)
        self.act = Eng("act", nc.scalar, mk("s_act"), True)
        self.pool = Eng("pool", nc.gpsimd, mk("s_pool"), True)
        self.sp = Eng("sp", nc.sync, mk("s_sp"), False)
        self.engs = [self.pe, self.dve, self.act, self.pool, self.sp]
        for q in (self.sp, self.act, self.pool):
            for i in range(n_dma_sems):
                q.dma_sems.append(mk(f"d_{q.name}{i}"))
                q.dma_tgt.append(0)
        self.nbuf = 0

    def sb(self, shape, dtype, name=None, stack=None):
        self.nbuf += 1
        name = name or f"sb{self.nbuf}"
        t = (stack or self.es).enter_context(self.nc.sbuf_tensor(f"{name}_{self.nbuf}", list(shape), dtype))
        return Buf(t, name)

    def ps(self, shape, dtype, name=None, stack=None):
        self.nbuf += 1
        name = name or f"ps{self.nbuf}"
        t = (stack or self.es).enter_context(self.nc.psum_tensor(f"{name}_{self.nbuf}", list(shape), dtype))
        return Buf(t, name)

    def dram(self, name, shape, dtype, kind="Internal"):
        t = self.nc.dram_tensor(name, list(shape), dtype, kind=kind)
        return Buf(t.ap(), name)

    def _collect(self, eng, reads, writes):
        need = {}

        def add(ev):
            if ev is None:
                return
            sem, val, key = ev
            if key == id(eng.sem) and not eng.same_sync:
                return
            if eng.seen.get(key, 0) >= val:
                return
            if key not in need or need[key][1] < val:
                need[key] = ev

        for b in reads:
            add(b.w)
        for b in writes:
            add(b.w)
            for ev in b.r.values():
                add(ev)
        for key, (sem, val, _) in need.items():
            eng.ops.append(("wait", sem, val))
            eng.seen[key] = val

    def _mark(self, ev, reads, writes):
        key = ev[2]
        for b in reads:
            old = b.r.get(key)
            if old is None or old[1] < ev[1]:
                b.r[key] = ev
        for b in writes:
            b.w = ev
            b.r = {}

    def op(self, eng, fn, reads=(), writes=(), inc=True):
        self._collect(eng, reads, writes)
        eng.ops.append(("op", fn, inc))
        if inc:
            eng.count += 1
            ev = (eng.sem, eng.count, id(eng.sem))
        else:
            ev = (eng.sem, eng.count + 1, id(eng.sem))
        self._mark(ev, reads, writes)

    def dma(self, q, out, in_, reads=(), writes=()):
        self._collect(q, reads, writes)
        i = q.dma_k % len(q.dma_sems)
        q.dma_k += 1
        sem = q.dma_sems[i]
        prev = q.dma_tgt[i]
        if prev > 0 and q.seen.get(id(sem), 0) < prev:
            q.ops.append(("wait", sem, prev))
            q.seen[id(sem)] = prev
        tgt = prev + 16
        q.dma_tgt[i] = tgt
        q.ops.append(("dma", out, in_, sem))
        ev = (sem, tgt, id(sem))
        self._mark(ev, reads, writes)

    def wait_all_dma(self, eng):
        for q in (self.sp, self.act, self.pool):
            for sem, tgt in zip(q.dma_sems, q.dma_tgt):
                if tgt > 0 and eng.seen.get(id(sem), 0) < tgt:
                    eng.ops.append(("wait", sem, tgt))
                    eng.seen[id(sem)] = tgt

    def simulate(self):
        names = {}
        for e in self.engs:
            names[id(e.sem)] = e.name
            for i, d in enumerate(e.dma_sems):
                names[id(d)] = f"dma_{e.name}{i}"
        if not hasattr(self, "_simval"):
            self._simval = {}
        val = self._simval
        pos = {e.name: 0 for e in self.engs}
        progress = True
        while progress:
            progress = False
            for e in self.engs:
                while pos[e.name] < len(e.ops):
                    o = e.ops[pos[e.name]]
                    if o[0] == "wait":
                        if val.get(id(o[1]), 0) >= o[2]:
                            pos[e.name] += 1; progress = True
                        else:
                            break
                    elif o[0] == "op":
                        if o[2]:
                            val[id(e.sem)] = val.get(id(e.sem), 0) + 1
                        pos[e.name] += 1; progress = True
                    else:
                        val[id(o[3])] = val.get(id(o[3]), 0) + 16
                        pos[e.name] += 1; progress = True
        stuck = [e for e in self.engs if pos[e.name] < len(e.ops)]
        if stuck:
            msg = []
            for e in stuck:
                o = e.ops[pos[e.name]]
                nxt = next((x for x in e.ops[pos[e.name]:] if x[0] != "wait"), None)
                line = nxt[1].__code__.co_firstlineno if nxt is not None and nxt[0] == "op" else "dma"
                msg.append(f"{e.name} blocked at op#{pos[e.name]} waiting {names.get(id(o[1]))}>={o[2]} (now {val.get(id(o[1]), 0)}), next op from source line {line}")
            raise RuntimeError("DEADLOCK in recorded program:\n" + "\n".join(msg))

    def emit(self):
        nc = self.nc
        with nc.Block() as block:
            def replay(eng):
                def body(h):
                    for o in eng.ops:
                        if o[0] == "wait":
                            h.wait_ge(o[1], o[2])
                        elif o[0] == "op":
                            ins = o[1](h)
                            if o[2]:
                                ins.then_inc(eng.sem, 1)
                        else:
                            h.dma_start(out=o[1], in_=o[2]).then_inc(o[3], 16)
                    eng.ops = []
                return body
            block.tensor(replay(self.pe))
            block.vector(replay(self.dve))
            block.scalar(replay(self.act))
            block.gpsimd(replay(self.pool))
            block.sync(replay(self.sp))


S = 4096; D = 1024; NT = 32; NCH = 8
EPS = 1e-6
TWO_PI = 6.283185307179586
C1 = 6.28125
C2 = TWO_PI - C1
PI_SAFE = 3.1415925
MAGIC = 12582912.0
D_FF = 2816; D_FFE = 3584; NE = 8


class PsPool:
    def __init__(self, bufs):
        self.bufs = bufs; self.i = 0
    def next(self):
        b = self.bufs[self.i % len(self.bufs)]; self.i += 1
        return b


def bc(ap, shape, axis):
    return ap.unsqueeze(axis).to_broadcast(list(shape))


def build(debug=None, stop_after=None):
    nc = bass.Bass("TRN2", target_bir_lowering=False)
    def din(name, shape, dt=F32):
        return nc.dram_tensor(name, list(shape), dt, kind="ExternalInput").ap()
    I = {}
    I["x"] = din("x", [S, D]); I["pos"] = din("pos", [128, NT], I32)
    I["ident"] = din("ident", [128, 128]); I["tri"] = din("tri", [128, 128]); I["invf"] = din("invf", [128, 16])
    for L in range(2):
        I[f"w_in{L}"] = din(f"w_in{L}", [1024, 3488]); I[f"g1{L}"] = din(f"g1{L}", [128, 8])
        I[f"w_uq{L}"] = din(f"w_uq{L}", [256, 768]); I[f"qng{L}"] = din(f"qng{L}", [128, 2])
        I[f"w_ukv{L}"] = din(f"w_ukv{L}", [128, 1024]); I[f"kvng{L}"] = din(f"kvng{L}", [128, 1])
        I[f"qkg{L}"] = din(f"qkg{L}", [128, 192]); I[f"w_upa{L}"] = din(f"w_upa{L}", [512, 1024])
        I[f"convw{L}"] = din(f"convw{L}", [128, 4, 4]); I[f"lruv{L}"] = din(f"lruv{L}", [128, 4, 4])
        I[f"w_rg{L}"] = din(f"w_rg{L}", [128, 4, 128]); I[f"w_ig{L}"] = din(f"w_ig{L}", [128, 4, 128])
        I[f"w_upl{L}"] = din(f"w_upl{L}", [512, 1024]); I[f"w_o{L}"] = din(f"w_o{L}", [1024, 1024])
        I[f"g2{L}"] = din(f"g2{L}", [128, 8])
    I["wg"] = din("wg", [1024, D_FF]); I["wu"] = din("wu", [1024, D_FF]); I["wd"] = din("wd", [D_FF, 1024])
    I["mwg"] = din("mwg", [NE, 1024, D_FFE]); I["mwu"] = din("mwu", [NE, 1024, D_FFE]); I["mwd"] = din("mwd", [NE, D_FFE, 1024])
    I["rtw"] = din("rtw", [128, 8, NE])
    y_out = nc.dram_tensor("y", [S, D], F32, kind="ExternalOutput").ap()
    dbg_out = {}
    if debug:
        for nm, (shape, dt) in debug.items():
            dbg_out[nm] = nc.dram_tensor("dbg_" + nm, list(shape), dt, kind="ExternalOutput").ap()

    with ExitStack() as es:
        k = K(nc, es)
        pe, dve, act, pool, sp = k.pe, k.dve, k.act, k.pool, k.sp

        def chunked(name, shape, dt, view_fn):
            t = nc.dram_tensor(name, list(shape), dt, kind="Internal").ap()
            return t, [Buf(view_fn(t, c), f"{name}{c}") for c in range(NCH)]
        tokview = lambda t, c: t.rearrange("(j p) d -> p j d", p=128)[:, 4 * c:4 * c + 4, :]
        Xin_c = [Buf(tokview(I["x"], c), f"x{c}") for c in range(NCH)]
        Y_c = [Buf(tokview(y_out, c), f"y{c}") for c in range(NCH)]
        fmview = lambda t, c: t[:, :, c * 512:(c + 1) * 512]
        H1T_t, H1T = chunked("h1t", [128, 8, S], BF16, fmview)
        H2T_t, H2T = chunked("h2t", [128, 8, S], BF16, fmview)
        QT_t, QT = chunked("qt", [96, 8, S], BF16, fmview)
        KT_t, KT = chunked("kt", [96, 8, S], BF16, fmview)
        V_t, Vd = chunked("vv", [128, NT, 768], BF16, lambda t, c: t[:, 4 * c:4 * c + 4, :])
        SGA_t, SGA = chunked("sga", [128, 8, S], BF16, fmview)
        MB_t, MB = chunked("mb", [128, 8, S], BF16, fmview)

        identb = k.sb([128, 128], BF16, "identb")
        identf = k.sb([128, 128], F32, "identf")
        trib = k.sb([128, 128], BF16, "trib")
        cost = k.sb([128, NT, 16], F32, "cost")
        sint = k.sb([128, NT, 16], F32, "sint")
        epsb = k.sb([128, 1], F32, "epsb")
        wr = k.sb([128, NT, NE], F32, "wr")

        phase_no = [0]

        def end_phase():
            k.wait_all_dma(sp)
            phase_no[0] += 1
            k.simulate()
            with nc.named_scope(f"ph{phase_no[0]:02d}"):
                k.emit()

        with ExitStack() as st:
            idf = identf; trf = k.sb([128, 128], F32, "trf", st)
            posi = k.sb([128, NT], I32, "posi", st); posf = k.sb([128, NT], F32, "posf", st)
            invf = k.sb([128, 16], F32, "invf", st)
            ang = k.sb([128, NT, 16], F32, "ang", st)
            u = k.sb([128, NT, 16], F32, "u", st); nn = k.sb([128, NT, 16], F32, "nn", st)
            k.dma(sp, idf[:], I["ident"], writes=[idf]); k.dma(sp, trf[:], I["tri"], writes=[trf])
            k.dma(sp, posi[:], I["pos"], writes=[posi]); k.dma(sp, invf[:], I["invf"], writes=[invf])
            k.op(pool, lambda h: h.memset(epsb[:], EPS), writes=[epsb])
            k.op(pool, lambda h: h.tensor_copy(out=identb[:], in_=idf[:]), reads=[idf], writes=[identb])
            k.op(pool, lambda h: h.tensor_copy(out=trib[:], in_=trf[:]), reads=[trf], writes=[trib])
            k.op(dve, lambda h: h.tensor_copy(out=posf[:], in_=posi[:]), reads=[posi], writes=[posf])
            k.op(dve, lambda h: h.tensor_tensor(out=ang[:], in0=bc(posf[:], [128, NT, 16], 2), in1=bc(invf[:], [128, NT, 16], 1), op=ALU.mult),
                 reads=[posf, invf], writes=[ang])
            for tab, shift in ((sint, 0.0), (cost, TWO_PI / 4)):
                k.op(dve, lambda h, shift=shift: h.tensor_scalar(out=u[:], in0=ang[:], scalar1=shift, scalar2=None, op0=ALU.add), reads=[ang], writes=[u])
                k.op(dve, lambda h: h.tensor_scalar(out=nn[:], in0=u[:], scalar1=1.0 / TWO_PI, scalar2=MAGIC, op0=ALU.mult, op1=ALU.add), reads=[u], writes=[nn])
                k.op(dve, lambda h: h.tensor_scalar(out=nn[:], in0=nn[:], scalar1=MAGIC, scalar2=None, op0=ALU.subtract), reads=[nn], writes=[nn])
                k.op(dve, lambda h: h.scalar_tensor_tensor(out=u[:], in0=nn[:], scalar=-C1, in1=u[:], op0=ALU.mult, op1=ALU.add), reads=[nn, u], writes=[u])
                k.op(dve, lambda h: h.scalar_tensor_tensor(out=u[:], in0=nn[:], scalar=-C2, in1=u[:], op0=ALU.mult, op1=ALU.add), reads=[nn, u], writes=[u])
                k.op(dve, lambda h: h.tensor_scalar(out=u[:], in0=u[:], scalar1=-PI_SAFE, scalar2=PI_SAFE, op0=ALU.max, op1=ALU.min), reads=[u], writes=[u])
                k.op(act, lambda h, tab=tab: h.activation(out=tab[:], in_=u[:], func=AF.Sin), reads=[u], writes=[tab])
            end_phase()

        cast_i = [0]

        def cast(out_ap, in_ap, scale_ap, reads, writes, eng=None):
            if eng is None:
                eng = act if cast_i[0] % 2 == 0 else dve
                cast_i[0] += 1
            if eng is act:
                if scale_ap is None:
                    k.op(act, lambda h: h.activation(out=out_ap, in_=in_ap, func=AF.Copy), reads=reads, writes=writes)
                else:
                    k.op(act, lambda h: h.activation(out=out_ap, in_=in_ap, func=AF.Copy, scale=scale_ap), reads=reads, writes=writes)
            else:
                if scale_ap is None:
                    k.op(eng, lambda h: h.tensor_copy(out=out_ap, in_=in_ap), reads=reads, writes=writes)
                else:
                    k.op(eng, lambda h: h.tensor_scalar(out=out_ap, in0=in_ap, scalar1=scale_ap, scalar2=None, op0=ALU.mult), reads=reads, writes=writes)

        def load_w(st_bufs, dst, dst_fn, src, nk, n, scale=None, c0=0, q=None, eng=None):
            q = q or sp
            for kc in range(nk):
                sg = st_bufs.next()
                k.dma(q, sg[:, 0:n], src[kc * 128:(kc + 1) * 128, c0:c0 + n], writes=[sg])
                if scale is None:
                    cast(dst_fn(kc), sg[:, 0:n], None, [sg], [dst], eng)
                else:
                    cast(dst_fn(kc), sg[:, 0:n], scale[:, kc:kc + 1], [sg, scale], [dst], eng)

        def rms_T(xin, ssq, rstd, hb, hT, pTs, junk):
            for j in range(4):
                k.op(act, lambda h, j=j: h.activation(out=junk[:], in_=xin[:, j, :], func=AF.Square, scale=1.0 / 32.0, accum_out=ssq[:, j:j + 1]),
                     reads=[xin.sub(j)], writes=[ssq])
            k.op(act, lambda h: h.activation(out=ssq[:], in_=ssq[:], func=AF.Sqrt, bias=epsb[:, 0:1], scale=1.0), reads=[ssq, epsb], writes=[ssq])
            k.op(dve, lambda h: h.reciprocal(out=rstd[:], in_=ssq[:]), reads=[ssq], writes=[rstd])
            for j in range(4):
                if j % 2 == 0:
                    k.op(act, lambda h, j=j: h.activation(out=hb[:, j, :], in_=xin[:, j, :], func=AF.Copy, scale=rstd[:, j:j + 1]), reads=[xin.sub(j), rstd], writes=[hb.sub(j)])
                else:
                    k.op(dve, lambda h, j=j: h.tensor_scalar(out=hb[:, j, :], in0=xin[:, j, :], scalar1=rstd[:, j:j + 1], scalar2=None, op0=ALU.mult),
                         reads=[xin.sub(j), rstd], writes=[hb.sub(j)])
            for kc in range(8):
                pT = pTs.next()
                for j in range(4):
                    k.op(pe, lambda h, j=j, kc=kc, pT=pT: h.transpose(out=pT[:, j * 128:(j + 1) * 128], in_=hb[:, j, kc * 128:(kc + 1) * 128], identity=identb[:]),
                         reads=[hb.sub(j), identb], writes=[pT], inc=(j == 3))
                e = act if kc % 2 == 0 else dve
                if e is act:
                    k.op(act, lambda h, kc=kc, pT=pT: h.activation(out=hT[:, kc, :], in_=pT[:, 0:512], func=AF.Copy), reads=[pT], writes=[hT.sub(kc)])
                else:
                    k.op(dve, lambda h, kc=kc, pT=pT: h.tensor_copy(out=hT[:, kc, :], in_=pT[:, 0:512]), reads=[pT], writes=[hT.sub(kc)])

        def rope(x1, x2, cs, sn, d1, d2, tmps, rd, wr_, tb, eng=None):
            eng = eng or dve
            t1, t2 = tmps
            b1, b2 = tb
            k.op(eng, lambda h: h.tensor_tensor(out=t1, in0=x1, in1=cs, op=ALU.mult), reads=rd, writes=[b1])
            yield
            k.op(eng, lambda h: h.tensor_tensor(out=t2, in0=x2, in1=sn, op=ALU.mult), reads=rd, writes=[b2])
            yield
            k.op(eng, lambda h: h.tensor_tensor(out=d1, in0=t1, in1=t2, op=ALU.subtract), reads=[b1, b2], writes=wr_)
            yield
            k.op(eng, lambda h: h.tensor_tensor(out=t1, in0=x2, in1=cs, op=ALU.mult), reads=rd, writes=[b1])
            yield
            k.op(eng, lambda h: h.tensor_tensor(out=t2, in0=x1, in1=sn, op=ALU.mult), reads=rd, writes=[b2])
            yield
            k.op(eng, lambda h: h.tensor_tensor(out=d2, in0=t1, in1=t2, op=ALU.add), reads=[b1, b2], writes=wr_)
            yield

        dbg_dump = []
        for L in range(2):
            Xc = Xin_c if L == 0 else Y_c
            with ExitStack() as st:
                stg = PsPool([k.sb([128, 1024], F32, f"stg{i}", st) for i in range(2)])
                Wsm = k.sb([128, 8, 416], BF16, "Wsm", st); Wuq = k.sb([128, 2, 768], BF16, "Wuq", st); Wukv = k.sb([128, 1024], BF16, "Wukv", st)
                g1 = k.sb([128, 8], F32, "g1", st); qng = k.sb([128, 2], F32, "qng", st); kvng = k.sb([128, 1], F32, "kvng", st)
                qkg = k.sb([128, 192], F32, "qkg", st)
                for b_, nm in ((g1, "g1"), (qng, "qng"), (kvng, "kvng"), (qkg, "qkg")):
                    k.dma(sp, b_[:], I[f"{nm}{L}"], writes=[b_])
                load_w(stg, Wsm, lambda kc: Wsm[:, kc, :], I[f"w_in{L}"], 8, 416, g1)
                load_w(stg, Wuq, lambda kc: Wuq[:, kc, :], I[f"w_uq{L}"], 2, 768, qng)
                load_w(stg, Wukv, lambda kc: Wukv[:, :], I[f"w_ukv{L}"], 1, 1024, kvng)
                xins = [k.sb([128, 4, 1024], F32, f"xin{i}", st) for i in range(2)]
                for xb_ in xins:
                    for j in range(4):
                        xb_.sub(j)
                hb = k.sb([128, 4, 1024], BF16, "hb", st)
                hTs = [k.sb([128, 8, 512], BF16, f"hT{i}", st) for i in range(2)]
                junk = k.sb([128, 1024], BF16, "junk", st)
                chunkB = []
                for i in range(2):
                    chunkB.append((k.sb([128, 4, 416], F32, f"csm{i}", st), k.sb([128, 4, 4], F32, f"st4{i}", st), k.sb([128, 4, 2], F32, f"rs4{i}", st),
                                   k.sb([128, 4, 384], BF16, f"cqn{i}", st), k.sb([128, 3, 512], BF16, f"cT{i}", st),
                                   k.sb([128, 4], F32, f"ssq{i}", st), k.sb([128, 4], F32, f"rstd{i}", st)))
                tileB = []
                for i in range(2):
                    tileB.append((k.sb([128, 8, 96], F32, f"q_s{i}", st), k.sb([128, 8, 128], F32, f"kv_s{i}", st),
                                  k.sb([128, 8, 96], F32, f"sqt{i}", st), k.sb([128, 8, 64], F32, f"sqk{i}", st),
                                  k.sb([128, 16], F32, f"ss{i}", st), k.sb([128, 16], F32, f"rs{i}", st),
                                  k.sb([128, 8, 96], F32, f"qf{i}", st), k.sb([128, 8, 64], F32, f"kf{i}", st),
                                  k.sb([128, 8, 96], BF16, f"qb{i}", st), k.sb([128, 8, 96], BF16, f"kb{i}", st),
                                  k.sb([128, 32], F32, f"krg{i}", st), k.sb([128, 32], F32, f"krr{i}", st),
                                  k.sb([128, 2, 8, 16], F32, f"tmpB{i}", st), k.sb([128, 2, 16], F32, f"tmpK{i}", st),
                                  (Buf(None, "tq1"), Buf(None, "tq2")), (Buf(None, "tk1"), Buf(None, "tk2"))))
                QTs = k.sb([128, 8, 512], BF16, "QTs", st); KTs = k.sb([128, 8, 512], BF16, "KTs", st)
                Vs = k.sb([128, 4, 768], BF16, "Vs", st)
                pTs = PsPool([k.ps([128, 1024], BF16, f"pT{i}", st) for i in range(2)])
                psms = PsPool([k.ps([128, 512], F32, f"psm{i}", st) for i in range(2)])
                pq = k.ps([128, 2, 512], F32, "pq", st); pkv = k.ps([128, 2, 512], F32, "pkv", st)
                for j in range(4):
                    Vs.sub(j); QTs.sub(j); KTs.sub(j)
                k.op(pool, lambda h: h.memset(Vs[:], 1.0), writes=Vs.all())
                gq = qkg[:, 0:96]; gk = qkg[:, 96:192]
                def tile_gen(c, j, B, CB):
                    t = 4 * c + j
                    q_s, kv_s, sqt, sqk, ss, rs, qf, kf, qb, kb, krg, krr, tmpB, tmpK, tbq, tbk = B
                    csm, st4, rs4, cqn, cT, ssq, rstd = CB
                    if True:
                        for hh in range(2):
                            for kc in range(2):
                                k.op(pe, lambda h, j=j, hh=hh, kc=kc: h.matmul(pq[:, hh, 0:384], lhsT=cT[:, kc, j * 128:(j + 1) * 128], rhs=Wuq[:, kc, hh * 384:(hh + 1) * 384], start=(kc == 0), stop=(kc == 1)),
                                     reads=[cT.sub(kc), Wuq], writes=[pq], inc=(kc == 1))
                        for hh in range(2):
                            k.op(pe, lambda h, j=j, hh=hh: h.matmul(pkv[:, hh, :], lhsT=cT[:, 2, j * 128:(j + 1) * 128], rhs=Wukv[:, hh * 512:(hh + 1) * 512], start=True, stop=True),
                                 reads=[cT.sub(2), Wukv], writes=[pkv])
                        for hh in range(2):
                            k.op(act, lambda h, hh=hh: h.activation(out=q_s[:, 4 * hh:4 * hh + 4, :], in_=pq[:, hh, 0:384].rearrange("p (a d) -> p a d", a=4), func=AF.Copy), reads=[pq], writes=[q_s])
                            k.op(act, lambda h, hh=hh: h.activation(out=kv_s[:, 4 * hh:4 * hh + 4, :], in_=pkv[:, hh, :].rearrange("p (a d) -> p a d", a=4), func=AF.Copy), reads=[pkv], writes=[kv_s])
                        k.op(act, lambda h: h.activation(out=sqt[:], in_=q_s[:], func=AF.Square), reads=[q_s], writes=[sqt])
                        yield
                        k.op(act, lambda h: h.activation(out=sqk[:], in_=kv_s[:, :, 0:64], func=AF.Square), reads=[kv_s], writes=[sqk])
                        yield
                    if True:
                        k.op(dve, lambda h: h.tensor_reduce(out=ss[:, 0:8], in_=sqt[:], axis=AX.X, op=ALU.add), reads=[sqt], writes=[ss])
                        yield
                        k.op(dve, lambda h: h.tensor_reduce(out=ss[:, 8:16], in_=sqk[:], axis=AX.X, op=ALU.add), reads=[sqk], writes=[ss])
                        yield
                        k.op(dve, lambda h, j=j: h.tensor_scalar(out=ss[:, 8:16], in0=ss[:, 8:16], scalar1=st4[:, j, 2:3], scalar2=None, op0=ALU.add), reads=[ss, st4.sub(j)], writes=[ss])
                        yield
                        k.op(act, lambda h: h.activation(out=ss[:], in_=ss[:], func=AF.Sqrt, bias=epsb[:, 0:1], scale=1.0 / 96.0), reads=[ss, epsb], writes=[ss])
                        yield
                        k.op(dve, lambda h: h.reciprocal(out=rs[:], in_=ss[:]), reads=[ss], writes=[rs])
                        yield
                        k.op(dve, lambda h: h.tensor_scalar(out=rs[:, 0:8], in0=rs[:, 0:8], scalar1=96.0 ** -0.5, scalar2=None, op0=ALU.mult), reads=[rs], writes=[rs])
                        yield
                    if True:
                        k.op(dve, lambda h: h.tensor_tensor(out=qf[:], in0=q_s[:], in1=bc(rs[:, 0:8], [128, 8, 96], 2), op=ALU.mult), reads=[q_s, rs], writes=[qf])
                        yield
                        k.op(dve, lambda h: h.tensor_tensor(out=qf[:], in0=qf[:], in1=bc(gq, [128, 8, 96], 1), op=ALU.mult), reads=[qf, qkg], writes=[qf])
                        yield
                        k.op(act, lambda h: h.activation(out=qb[:, :, 0:64], in_=qf[:, :, 0:64], func=AF.Copy), reads=[qf], writes=[qb])
                        yield
                        cs8 = bc(cost[:, t, :], [128, 8, 16], 1); sn8 = bc(sint[:, t, :], [128, 8, 16], 1)
                        yield from rope(qf[:, :, 64:80], qf[:, :, 80:96], cs8, sn8, qb[:, :, 64:80], qb[:, :, 80:96], (tmpB[:, 0, :, :], tmpB[:, 1, :, :]), [qf, cost, sint], [qb], tbq)
                        k.op(dve, lambda h: h.tensor_tensor(out=kf[:], in0=kv_s[:, :, 0:64], in1=bc(rs[:, 8:16], [128, 8, 64], 2), op=ALU.mult), reads=[kv_s, rs], writes=[kf])
                        yield
                        k.op(dve, lambda h: h.tensor_tensor(out=kb[:, :, 0:64], in0=kf[:], in1=bc(qkg[:, 96:160], [128, 8, 64], 1), op=ALU.mult), reads=[kf, qkg], writes=[kb])
                        yield
                        k.op(pool, lambda h, j=j: h.tensor_tensor(out=krg[:], in0=csm[:, j, 384:416], in1=qkg[:, 160:192], op=ALU.mult), reads=[csm.sub(j), qkg], writes=[krg])
                        yield
                        yield from rope(krg[:, 0:16], krg[:, 16:32], cost[:, t, :], sint[:, t, :], krr[:, 0:16], krr[:, 16:32], (tmpK[:, 0, :], tmpK[:, 1, :]), [krg, cost, sint], [krr], tbk, pool)
                        k.op(pool, lambda h: h.tensor_tensor(out=kb[:, :, 64:96], in0=bc(krr[:], [128, 8, 32], 1), in1=bc(rs[:, 8:16], [128, 8, 32], 2), op=ALU.mult), reads=[krr, rs], writes=[kb])
                        yield
                        kv4 = kv_s[:].rearrange("p (a b) d -> p a b d", b=2)
                        Vv = Vs[:, j, :].rearrange("p (a c) -> p a c", c=192)
                        k.op(act, lambda h, kv4=kv4, Vv=Vv: h.activation(out=Vv[:, :, 0:64], in_=kv4[:, :, 0, 64:128], func=AF.Copy), reads=[kv_s], writes=[Vs.sub(j)])
                        yield
                        k.op(act, lambda h, kv4=kv4, Vv=Vv: h.activation(out=Vv[:, :, 128:192], in_=kv4[:, :, 1, 64:128], func=AF.Copy), reads=[kv_s], writes=[Vs.sub(j)])
                        yield
                    if True:
                        for src, dstT in ((qb, QTs), (kb, KTs)):
                            pT = pTs.next()
                            for hd in range(8):
                                k.op(pe, lambda h, hd=hd, pT=pT, src=src: h.transpose(out=pT[0:96, hd * 128:(hd + 1) * 128], in_=src[:, hd, :], identity=identb[:]),
                                     reads=[src, identb], writes=[pT], inc=(hd == 7))
                            k.op(dve, lambda h, j=j, pT=pT, dstT=dstT: h.tensor_copy(out=dstT[0:96, :, j * 128:(j + 1) * 128], in_=pT[0:96, :].rearrange("p (a d) -> p a d", a=8)), reads=[pT], writes=[dstT.sub(j)])

                k.dma(sp, xins[0][:], Xc[0].t, reads=[Xc[0]], writes=xins[0].all())

                def p1a_chunk(c, CB):
                    csm, st4, rs4, cqn, cT, ssq, rstd = CB
                    xin = xins[c % 2]; hT = hTs[c % 2]
                    if c + 1 < NCH:
                        k.dma(sp, xins[(c + 1) % 2][:], Xc[c + 1].t, reads=[Xc[c + 1]], writes=xins[(c + 1) % 2].all())
                    rms_T(xin, ssq, rstd, hb, hT, pTs, junk)
                    k.dma(sp, H1T[c].t, hT[:], reads=hT.all(), writes=[H1T[c]])
                    for j in range(4):
                        psm = psms.next()
                        for kc in range(8):
                            k.op(pe, lambda h, j=j, kc=kc, psm=psm, hT=hT: h.matmul(psm[:, 0:416], lhsT=hT[:, kc, j * 128:(j + 1) * 128], rhs=Wsm[:, kc, :], start=(kc == 0), stop=(kc == 7)),
                                 reads=[hT.sub(kc), Wsm], writes=[psm], inc=(kc == 7))
                        k.op(act, lambda h, j=j, psm=psm: h.activation(out=csm[:, j, :], in_=psm[:, 0:416], func=AF.Copy), reads=[psm], writes=[csm.sub(j)])
                        k.op(act, lambda h, j=j: h.activation(out=junk[:, 0:256], in_=csm[:, j, 0:256], func=AF.Square, scale=1.0 / 16.0, accum_out=st4[:, j, 0:1]), reads=[csm.sub(j)], writes=[st4.sub(j)])
                        k.op(act, lambda h, j=j: h.activation(out=junk[:, 0:128], in_=csm[:, j, 256:384], func=AF.Square, scale=128.0 ** -0.5, accum_out=st4[:, j, 1:2]), reads=[csm.sub(j)], writes=[st4.sub(j)])
                        k.op(act, lambda h, j=j: h.activation(out=junk[:, 0:32], in_=csm[:, j, 384:416], func=AF.Square, accum_out=st4[:, j, 2:3]), reads=[csm.sub(j)], writes=[st4.sub(j)])
                    k.op(act, lambda h: h.activation(out=rs4[:], in_=st4[:, :, 0:2], func=AF.Sqrt, bias=epsb[:, 0:1], scale=1.0), reads=st4.all() + [epsb], writes=[rs4])
                    k.op(dve, lambda h: h.reciprocal(out=rs4[:], in_=rs4[:]), reads=[rs4], writes=[rs4])
                    for j in range(4):
                        k.op(act, lambda h, j=j: h.activation(out=cqn[:, j, 0:256], in_=csm[:, j, 0:256], func=AF.Copy, scale=rs4[:, j, 0:1]), reads=[csm.sub(j), rs4], writes=[cqn.sub(j)])
                        k.op(act, lambda h, j=j: h.activation(out=cqn[:, j, 256:384], in_=csm[:, j, 256:384], func=AF.Copy, scale=rs4[:, j, 1:2]), reads=[csm.sub(j), rs4], writes=[cqn.sub(j)])
                    for kk in range(3):
                        pT = pTs.next()
                        for j in range(4):
                            k.op(pe, lambda h, j=j, kk=kk, pT=pT: h.transpose(out=pT[:, j * 128:(j + 1) * 128], in_=cqn[:, j, kk * 128:(kk + 1) * 128], identity=identb[:]),
                                 reads=[cqn.sub(j), identb], writes=[pT], inc=(j == 3))
                        k.op(act, lambda h, kk=kk, pT=pT: h.activation(out=cT[:, kk, :], in_=pT[:, 0:512], func=AF.Copy), reads=[pT], writes=[cT.sub(kk)])
                    gens = [tile_gen(c, j, tileB[j % 2], CB) for j in range(4)]
                    active = [gens[0], gens[1]]; nxt = 2
                    while active:
                        for g in list(active):
                            try:
                                next(g)
                            except StopIteration:
                                active.remove(g)
                                if nxt < 4:
                                    active.append(gens[nxt]); nxt += 1
                    k.dma(sp, QT[c].t, QTs[0:96, :, :], reads=QTs.all(), writes=[QT[c]])
                    k.dma(sp, KT[c].t, KTs[0:96, :, :], reads=KTs.all(), writes=[KT[c]])
                    k.dma(sp, Vd[c].t, Vs[:], reads=Vs.all(), writes=[Vd[c]])

                for c in range(NCH):
                    p1a_chunk(c, chunkB[c % 2])
                end_phase()
            if stop_after == f"P1a{L}":
                break

            with ExitStack() as st:
                stg = PsPool([k.sb([128, 1024], F32, f"stg{i}", st) for i in range(2)])
                Wbig = k.sb([128, 8, 3072], BF16, "Wbig", st)
                Wrg = k.sb([128, 4, 128], BF16, "Wrg", st); Wig = k.sb([128, 4, 128], BF16, "Wig", st)
                Wupl = k.sb([128, 4, 1024], BF16, "Wupl", st)
                g1 = k.sb([128, 8], F32, "g1", st); cw = k.sb([128, 4, 4], F32, "cw", st); lv = k.sb([128, 4, 4], F32, "lv", st)
                cA = k.sb([128, 4], F32, "cA", st); cA2 = k.sb([128, 4], F32, "cA2", st)
                for b_, nm in ((g1, "g1"), (cw, "convw"), (lv, "lruv")):
                    k.dma(sp, b_[:], I[f"{nm}{L}"], writes=[b_])
                for pc in range(3):
                    load_w(stg, Wbig, lambda kc, pc=pc: Wbig[:, kc, pc * 1024:(pc + 1) * 1024], I[f"w_in{L}"], 8, 1024, g1, c0=416 + pc * 1024)
                sgx = stg.next()
                k.dma(sp, sgx[:, 0:512], I[f"w_rg{L}"].rearrange("p a b -> p (a b)"), writes=[sgx])
                cast(Wrg[:].rearrange("p a b -> p (a b)"), sgx[:, 0:512], None, [sgx], [Wrg])
                sgx2 = stg.next()
                k.dma(sp, sgx2[:, 0:512], I[f"w_ig{L}"].rearrange("p a b -> p (a b)"), writes=[sgx2])
                cast(Wig[:].rearrange("p a b -> p (a b)"), sgx2[:, 0:512], None, [sgx2], [Wig])
                load_w(stg, Wupl, lambda kc: Wupl[:, kc, :], I[f"w_upl{L}"], 4, 1024)
                k.op(act, lambda h: h.activation(out=cA[:], in_=lv[:, :, 3], func=AF.Exp, scale=-1.0), reads=[lv], writes=[cA])
                k.op(dve, lambda h: h.tensor_scalar(out=cA[:], in0=cA[:], scalar1=1.0, scalar2=None, op0=ALU.add), reads=[cA], writes=[cA])
                k.op(act, lambda h: h.activation(out=cA[:], in_=cA[:], func=AF.Ln), reads=[cA], writes=[cA])
                k.op(dve, lambda h: h.tensor_scalar(out=cA2[:], in0=cA[:], scalar1=-16.0, scalar2=None, op0=ALU.mult), reads=[cA], writes=[cA2])
                k.op(dve, lambda h: h.tensor_scalar(out=cA[:], in0=cA[:], scalar1=-8.0, scalar2=None, op0=ALU.mult), reads=[cA], writes=[cA])
                hTs = [k.sb([128, 8, 512], BF16, f"hT{i}", st) for i in range(1)]
                xl = k.sb([128, 4, 515], F32, "xl", st)
                hprev = k.sb([128, 4], F32, "hprev", st)
                tA = [k.sb([128, 512], F32, f"tA{i}", st) for i in range(4)]
                xcb = [k.sb([128, 512], BF16, f"xcb{i}", st) for i in range(4)]
                tR = [k.sb([128, 512], F32, f"tR{i}", st) for i in range(4)]
                tI = [k.sb([128, 512], F32, f"tI{i}", st) for i in range(4)]
                tM = [k.sb([128, 512], F32, f"tM{i}", st) for i in range(4)]
                tH = [k.sb([128, 512], F32, f"tH{i}", st) for i in range(4)]
                tG = [k.sb([128, 512], F32, f"tG{i}", st) for i in range(4)]
                yl = k.sb([128, 4, 512], BF16, "yl", st)
                sga_s = k.sb([128, 8, 512], BF16, "sgas", st)
                sgb_s = k.sb([128, 8, 512], BF16, "sgbs", st)
                mb_s = k.sb([128, 8, 512], BF16, "mbs", st)
                pp = PsPool([k.ps([128, 512], F32, f"pp{i}", st) for i in range(8)])
                for fc in range(4):
                    xl.sub(fc)
                k.op(pool, lambda h: h.memset(xl[:], 0.0), writes=xl.all())
                k.op(pool, lambda h: h.memset(hprev[:], 0.0), writes=[hprev])

                def proj(hT, col):
                    p_ = pp.next()
                    for kc in range(8):
                        k.op(pe, lambda h, kc=kc, p_=p_: h.matmul(p_[:], lhsT=Wbig[:, kc, col:col + 128], rhs=hT[:, kc, :], start=(kc == 0), stop=(kc == 7)),
                             reads=[hT, Wbig], writes=[p_], inc=(kc == 7))
                    return p_

                k.dma(sp, hTs[0][:], H1T[0].t, reads=[H1T[0]], writes=[hTs[0]])
                for c in range(NCH):
                    hT = hTs[0]
                    for fc in range(4):
                        px = proj(hT, fc * 128)
                        k.op(act, lambda h, fc=fc, px=px: h.activation(out=xl[:, fc, 3:515], in_=px[:], func=AF.Copy), reads=[px], writes=[xl.sub(fc)])
                    for fc in range(4):
                        xc = tA[fc]
                        k.op(dve, lambda h, fc=fc, xc=xc: h.tensor_scalar(out=xc[:], in0=xl[:, fc, 0:512], scalar1=cw[:, fc, 0:1], scalar2=lv[:, fc, 0:1], op0=ALU.mult, op1=ALU.add),
                             reads=[xl.sub(fc), cw, lv], writes=[xc])
                        for tp in range(1, 4):
                            k.op(dve, lambda h, fc=fc, tp=tp, xc=xc: h.scalar_tensor_tensor(out=xc[:], in0=xl[:, fc, tp:tp + 512], scalar=cw[:, fc, tp:tp + 1], in1=xc[:], op0=ALU.mult, op1=ALU.add),
                                 reads=[xl.sub(fc), cw, xc], writes=[xc])
                        k.op(act, lambda h, fc=fc, xc=xc: h.activation(out=xcb[fc][:], in_=xc[:], func=AF.Copy), reads=[xc], writes=[xcb[fc]])
                    for fc in range(4):
                        k.op(pool, lambda h, fc=fc: h.tensor_copy(out=xl[:, fc, 0:3], in_=xl[:, fc, 512:515]), reads=[xl.sub(fc)], writes=[xl.sub(fc)])
                    for fc in range(4):
                        pg = proj(hT, 512 + fc * 128)
                        k.op(act, lambda h, fc=fc, pg=pg: h.activation(out=tG[fc][:], in_=pg[:], func=AF.Copy), reads=[pg], writes=[tG[fc]])
                    for dc in range(8):
                        pga = proj(hT, 1024 + dc * 128)
                        k.op(act, lambda h, dc=dc, pga=pga: h.activation(out=sga_s[:, dc, :], in_=pga[:], func=AF.Sigmoid), reads=[pga], writes=[sga_s.sub(dc)])
                    for fc in range(4):
                        pr_ = pp.next(); pi_ = pp.next()
                        k.op(pe, lambda h, fc=fc, pr_=pr_: h.matmul(pr_[:], lhsT=Wrg[:, fc, :], rhs=xcb[fc][:], start=True, stop=True), reads=[Wrg, xcb[fc]], writes=[pr_])
                        k.op(pe, lambda h, fc=fc, pi_=pi_: h.matmul(pi_[:], lhsT=Wig[:, fc, :], rhs=xcb[fc][:], start=True, stop=True), reads=[Wig, xcb[fc]], writes=[pi_])
                        k.op(act, lambda h, fc=fc, pr_=pr_: h.activation(out=tR[fc][:], in_=pr_[:], func=AF.Sigmoid, bias=lv[:, fc, 1:2], scale=1.0), reads=[pr_, lv], writes=[tR[fc]])
                        k.op(act, lambda h, fc=fc, pi_=pi_: h.activation(out=tI[fc][:], in_=pi_[:], func=AF.Sigmoid, bias=lv[:, fc, 2:3], scale=1.0), reads=[pi_, lv], writes=[tI[fc]])
                    pgbs = []
                    for dc in range(8):
                        pgb = proj(hT, 2048 + dc * 128)
                        pgbs.append((dc, pgb))
                    if c + 1 < NCH:
                        k.dma(sp, hT[:], H1T[c + 1].t, reads=[H1T[c + 1]], writes=[hT])
                    for fc in range(4):
                        k.op(act, lambda h, fc=fc: h.activation(out=tM[fc][:], in_=tR[fc][:], func=AF.Exp, scale=cA2[:, fc:fc + 1]), reads=[tR[fc], cA2], writes=[tM[fc]])
                        k.op(act, lambda h, fc=fc: h.activation(out=tR[fc][:], in_=tR[fc][:], func=AF.Exp, scale=cA[:, fc:fc + 1]), reads=[tR[fc], cA], writes=[tR[fc]])
                        k.op(dve, lambda h, fc=fc: h.tensor_scalar(out=tM[fc][:], in0=tM[fc][:], scalar1=-1.0, scalar2=1.0, op0=ALU.mult, op1=ALU.add), reads=[tM[fc]], writes=[tM[fc]])
                        k.op(dve, lambda h, fc=fc: h.tensor_tensor(out=tI[fc][:], in0=tI[fc][:], in1=tA[fc][:], op=ALU.mult), reads=[tI[fc], tA[fc]], writes=[tI[fc]])
                    for fc in range(4):
                        k.op(act, lambda h, fc=fc: h.activation(out=tM[fc][:], in_=tM[fc][:], func=AF.Sqrt), reads=[tM[fc]], writes=[tM[fc]])
                        k.op(dve, lambda h, fc=fc: h.tensor_tensor(out=tI[fc][:], in0=tI[fc][:], in1=tM[fc][:], op=ALU.mult), reads=[tI[fc], tM[fc]], writes=[tI[fc]])
                        k.op(dve, lambda h, fc=fc: h.tensor_tensor_scan(out=tH[fc][:], data0=tR[fc][:], data1=tI[fc][:], initial=hprev[:, fc:fc + 1], op0=ALU.mult, op1=ALU.add),
                             reads=[tR[fc], tI[fc], hprev], writes=[tH[fc]])
                        k.op(dve, lambda h, fc=fc: h.tensor_copy(out=hprev[:, fc:fc + 1], in_=tH[fc][:, 511:512]), reads=[tH[fc]], writes=[hprev])
                    for fc in range(4):
                        k.op(act, lambda h, fc=fc: h.activation(out=tG[fc][:], in_=tG[fc][:], func=AF.Gelu_apprx_tanh), reads=[tG[fc]], writes=[tG[fc]])
                        k.op(dve, lambda h, fc=fc: h.tensor_tensor(out=yl[:, fc, :], in0=tH[fc][:], in1=tG[fc][:], op=ALU.mult), reads=[tH[fc], tG[fc]], writes=[yl.sub(fc)])
                    for (dc, pgb) in pgbs:
                        k.op(act, lambda h, dc=dc, pgb=pgb: h.activation(out=sgb_s[:, dc, :], in_=pgb[:], func=AF.Sigmoid), reads=[pgb], writes=[sgb_s.sub(dc)])
                    for dc in range(8):
                        pu = pp.next()
                        for fc in range(4):
                            k.op(pe, lambda h, fc=fc, dc=dc, pu=pu: h.matmul(pu[:], lhsT=Wupl[:, fc, dc * 128:(dc + 1) * 128], rhs=yl[:, fc, :], start=(fc == 0), stop=(fc == 3)),
                                 reads=[Wupl, yl.sub(fc)], writes=[pu], inc=(fc == 3))
                        k.op(dve, lambda h, dc=dc, pu=pu: h.tensor_tensor(out=mb_s[:, dc, :], in0=pu[:], in1=sgb_s[:, dc, :], op=ALU.mult), reads=[pu, sgb_s.sub(dc)], writes=[mb_s.sub(dc)])
                    k.dma(sp, SGA[c].t, sga_s[:], reads=sga_s.all(), writes=[SGA[c]])
                    k.dma(sp, MB[c].t, mb_s[:], reads=mb_s.all(), writes=[MB[c]])
                end_phase()
            if stop_after == f"P1b{L}":
                break

            st23 = ExitStack()
            OT = k.sb([128, 4, S], BF16, f"OT{L}", st23)
            Wupa = k.sb([128, 4, 1024], BF16, "Wupa", st23); Wo = k.sb([128, 8, 1024], BF16, "Wo", st23)
            with ExitStack() as st:
                stgw = PsPool([k.sb([128, 1024], F32, f"stgw{i}", st) for i in range(2)])
                Vp = [k.sb([128, NT, 192], BF16, f"Vp{i}", st) for i in range(2)]
                KTh = [k.sb([128, S], BF16, f"KTh{i}", st) for i in range(2)]
                QTh = [k.sb([128, S], BF16, f"QTh{i}", st) for i in range(2)]
                pts = PsPool([k.sb([128, 512], BF16, f"pt{i}", st) for i in range(6)])
                rcs = PsPool([k.sb([128, 512], F32, f"rc{i}", st) for i in range(2)])
                pss = PsPool([k.ps([128, 512], F32, f"ps{i}", st) for i in range(5)])
                pos_ = PsPool([k.ps([128, 512], F32, f"po{i}", st) for i in range(3)])

                def load_head(hd):
                    i2 = hd % 2
                    if hd % 2 == 0:
                        pr = hd // 2
                        k.dma(sp, Vp[pr % 2][:], V_t[:, :, pr * 192:(pr + 1) * 192], reads=Vd, writes=[Vp[pr % 2]])
                    k.dma(sp, KTh[i2][0:96, :], KT_t[:, hd, :], reads=KT, writes=[KTh[i2]])
                    k.dma(sp, QTh[i2][0:96, :], QT_t[:, hd, :], reads=QT, writes=[QTh[i2]])
                load_head(0)

                def wprefetch():
                    for (W_, src, nk) in ((Wupa, I[f"w_upa{L}"], 4), (Wo, I[f"w_o{L}"], 8)):
                        for kc in range(nk):
                            sg = stgw.next()
                            k.dma(sp, sg[:, 0:1024], src[kc * 128:(kc + 1) * 128, 0:1024], writes=[sg])
                            yield
                            cast(W_[:, kc, :], sg[:, 0:1024], None, [sg], [W_], dve)
                            yield
                wgen = wprefetch(); wcnt = [0]
                LA = 3
                items = []
                for hd in range(8):
                    for c in range(NCH):
                        nk = 4 * c + 4
                        for kt in range(nk):
                            items.append((hd, c, kt, nk))
                state = {}

                def issue_S(it):
                    hd, c, kt, nk = it
                    if c == 0 and kt == 0 and hd + 1 < 8:
                        load_head(hd + 1)
                    if hd >= 2:
                        wcnt[0] += 1
                        if wcnt[0] % 3 == 0:
                            next(wgen, None)
                    Kh = KTh[hd % 2]; Qh = QTh[hd % 2]
                    dd = kt - 4 * c
                    q0 = dd * 128 if dd > 0 else 0
                    ps_ = pss.next(); pt = pts.next()
                    k.op(pe, lambda h: h.matmul(ps_[:, q0:512], lhsT=Kh[0:96, kt * 128:(kt + 1) * 128], rhs=Qh[0:96, c * 512 + q0:(c + 1) * 512], start=True, stop=True),
                         reads=[Kh, Qh], writes=[ps_])
                    k.op(act, lambda h: h.activation(out=pt[:, q0:512], in_=ps_[:, q0:512], func=AF.Exp), reads=[ps_], writes=[pt])
                    if dd >= 0:
                        k.op(dve, lambda h: h.tensor_tensor(out=pt[:, q0:q0 + 128], in0=pt[:, q0:q0 + 128], in1=trib[:], op=ALU.mult), reads=[pt, trib], writes=[pt])
                    state[it] = (pt, q0)

                def issue_PV(it):
                    hd, c, kt, nk = it
                    pt, q0 = state.pop(it)
                    pr = hd // 2; odd = hd % 2
                    Vh = Vp[pr % 2]; voff = 64 if odd else 0
                    if kt == 0:
                        state[("po", hd, c)] = pos_.next()
                    po = state[("po", hd, c)]
                    k.op(pe, lambda h: h.matmul(po[:, q0:512], lhsT=Vh[:, kt, voff:voff + 128], rhs=pt[:, q0:512], start=(kt == 0), stop=(kt == nk - 1)),
                         reads=[Vh, pt], writes=[po], inc=(kt == nk - 1))
                    if kt == nk - 1:
                        del state[("po", hd, c)]
                        rc = rcs.next()
                        if not odd:
                            k.op(dve, lambda h: h.reciprocal(out=rc[64:128, :], in_=po[64:128, :]), reads=[po], writes=[rc])
                            k.op(dve, lambda h: h.tensor_tensor(out=OT[0:64, pr, c * 512:(c + 1) * 512], in0=po[0:64, :], in1=rc[64:128, :], op=ALU.mult), reads=[po, rc], writes=[OT])
                        else:
                            k.op(dve, lambda h: h.reciprocal(out=rc[0:64, :], in_=po[0:64, :]), reads=[po], writes=[rc])
                            k.op(dve, lambda h: h.tensor_tensor(out=OT[64:128, pr, c * 512:(c + 1) * 512], in0=po[64:128, :], in1=rc[0:64, :], op=ALU.mult), reads=[po, rc], writes=[OT])

                for i in range(len(items) + LA):
                    if i < len(items):
                        issue_S(items[i])
                    if i - LA >= 0:
                        issue_PV(items[i - LA])
                for _ in wgen:
                    pass
                if debug and "ot" in debug and L == 0:
                    k.dma(sp, dbg_out["ot"], OT[:], reads=[OT])
                end_phase()
            if stop_after == f"P2{L}":
                st23.close()
                break

            with ExitStack() as st:
                xins = [k.sb([128, 4, 1024], F32, f"xin{i}", st) for i in range(2)]
                for xb_ in xins:
                    for j in range(4):
                        xb_.sub(j)
                sgl = [k.sb([128, 8, 512], BF16, f"sgl{i}", st) for i in range(2)]
                mbl = [k.sb([128, 8, 512], BF16, f"mbl{i}", st) for i in range(2)]
                mg = k.sb([128, 8, 512], BF16, "mg", st)
                tmpf = [k.sb([128, 512], F32, f"tmpf{i}", st) for i in range(2)]
                hb = k.sb([128, 4, 1024], BF16, "hb", st); hT = k.sb([128, 8, 512], BF16, "hT", st)
                junk = k.sb([128, 1024], BF16, "junk", st)
                ssq = k.sb([128, 4], F32, "ssq", st); rstd = k.sb([128, 4], F32, "rstd", st)
                pTs = PsPool([k.ps([128, 1024], BF16, f"pT{i}", st) for i in range(2)])
                pp = PsPool([k.ps([128, 512], F32, f"pp{i}", st) for i in range(6 if L == 0 else 3)])
                if L == 1:
                    pTfs = PsPool([k.ps([128, 512], F32, f"pTf{i}", st) for i in range(2)])
                    plg = k.ps([128, 512], F32, "plg", st)
                    xTf = k.sb([128, 8, 128], F32, "xTf", st)
                    rtw = k.sb([128, 8, NE], F32, "rtw", st); g2r = k.sb([128, 8], F32, "g2r", st)
                    k.dma(sp, rtw[:], I["rtw"], writes=[rtw]); k.dma(sp, g2r[:], I["g21"], writes=[g2r])
                    k.op(dve, lambda h: h.tensor_tensor(out=rtw[:], in0=rtw[:], in1=bc(g2r[:], [128, 8, NE], 2), op=ALU.mult), reads=[rtw, g2r], writes=[rtw])
                    lg = k.sb([128, 4, NE], F32, "lg", st); m1 = k.sb([128, 4], F32, "m1", st); m2 = k.sb([128, 4], F32, "m2", st)
                    msk = k.sb([128, 4, NE], F32, "msk", st); lg2 = k.sb([128, 4, NE], F32, "lg2", st); ex = k.sb([128, 4, NE], F32, "ex", st)
                    den = k.sb([128, 4], F32, "den", st)

                def route(c, xin):
                    for j in range(4):
                        for half in range(2):
                            pTf = pTfs.next()
                            for q4 in range(4):
                                kc = half * 4 + q4
                                k.op(pe, lambda h, j=j, kc=kc, q4=q4, pTf=pTf: h.transpose(out=pTf[:, q4 * 128:(q4 + 1) * 128], in_=xin[:, j, kc * 128:(kc + 1) * 128], identity=identf[:]),
                                     reads=[xin.sub(j), identf], writes=[pTf], inc=(q4 == 3))
                            k.op(act, lambda h, half=half, pTf=pTf: h.activation(out=xTf[:, half * 4:(half + 1) * 4, :].rearrange("p a b -> p (a b)"), in_=pTf[:], func=AF.Copy), reads=[pTf], writes=[xTf])
                        for kc in range(8):
                            k.op(pe, lambda h, kc=kc: h.matmul(plg[:, 0:NE], lhsT=xTf[:, kc, :], rhs=rtw[:, kc, :], start=(kc == 0), stop=(kc == 7)), reads=[xTf, rtw], writes=[plg], inc=(kc == 7))
                        k.op(dve, lambda h, j=j: h.tensor_scalar(out=lg[:, j, :], in0=plg[:, 0:NE], scalar1=rstd[:, j:j + 1], scalar2=None, op0=ALU.mult), reads=[plg, rstd], writes=[lg])
                    sh = [128, 4, NE]
                    k.op(dve, lambda h: h.tensor_reduce(out=m1[:], in_=lg[:], axis=AX.X, op=ALU.max), reads=[lg], writes=[m1])
                    k.op(dve, lambda h: h.tensor_tensor(out=msk[:], in0=lg[:], in1=bc(m1[:], sh, 2), op=ALU.is_equal), reads=[lg, m1], writes=[msk])
                    k.op(dve, lambda h: h.scalar_tensor_tensor(out=lg2[:], in0=msk[:], scalar=-1e30, in1=lg[:], op0=ALU.mult, op1=ALU.add), reads=[msk, lg], writes=[lg2])
                    k.op(dve, lambda h: h.tensor_reduce(out=m2[:], in_=lg2[:], axis=AX.X, op=ALU.max), reads=[lg2], writes=[m2])
                    k.op(dve, lambda h: h.tensor_tensor(out=msk[:], in0=lg[:], in1=bc(m2[:], sh, 2), op=ALU.is_ge), reads=[lg, m2], writes=[msk])
                    k.op(dve, lambda h: h.tensor_tensor(out=ex[:], in0=lg[:], in1=bc(m1[:], sh, 2), op=ALU.subtract), reads=[lg, m1], writes=[ex])
                    k.op(dve, lambda h: h.tensor_scalar(out=ex[:], in0=ex[:], scalar1=-80.0, scalar2=None, op0=ALU.max), reads=[ex], writes=[ex])
                    k.op(act, lambda h: h.activation(out=ex[:], in_=ex[:], func=AF.Exp), reads=[ex], writes=[ex])
                    k.op(dve, lambda h: h.tensor_tensor(out=ex[:], in0=ex[:], in1=msk[:], op=ALU.mult), reads=[ex, msk], writes=[ex])
                    k.op(dve, lambda h: h.tensor_reduce(out=den[:], in_=ex[:], axis=AX.X, op=ALU.add), reads=[ex], writes=[den])
                    k.op(dve, lambda h: h.reciprocal(out=den[:], in_=den[:]), reads=[den], writes=[den])
                    k.op(dve, lambda h: h.tensor_tensor(out=wr[:, 4 * c:4 * c + 4, :], in0=ex[:], in1=bc(den[:], sh, 2), op=ALU.mult), reads=[ex, den], writes=[wr])

                def loads(c):
                    i2 = c % 2
                    k.dma(sp, sgl[i2][:], SGA[c].t, reads=[SGA[c]], writes=[sgl[i2]])
                    k.dma(sp, mbl[i2][:], MB[c].t, reads=[MB[c]], writes=[mbl[i2]])
                    k.dma(sp, xins[i2][:], Xc[c].t, reads=[Xc[c]], writes=xins[i2].all())
                def ua_part(c):
                    sg_ = sgl[c % 2]; mb_ = mbl[c % 2]
                    for dc in range(8):
                        pu = pp.next(); tf = tmpf[dc % 2]
                        for pr in range(4):
                            k.op(pe, lambda h, pr=pr, dc=dc, pu=pu: h.matmul(pu[:], lhsT=Wupa[:, pr, dc * 128:(dc + 1) * 128], rhs=OT[:, pr, c * 512:(c + 1) * 512], start=(pr == 0), stop=(pr == 3)),
                                 reads=[Wupa, OT], writes=[pu], inc=(pr == 3))
                        k.op(dve, lambda h, dc=dc, pu=pu, tf=tf: h.tensor_tensor(out=tf[:], in0=pu[:], in1=sg_[:, dc, :], op=ALU.mult), reads=[pu, sg_], writes=[tf])
                        k.op(dve, lambda h, dc=dc, tf=tf: h.tensor_tensor(out=mg[:, dc, :], in0=tf[:], in1=mb_[:, dc, :], op=ALU.add), reads=[tf, mb_], writes=[mg.sub(dc)])

                def wo_part(c):
                    xin = xins[c % 2]
                    for j in range(4):
                        for nh in range(2):
                            py = pp.next()
                            for dc in range(8):
                                k.op(pe, lambda h, j=j, nh=nh, dc=dc, py=py: h.matmul(py[:], lhsT=mg[:, dc, j * 128:(j + 1) * 128], rhs=Wo[:, dc, nh * 512:(nh + 1) * 512], start=(dc == 0), stop=(dc == 7)),
                                     reads=[mg.sub(dc), Wo], writes=[py], inc=(dc == 7))
                            k.op(dve, lambda h, j=j, nh=nh, py=py: h.tensor_tensor(out=xin[:, j, nh * 512:(nh + 1) * 512], in0=py[:], in1=xin[:, j, nh * 512:(nh + 1) * 512], op=ALU.add), reads=[py, xin.sub(j)], writes=[xin.sub(j)])

                def tail_part(c):
                    xin = xins[c % 2]
                    k.dma(sp, Y_c[c].t, xin[:], reads=xin.all(), writes=[Y_c[c]])
                    rms_T(xin, ssq, rstd, hb, hT, pTs, junk)
                    k.dma(sp, H2T[c].t, hT[:], reads=hT.all(), writes=[H2T[c]])
                    if L == 1:
                        route(c, xin)

                loads(0)
                ua_part(0)
                for c in range(NCH):
                    if c + 1 < NCH:
                        loads(c + 1)
                    wo_part(c)
                    if c + 1 < NCH:
                        ua_part(c + 1)
                    tail_part(c)
                end_phase()
            st23.close()
            if stop_after == f"P3{L}":
                break

            if L == 0:
                units = [dict(wg=I["wg"], wu=I["wu"], wd=I["wd"], f0=f0, nf=nf, e=None) for (f0, nf) in ((0, 8), (8, 7), (15, 7))]
            else:
                units = [dict(wg=I["mwg"][e], wu=I["mwu"][e], wd=I["mwd"][e], f0=q4 * 7, nf=7, e=e) for e in range(NE) for q4 in range(4)]
            NF = 8
            with ExitStack() as st:
                stg = PsPool([k.sb([128, 1024], F32, f"stg{i}", st) for i in range(4)])
                g2 = k.sb([128, 8], F32, "g2", st)
                k.dma(sp, g2[:], I[f"g2{L}"], writes=[g2])
                Wsets = []
                for i in range(2):
                    Wg_t = k.sb([128, 8, NF * 128], BF16, f"Wg{i}", st); Wu_t = k.sb([128, 8, NF * 128], BF16, f"Wu{i}", st)
                    Wd_t = k.sb([128, NF, 1024], BF16, f"Wd{i}", st)
                    Wsets.append((Wg_t, Wu_t, Wd_t))
                xin = k.sb([128, 4, 1024], F32, "xin", st)
                hTs = [k.sb([128, 8, 512], BF16, f"hT{i}", st) for i in range(2)]
                acts = [k.sb([128, NF, 512], BF16, f"actb{i}", st) for i in range(2)]
                sgt = [k.sb([128, 512], F32, f"sgt{i}", st) for i in range(2)]
                pp = PsPool([k.ps([128, 512], F32, f"pp{i}", st) for i in range(8)])

                def loads(u, c):
                    i2 = (u * NCH + c) % 2
                    k.dma(sp, hTs[i2][:], H2T[c].t, reads=[H2T[c]], writes=[hTs[i2]])

                def loadx(c):
                    k.dma(sp, xin[:], Y_c[c].t, reads=[Y_c[c]], writes=[xin])

                def unit_pieces(u):
                    un = units[u]; nf = un["nf"]; c0 = un["f0"] * 128
                    Wg_t, Wu_t, Wd_t = Wsets[u % 2]
                    for (src, Wt) in ((un["wg"], Wg_t), (un["wu"], Wu_t)):
                        for kc in range(8):
                            sg = stg.next()
                            k.dma(pool, sg[:, 0:nf * 128], src[kc * 128:(kc + 1) * 128, c0:c0 + nf * 128], writes=[sg])
                            cast(Wt[:, kc, 0:nf * 128], sg[:, 0:nf * 128], g2[:, kc:kc + 1], [sg, g2], [Wt])
                            yield
                    for f in range(nf):
                        sg = stg.next()
                        k.dma(pool, sg[:, 0:1024], un["wd"][c0 + f * 128:c0 + (f + 1) * 128, :], writes=[sg])
                        cast(Wd_t[:, f, :], sg[:, 0:1024], None, [sg], [Wd_t])
                        yield

                for _ in unit_pieces(0):
                    pass
                loads(0, 0)
                loadx(0)
                for u, un in enumerate(units):
                    nf = un["nf"]; e = un["e"]
                    Wg_t, Wu_t, Wd_t = Wsets[u % 2]
                    nxt = unit_pieces(u + 1) if u + 1 < len(units) else iter(())
                    for c in range(NCH):
                        i2 = (u * NCH + c) % 2
                        nu, ncn = (u, c + 1) if c + 1 < NCH else (u + 1, 0)
                        if nu < len(units):
                            loads(nu, ncn)
                        hT = hTs[i2]; ab = acts[i2]
                        for f in range(nf):
                            pg = pp.next(); pu = pp.next(); sg_ = sgt[f % 2]
                            for (p_, Wt) in ((pg, Wg_t), (pu, Wu_t)):
                                for kc in range(8):
                                    k.op(pe, lambda h, kc=kc, f=f, p_=p_, Wt=Wt, hT=hT: h.matmul(p_[:], lhsT=Wt[:, kc, f * 128:(f + 1) * 128], rhs=hT[:, kc, :], start=(kc == 0), stop=(kc == 7)),
                                         reads=[Wt, hT], writes=[p_], inc=(kc == 7))
                            k.op(act, lambda h, pg=pg, sg_=sg_: h.activation(out=sg_[:], in_=pg[:], func=AF.Silu), reads=[pg], writes=[sg_])
                            k.op(dve, lambda h, f=f, pu=pu, sg_=sg_, ab=ab: h.tensor_tensor(out=ab[:, f, :], in0=pu[:], in1=sg_[:], op=ALU.mult), reads=[pu, sg_], writes=[ab.sub(f)])
                            if f % 2 == 1:
                                next(nxt, None)
                        for j in range(4):
                            for nh in range(2):
                                py = pp.next()
                                for f in range(nf):
                                    k.op(pe, lambda h, j=j, nh=nh, f=f, py=py, ab=ab, nf=nf, Wd_t=Wd_t: h.matmul(py[:], lhsT=ab[:, f, j * 128:(j + 1) * 128], rhs=Wd_t[:, f, nh * 512:(nh + 1) * 512], start=(f == 0), stop=(f == nf - 1)),
                                         reads=[ab.sub(f), Wd_t], writes=[py], inc=(f == nf - 1))
                                xs_ = xin[:, j, nh * 512:(nh + 1) * 512]
                                if e is None:
                                    k.op(dve, lambda h, py=py, xs_=xs_: h.tensor_tensor(out=xs_, in0=py[:], in1=xs_, op=ALU.add), reads=[py, xin], writes=[xin])
                                else:
                                    t = 4 * c + j
                                    k.op(dve, lambda h, py=py, xs_=xs_, t=t, e=e: h.scalar_tensor_tensor(out=xs_, in0=py[:], scalar=wr[:, t, e:e + 1], in1=xs_, op0=ALU.mult, op1=ALU.add),
                                         reads=[py, xin, wr], writes=[xin])
                        k.dma(sp, Y_c[c].t, xin[:], reads=[xin], writes=[Y_c[c]])
                        if nu < len(units):
                            loadx(ncn)
                    for _ in nxt:
                        pass
                end_phase()
            if stop_after == f"P5{L}":
                break

        if debug:
            srcs = dict(h1t=H1T_t, h2t=H2T_t, qt=QT_t, kt=KT_t, vv=V_t, sga=SGA_t, mb=MB_t)
            with ExitStack() as st:
                for nm in debug:
                    if nm in srcs:
                        k.dma(sp, dbg_out[nm], srcs[nm], reads=H1T + H2T + QT + KT + Vd + SGA + MB)
                    elif nm == "wr":
                        k.dma(sp, dbg_out[nm], wr[:], reads=[wr])
                    elif nm == "rope":
                        k.dma(sp, dbg_out[nm][:, 0], cost[:], reads=[cost]); k.dma(sp, dbg_out[nm][:, 1], sint[:], reads=[sint])
                end_phase()
    return nc


def host_inputs(inp):
    f32 = np.float32
    A = lambda a: np.ascontiguousarray(np.asarray(a))
    com = {}
    com["ident"] = np.eye(128, dtype=f32)
    com["tri"] = np.triu(np.ones((128, 128), dtype=f32))
    invf = (np.float32(10000.0) ** (-np.arange(0, 32, 2, dtype=f32) / np.float32(32))).astype(f32)
    com["invf"] = A(np.broadcast_to(invf[None, :], (128, 16)))
    pk = lambda v, n: A(np.asarray(v, dtype=f32).reshape(n, 128).T)
    for L in range(2):
        com[f"w_in{L}"] = A(inp["w_in"][L]); com[f"g1{L}"] = pk(inp["norm1_g"][L], 8)
        com[f"w_uq{L}"] = A(inp["w_uq"][L]); com[f"qng{L}"] = pk(inp["q_norm_g"][L], 2)
        com[f"w_ukv{L}"] = A(inp["w_ukv"][L]); com[f"kvng{L}"] = pk(inp["kv_norm_g"][L], 1)
        com[f"qkg{L}"] = A(np.broadcast_to(np.concatenate([inp["qk_q_g"][L], inp["qk_k_g"][L]])[None, :], (128, 192)))
        com[f"w_upa{L}"] = A(inp["w_up_attn"][L])
        com[f"convw{L}"] = A(np.asarray(inp["conv_w"][L]).reshape(4, 4, 128).transpose(2, 1, 0))
        lv = np.stack([inp["conv_b"][L], inp["b_rg"][L], inp["b_ig"][L], inp["lru_lambda"][L]], axis=-1)
        com[f"lruv{L}"] = A(lv.reshape(4, 128, 4).transpose(1, 0, 2))
        for nm, key in ((f"w_rg{L}", "w_rg"), (f"w_ig{L}", "w_ig")):
            w = np.asarray(inp[key][L])
            bd = np.zeros((128, 4, 128), dtype=f32)
            for fc in range(4):
                bd[0:64, fc, 0:64] = w[2 * fc]; bd[64:128, fc, 64:128] = w[2 * fc + 1]
            com[nm] = bd
        com[f"w_upl{L}"] = A(inp["w_up_lru"][L]); com[f"w_o{L}"] = A(inp["w_o"][L]); com[f"g2{L}"] = pk(inp["norm2_g"][L], 8)
    com["wg"] = A(inp["ffn_w_gate"][0]); com["wu"] = A(inp["ffn_w_up"][0]); com["wd"] = A(inp["ffn_w_down"][0])
    com["mwg"] = A(inp["moe_w_gate"][0]); com["mwu"] = A(inp["moe_w_up"][0]); com["mwd"] = A(inp["moe_w_down"][0])
    com["rtw"] = A(np.asarray(inp["moe_router"][0], dtype=f32).reshape(8, 128, NE).transpose(1, 0, 2))
    maps = []
    for b in range(8):
        m = dict(com)
        m["x"] = A(inp["x"][b]); m["pos"] = A(np.asarray(inp["positions"][b], dtype=np.int32).reshape(NT, 128).T)
        maps.append(m)
    return maps


def kernel(**inputs):
    nc = build()
    maps = host_inputs(inputs)
    res = run_bass_kernel_spmd(nc, maps, core_ids=list(range(8)))
    return np.stack([np.asarray(r["y"], dtype=np.float32).reshape(S, D) for r in res.results], axis=0)
```
